# Optimizing a Trainium2 kernel written in Bass

```python
import math
import jax, jax.numpy as jnp
from jax import lax
import numpy as np


D_MODEL = 1024
BATCH = 8
SEQ = 4096
DEPTH = 4

CHUNK = 64
A_HEADS = 4
A_HEAD_DIM = 64
A_WIDTH = A_HEADS * A_HEAD_DIM
SGU_BLOCK = 128
B_WIDTH = 256
CONV_WIDTH = 31
C_HEADS = 8
C_HEAD_DIM = 64
C_WIDTH = C_HEADS * C_HEAD_DIM
DECAY_LORA = 64
ICLR_LORA = 64
MIX_WIDTH = A_WIDTH + B_WIDTH + C_WIDTH
PLE_DIM = 256
C_SHIFT_COLS = 3 * C_WIDTH + DECAY_LORA + ICLR_LORA
IN_COLS = 3 * A_WIDTH + 3 * B_WIDTH + C_SHIFT_COLS + C_WIDTH
RMS_EPS = 1e-6
LN_EPS = 1e-5
GN_EPS = 64e-5
DECAY_SCALE = math.exp(-0.5)

kernel_name = "hybrid_sgu_conformer_rwkv7_stream"


def rms_norm(x, g):
    xf = x.astype(jnp.float32)
    y = xf * lax.rsqrt(jnp.mean(xf * xf, axis=-1, keepdims=True) + RMS_EPS)
    return (y * g.astype(jnp.float32)).astype(x.dtype)


def layer_norm(x, g, b):
    xf = x.astype(jnp.float32)
    mu = jnp.mean(xf, axis=-1, keepdims=True)
    var = jnp.mean(jnp.square(xf - mu), axis=-1, keepdims=True)
    y = (xf - mu) * lax.rsqrt(var + LN_EPS)
    return (y * g.astype(jnp.float32) + b.astype(jnp.float32)).astype(x.dtype)


def chunk_causal_mask(n):
    ci = jnp.arange(n)[:, None] // CHUNK
    cj = jnp.arange(n)[None, :] // CHUNK
    return cj <= ci


def spatial_gating(u, v, ln_g, ln_b, w_s, b_s):
    bsz, seq, _ = v.shape
    v = layer_norm(v, ln_g, ln_b)
    vb = v.reshape(bsz, seq // SGU_BLOCK, SGU_BLOCK, A_HEADS, A_HEAD_DIM)
    w = jnp.where(chunk_causal_mask(SGU_BLOCK)[None], w_s, jnp.zeros_like(w_s))
    mixed = jnp.einsum('hij,bnjhd->bnihd', w, vb) + b_s.T[None, None, :, :, None]
    return u * mixed.reshape(bsz, seq, A_WIDTH)


def conv_module(val, glu_gate, conv_w, conv_b, ln_g, ln_b, pw_w, pw_b):
    y = val * jax.nn.sigmoid(glu_gate)
    y = lax.conv_general_dilated(
        y, conv_w[:, None, :], window_strides=(1,), padding=[(CONV_WIDTH - 1, 0)],
        dimension_numbers=('NWC', 'WIO', 'NWC'), feature_group_count=B_WIDTH) + conv_b
    y = jax.nn.silu(layer_norm(y, ln_g, ln_b))
    return y @ pw_w + pw_b


def token_shift(z, mu):
    prev = jnp.pad(z, ((0, 0), (1, 0), (0, 0)))[:, :-1]
    return z + mu * (prev - z)


def rwkv7_scan(r, w, k, v, a, b):
    bsz = r.shape[0]

    def step(state, inp):
        r_t, w_t, k_t, v_t, a_t, b_t = inp
        sa = jnp.einsum('bhvk,bhk->bhv', state, a_t)
        state = (state * w_t[:, :, None, :] + sa[..., None] * b_t[:, :, None, :]
                 + v_t[..., None] * k_t[:, :, None, :])
        y_t = jnp.einsum('bhvk,bhk->bhv', state, r_t)
        return state, y_t

    xs = (r.transpose(1, 0, 2, 3), w.transpose(1, 0, 2, 3), k.transpose(1, 0, 2, 3),
          v.transpose(1, 0, 2, 3), a.transpose(1, 0, 2, 3), b.transpose(1, 0, 2, 3))
    init = jnp.zeros((bsz, C_HEADS, C_HEAD_DIM, C_HEAD_DIM), jnp.float32)
    _, ys = lax.scan(step, init, xs)
    return ys.transpose(1, 0, 2, 3)


def rwkv7_mix(z, mu, w0, w_up, a0, a_up, k_k, k_a, r_k, lnx_g, lnx_b):
    bsz, seq, _ = z.shape
    f32 = jnp.float32
    z = token_shift(z, mu)
    r = z[..., :C_WIDTH]
    k = z[..., C_WIDTH:2 * C_WIDTH]
    v = z[..., 2 * C_WIDTH:3 * C_WIDTH]
    wd = z[..., 3 * C_WIDTH:3 * C_WIDTH + DECAY_LORA]
    ad = z[..., 3 * C_WIDTH + DECAY_LORA:]
    log_w = -DECAY_SCALE * jax.nn.sigmoid((w0 + jnp.tanh(wd) @ w_up).astype(f32))
    a = jax.nn.sigmoid((a0 + ad @ a_up).astype(f32))

    def heads(t):
        return t.astype(f32).reshape(bsz, seq, C_HEADS, C_HEAD_DIM)

    def hvec(t):
        return t.astype(f32).reshape(C_HEADS, C_HEAD_DIM)

    r, k, v, w, a = heads(r), heads(k), heads(v), jnp.exp(heads(log_w)), heads(a)
    kk = k * hvec(k_k)
    kk = kk * lax.rsqrt(jnp.maximum(jnp.sum(kk * kk, axis=-1, keepdims=True), 1e-12))
    k = k * (1.0 + (a - 1.0) * hvec(k_a))
    y = rwkv7_scan(r, w, k, v, -kk, kk * a)
    mu_y = jnp.mean(y, axis=-1, keepdims=True)
    var_y = jnp.mean(jnp.square(y - mu_y), axis=-1, keepdims=True)
    y = (y - mu_y) * lax.rsqrt(var_y + GN_EPS) * hvec(lnx_g) + hvec(lnx_b)
    y = y + jnp.sum(r * k * r_k.astype(f32), axis=-1, keepdims=True) * v
    return y.reshape(bsz, seq, C_WIDTH).astype(z.dtype)


def setup_inputs(seed: int = 0) -> dict:
    key = jax.random.key(seed)
    ks = jax.random.split(key, 40)
    f32 = jnp.float32

    def nrm(k, shape, scale):
        return jax.random.normal(k, shape, f32) * scale

    def gain(k, shape):
        return 1.0 + 0.05 * jax.random.normal(k, shape, f32)

    L = DEPTH
    return {
        'x': nrm(ks[0], (BATCH, SEQ, D_MODEL), 1.0),
        'p': nrm(ks[1], (DEPTH, BATCH, SEQ, PLE_DIM), 1.0),
        'pre_norm_g': gain(ks[2], (L, D_MODEL)),
        'w_in': nrm(ks[3], (L, D_MODEL, IN_COLS), D_MODEL ** -0.5),
        'sgu_ln_g': gain(ks[4], (L, A_WIDTH)),
        'sgu_ln_b': nrm(ks[5], (L, A_WIDTH), 0.01),
        'sgu_w': nrm(ks[6], (L, A_HEADS, SGU_BLOCK, SGU_BLOCK), SGU_BLOCK ** -0.5),
        'sgu_b': 1.0 + nrm(ks[7], (L, A_HEADS, SGU_BLOCK), 0.1),
        'conv_w': nrm(ks[8], (L, CONV_WIDTH, B_WIDTH), CONV_WIDTH ** -0.5),
        'conv_b': nrm(ks[9], (L, B_WIDTH), 0.01),
        'conv_ln_g': gain(ks[10], (L, B_WIDTH)),
        'conv_ln_b': nrm(ks[11], (L, B_WIDTH), 0.01),
        'pw_w': nrm(ks[12], (L, B_WIDTH, B_WIDTH), B_WIDTH ** -0.5),
        'pw_b': nrm(ks[13], (L, B_WIDTH), 0.01),
        'shift_mu': jax.random.uniform(ks[14], (L, C_SHIFT_COLS), f32),
        'w0': nrm(ks[15], (L, C_WIDTH), 0.5),
        'w_up': nrm(ks[16], (L, DECAY_LORA, C_WIDTH), 0.5 * DECAY_LORA ** -0.5),
        'a0': nrm(ks[17], (L, C_WIDTH), 0.5),
        'a_up': nrm(ks[18], (L, ICLR_LORA, C_WIDTH), 0.5 * ICLR_LORA ** -0.5),
        'k_k': 0.85 + nrm(ks[19], (L, C_WIDTH), 0.05),
        'k_a': gain(ks[20], (L, C_WIDTH)),
        'r_k': nrm(ks[21], (L, C_HEADS, C_HEAD_DIM), 0.1),
        'lnx_g': gain(ks[22], (L, C_WIDTH)),
        'lnx_b': nrm(ks[23], (L, C_WIDTH), 0.01),
        'w_out': nrm(ks[24], (L, MIX_WIDTH, D_MODEL), MIX_WIDTH ** -0.5),
        'post_norm_g': gain(ks[25], (L, D_MODEL)),
        'ple_w': nrm(ks[26], (L, PLE_DIM, D_MODEL), PLE_DIM ** -0.5),
        'ple_gate_w': nrm(ks[27], (L, D_MODEL, D_MODEL), D_MODEL ** -0.5),
        'ple_gate_b': nrm(ks[28], (L, D_MODEL), 0.01),
    }


def reference(x, p, pre_norm_g, w_in, sgu_ln_g, sgu_ln_b, sgu_w, sgu_b, conv_w, conv_b,
              conv_ln_g, conv_ln_b, pw_w, pw_b, shift_mu, w0, w_up, a0, a_up, k_k, k_a,
              r_k, lnx_g, lnx_b, w_out, post_norm_g, ple_w, ple_gate_w, ple_gate_b):
    oa = 0
    ob = 3 * A_WIDTH
    oc = ob + 3 * B_WIDTH
    og = oc + C_SHIFT_COLS
    for i in range(DEPTH):
        h = rms_norm(x, pre_norm_g[i])
        z = h @ w_in[i]
        u_a = z[..., oa:oa + A_WIDTH]
        v_a = z[..., oa + A_WIDTH:oa + 2 * A_WIDTH]
        g_a = z[..., oa + 2 * A_WIDTH:ob]
        out_a = spatial_gating(u_a, v_a, sgu_ln_g[i], sgu_ln_b[i], sgu_w[i], sgu_b[i]) * jax.nn.silu(g_a)
        val_b = z[..., ob:ob + B_WIDTH]
        glu_b = z[..., ob + B_WIDTH:ob + 2 * B_WIDTH]
        g_b = z[..., ob + 2 * B_WIDTH:oc]
        out_b = conv_module(val_b, glu_b, conv_w[i], conv_b[i], conv_ln_g[i], conv_ln_b[i],
                            pw_w[i], pw_b[i]) * jax.nn.silu(g_b)
        out_c = rwkv7_mix(z[..., oc:og], shift_mu[i], w0[i], w_up[i], a0[i], a_up[i], k_k[i],
                          k_a[i], r_k[i], lnx_g[i], lnx_b[i]) * jax.nn.silu(z[..., og:])
        mix = jnp.concatenate([out_a, out_b, out_c], axis=-1) @ w_out[i]
        x = x + rms_norm(mix, post_norm_g[i])
        gate = jax.nn.sigmoid(x @ ple_gate_w[i] + ple_gate_b[i])
        x = x + (p[i] @ ple_w[i]) * gate
    return x
```

```python
import math
from contextlib import ExitStack

import numpy as np
import concourse.bass as bass
import concourse.mybir as mybir
from concourse.bass_utils import run_bass_kernel_spmd

F32 = mybir.dt.float32
BF16 = mybir.dt.bfloat16
AF = mybir.ActivationFunctionType
ALU = mybir.AluOpType

D = 1024
SEQ = 4096
NLAYER = 4
NCORE = 8
AW = 256
BW = 256
CW = 512
PLE = 256
CONVW = 31
HALO = CONVW - 1
INC = 3712
NOC = INC // 128
CH = 64
RMS_EPS = 1e-6
LN_EPS = 1e-5
GN_EPS = 64e-5
DECAY = math.exp(-0.5)

V_PREG, V_POSTG, V_GATEB = 0, 8, 16
V_SLG, V_SLB = 24, 26
V_CB, V_CLG, V_CLB, V_PWB = 28, 30, 32, 34
V_MU = 36
V_W0, V_A0, V_KK, V_KA, V_RK, V_LXG, V_LXB = 49, 53, 57, 61, 65, 69, 73
V_CW = 77
NV = V_CW + 2 * CONVW
DV_OMM, DV_OMKA = 0, 13
NDV = 17
C_ID, C_ONES, C_BLK, C_MSU, C_MSL, C_MUI, C_PAD, C_NPAD, C_RESET = 0, 128, 256, 384, 512, 640, 704, 706, 708


class Sem:
    def __init__(self, h):
        self.h = h
        self.cnt = 0


class Buf:
    __slots__ = ("name", "w", "r", "excl")

    def __init__(self, name, excl=False):
        self.name = name
        self.w = None
        self.r = {}
        self.excl = excl


class Eng:
    def __init__(self, name, sem):
        self.name = name
        self.sem = sem
        self.seen = {}
        self.items = []


class Sched:
    def __init__(self, nc, es):
        self.nc = nc
        self.es = es
        self.eng = {}
        for n in ("pe", "act", "dve", "pool", "sp"):
            self.eng[n] = Eng(n, self.new_sem("s_" + n))
        self.nops = 0

    def new_sem(self, name):
        return Sem(self.es.enter_context(self.nc.semaphore(name)))

    def _need(self, E, ev):
        sem, val = ev
        if E.seen.get(sem, 0) >= val:
            return
        E.items.append(("w", sem, val))
        E.seen[sem] = val

    def op(self, en, fn, reads=(), writes=(), dsem=None, force=False):
        import os
        lim = int(os.environ.get("KLIMIT", "0"))
        if lim and self.nops >= lim and not force:
            return None
        E = self.eng[en]
        if any(b.excl for b in reads):
            writes = list(writes) + [b for b in reads if b.excl]
            reads = [b for b in reads if not b.excl]
        for b in reads:
            if b.w is not None:
                if b.w[0] is E.sem and en == "pe":
                    continue
                self._need(E, b.w)
        for b in writes:
            if b.w is not None and b.w[0] is not E.sem:
                self._need(E, b.w)
            for sem, val in b.r.items():
                if sem is not E.sem:
                    self._need(E, (sem, val))
        if dsem is None:
            sem = E.sem
            sem.cnt += 1
            inc = 1
        else:
            sem = dsem
            sem.cnt += 16
            inc = 16
        ev = (sem, sem.cnt)
        E.items.append(("o", fn, sem, inc))
        for b in reads:
            b.r[sem] = sem.cnt
        for b in writes:
            b.w = ev
            b.r = {}
        self.nops += 1
        return ev

    def wait(self, en, ev):
        self._need(self.eng[en], ev)

    def replay(self, en, e):
        for it in self.eng[en].items:
            if it[0] == "w":
                e.wait_ge(it[1].h, it[2])
            else:
                it[1](e).then_inc(it[2].h, it[3])


class PsTile:
    def __init__(self, ap, bufs):
        self.ap = ap
        self.bufs = bufs


class Prog:
    def __init__(self, NL, NT, T, wdt=F32, sdt=F32, nslot=2, ugroup=4, stage=99):
        self.stage = stage
        self.NL, self.NT, self.T = NL, NT, T
        self.S = NT * T
        self.NCH = T // CH
        self.NBLK = T // 128
        self.wdt, self.sdt = wdt, sdt
        self.nslot = nslot
        self.ugroup = ugroup
        self.NC = C_RESET + T
        self.nc = bass.Bass("TRN2", target_bir_lowering=False)
        self.es = ExitStack()

    def dram(self, name, shape, kind="ExternalInput", dt=F32):
        return self.nc.dram_tensor(name, list(shape), dt, kind=kind).ap()

    def sb(self, name, shape, dt=F32):
        t = self.es.enter_context(self.nc.sbuf_tensor("sb_" + name, list(shape), dt))
        return t

    def build(self):
        nc, NL, T, S = self.nc, self.NL, self.T, self.S
        es = self.es
        with es:
            self.sc = Sched(nc, es)
            self.d_x = self.dram("xT", [D, S])
            self.d_p = self.dram("pT", [NL, PLE, S])
            self.d_win = self.dram("win", [NL, NOC, 128, 1024])
            self.d_wout = self.dram("wout", [NL, 8, 128, 1024])
            self.d_gw = self.dram("gw", [NL, 8, 128, 1024])
            self.d_plew = self.dram("plew", [NL, 8, 128, 256])
            self.d_vecs = self.dram("vecs", [128, NL * NV])
            self.d_sguT = self.dram("sguT", [NL, 128, 512])
            self.d_sgub = self.dram("sgub", [NL, 128, 256])
            self.d_pww = self.dram("pww", [NL, 128, 512])
            self.d_lora = self.dram("lora", [NL, 128, 1024])
            self.d_consts = self.dram("consts", [128, self.NC])
            self.d_y = self.dram("yT", [D, S], kind="ExternalOutput")
            self.alloc()
            self.emit()
            with nc.Block() as block:
                @block.tensor
                def _(e):
                    self.sc.replay("pe", e)

                @block.scalar
                def _(e):
                    self.sc.replay("act", e)

                @block.vector
                def _(e):
                    self.sc.replay("dve", e)

                @block.gpsimd
                def _(e):
                    self.sc.replay("pool", e)

                @block.sync
                def _(e):
                    self.sc.replay("sp", e)
        return nc

    def alloc(self):
        NL, T, NCH = self.NL, self.T, self.NCH
        sb = self.sb
        B = Buf
        self.xt = sb("xt", [128, 8, T]); self.b_xt = [B(f"xt{c}") for c in range(8)]
        self.hT = sb("hT", [128, 8, T], self.wdt); self.b_hT = [B(f"hT{c}") for c in range(8)]
        self.zs = sb("zs", [128, 13, T]); self.b_zs = [B(f"zs{c}") for c in range(13)]
        self.va = sb("va", [128, 2, T]); self.b_va = [B(f"va{c}") for c in range(2)]
        self.mixT = sb("mixT", [128, 8, T], self.wdt); self.b_mix = [B(f"mix{c}") for c in range(8)]
        self.mo = sb("mo", [128, 8, T]); self.b_mo = [B(f"mo{c}") for c in range(8)]
        self.wring = [sb(f"wr{i}", [128, 1024], self.wdt) for i in range(self.nslot)]
        self.b_wring = [B(f"wr{i}") for i in range(self.nslot)]
        self.s_wring = [self.sc.new_sem(f"swr{i}") for i in range(self.nslot)]
        self.wptr = 0
        self.NST = 10
        self.st = [sb(f"st{i}", [128, T]) for i in range(self.NST)]
        self.b_st = [B(f"st{i}") for i in range(self.NST)]
        self.stptr = 0
        self.pt = sb("pt", [128, 2, T], self.wdt); self.b_pt = B("pt"); self.s_pt = self.sc.new_sem("spt")
        self.vecs = sb("vecs", [128, NL * NV]); self.b_vecs = B("vecs")
        self.dv = sb("dv", [128, NL * NDV]); self.b_dv = B("dv")
        self.consts = sb("consts", [128, self.NC]); self.b_consts = B("consts")
        self.s_misc = self.sc.new_sem("smisc")
        self.s_x = self.sc.new_sem("sx")
        self.s_y = self.sc.new_sem("sy")
        self.lp = sb("lp", [128, 512 + 256 + 512 + 1024]); self.b_lp = B("lp"); self.s_lp = self.sc.new_sem("slp")
        self.ybuf = sb("ybuf", [128, 2, HALO + T]); self.b_ybuf = [B(f"ybuf{c}") for c in range(2)]
        self.halo = sb("halo", [128, NL * 2 * HALO]); self.b_halo = B("halo")
        self.zlast = sb("zlast", [128, NL * 13]); self.b_zlast = B("zlast")
        self.NDG = 8
        self.diag = [sb(f"dg{i}", [128, 128]) for i in range(self.NDG)]
        self.b_diag = [B(f"dg{i}") for i in range(self.NDG)]
        self.dgptr = 0
        self.state = sb("state", [128, NL * 4 * 128], self.sdt)
        self.b_state = [[B(f"state{l}_{p}") for p in range(4)] for l in range(NL)]
        self.vn = sb("vn", [128, 2, T]); self.b_vn = [B(f"vn{c}") for c in range(2)]
        self.vntok = [sb(f"vntok{hh}", [128, self.NBLK, 256]) for hh in range(2)]; self.b_vntok = [B(f"vntok{b}") for b in range(self.NBLK)]
        self.ug = sb("ug", [128, 2, T]); self.b_ug = [B(f"ug{c}") for c in range(2)]
        self.sgb = sb("sgb", [128, 2, T]); self.b_sgb = [B(f"sgb{c}") for c in range(2)]
        self.yc = sb("yc", [128, 2, T]); self.b_yc = [B(f"yc{c}") for c in range(2)]
        self.yn = sb("yn", [128, 2, T]); self.b_yn = [B(f"yn{c}") for c in range(2)]
        self.sgc = sb("sgc", [128, 4, T]); self.b_sgc = [B(f"sgc{c}") for c in range(4)]
        names = ["lw", "aa", "cum", "cume", "E1", "E2", "E3", "kk0", "kkn", "kmod", "bvec", "yy"]
        self.pt_ = {n: sb("c_" + n, [128, T]) for n in names}
        self.b_pt_ = {n: B("c_" + n) for n in names}
        self.Rt = sb("Rt", [128, T], self.sdt); self.b_Rt = B("Rt")
        self.pads = {n: sb("pad_" + n, [128, NCH * 128], self.sdt) for n in ("A", "B", "K", "V")}
        self.b_pads = {n: [B(f"pad_{n}{j}") for j in range(NCH)] for n in ("A", "B", "K", "V")}
        self.NU = self.ugroup * 2
        self.ut = []
        for u in range(self.NU):
            d = {}
            for n, w in (("A0", 128), ("A1", 128), ("B0", 128), ("B1", 128), ("MakT", 128), ("MrbT", 64), ("MrkT", 64),
                         ("Z0", 256), ("Z1", 256), ("Btok", 128), ("Ktok", 128), ("Vbd", 128), ("Rp", 64), ("G0", 128)):
                d[n] = (sb(f"u{u}_{n}", [128, w], self.sdt), B(f"u{u}_{n}"))
            self.ut.append(d)
        self.psb = [self.es.enter_context(self.nc.psum_tensor(f"ps{b}", [128, 512], F32)) for b in range(8)]
        self.b_psq = [B(f"psbank{b}", excl=True) for b in range(8)]
        self.psptr = 0

    def ps(self, ncols):
        b = self.psptr
        self.psptr = (b + 1) % 8
        return PsTile(self.psb[b][:, 0:ncols], [self.b_psq[b]])

    def stt(self):
        i = self.stptr
        self.stptr = (i + 1) % self.NST
        return self.st[i], self.b_st[i]

    def cst(self, off, n, rows=slice(0, 128)):
        return self.consts[rows, off:off + n]

    def vcol(self, l, col, rows=slice(0, 128)):
        return self.vecs[rows, l * NV + col:l * NV + col + 1]

    def dcol(self, l, col):
        return self.dv[:, l * NDV + col:l * NDV + col + 1]

    def emit(self):
        sc, NL, NT, T = self.sc, self.NL, self.NT, self.T
        sc.op("sp", lambda e: e.dma_start(out=self.consts[:], in_=self.d_consts), writes=[self.b_consts], dsem=self.s_misc)
        sc.op("sp", lambda e: e.dma_start(out=self.vecs[:], in_=self.d_vecs), writes=[self.b_vecs], dsem=self.s_misc)
        ev = (self.s_misc, self.s_misc.cnt)
        self.b_consts.w = ev
        self.b_vecs.w = ev
        for l in range(NL):
            sc.op("dve", lambda e, l=l: e.tensor_scalar(self.dv[:, l * NDV + DV_OMM:l * NDV + DV_OMM + 13],
                                                       self.vecs[:, l * NV + V_MU:l * NV + V_MU + 13], -1.0, 1.0, ALU.mult, ALU.add),
                  reads=[self.b_vecs], writes=[self.b_dv])
            sc.op("dve", lambda e, l=l: e.tensor_scalar(self.dv[:, l * NDV + DV_OMKA:l * NDV + DV_OMKA + 4],
                                                       self.vecs[:, l * NV + V_KA:l * NV + V_KA + 4], -1.0, 1.0, ALU.mult, ALU.add),
                  reads=[self.b_vecs], writes=[self.b_dv])
        sc.op("pool", lambda e: e.memset(self.state[:], 0.0), writes=[b for bl in self.b_state for b in bl])
        sc.op("pool", lambda e: e.memset(self.halo[:], 0.0), writes=[self.b_halo])
        sc.op("pool", lambda e: e.memset(self.zlast[:], 0.0), writes=[self.b_zlast])
        for n in ("A", "B", "K", "V"):
            sc.op("pool", lambda e, n=n: e.memset(self.pads[n][:], 0.0), writes=self.b_pads[n])
        for hh in range(2):
            sc.op("pool", lambda e, hh=hh: e.memset(self.vntok[hh][:], 0.0), writes=self.b_vntok)
        for ti in range(NT):
            t0 = ti * T
            sc.op("sp", lambda e, t0=t0: e.dma_start(out=self.xt[:], in_=self.d_x[:, t0:t0 + T].rearrange("(c p) t -> p c t", p=128)),
                  writes=self.b_xt, dsem=self.s_x)
            for l in range(NL):
                self.tile_layer(ti, l)
            ev = sc.op("sp", lambda e, t0=t0: e.dma_start(out=self.d_y[:, t0:t0 + T].rearrange("(c p) t -> p c t", p=128), in_=self.xt[:]),
                       reads=self.b_xt, dsem=self.s_y, force=True)
        sc.wait("sp", (self.s_y, self.s_y.cnt))

    def wload(self, src_ap, ncols=1024):
        i = self.wptr
        self.wptr = (i + 1) % self.nslot
        tile, buf, sem = self.wring[i], self.b_wring[i], self.s_wring[i]
        q = "sp" if self.wdt == F32 else "pool"
        self.sc.op(q, lambda e: e.dma_start(out=tile[:, 0:ncols], in_=src_ap), writes=[buf], dsem=sem)
        return tile, buf

    def rstd_from(self, src_ap, src_bufs, scale, eps, clamp=None):
        sc = self.sc
        t, b = self.stt()
        if clamp is not None:
            sc.op("dve", lambda e: e.tensor_scalar(t[:], src_ap, clamp, None, ALU.max), reads=src_bufs, writes=[b])
            sc.op("act", lambda e: e.activation(t[:], t[:], AF.Ln), reads=[b], writes=[b])
        else:
            sc.op("act", lambda e: e.activation(t[:], src_ap, AF.Ln, bias=float(eps), scale=scale), reads=src_bufs + [self.b_consts], writes=[b])
        sc.op("act", lambda e: e.activation(t[:], t[:], AF.Exp, scale=-0.5), reads=[b], writes=[b])
        return t, b

    def ln_stats(self, x_aps, x_bufs, ones_off, nfeat, eps):
        sc, T = self.sc, self.T
        ones = self.cst(ones_off, 128)
        n = len(x_aps)
        sqs = []
        for i in range(n):
            t, b = self.stt()
            sc.op("dve", lambda e, t=t, i=i: e.tensor_tensor(t[:], x_aps[i], x_aps[i], ALU.mult), reads=[x_bufs[i]], writes=[b])
            sqs.append((t, b))
        p1 = self.ps(T)
        for i in range(n):
            sc.op("pe", lambda e, i=i: e.matmul(p1.ap, ones, x_aps[i], start=(i == 0), stop=(i == n - 1)),
                  reads=[x_bufs[i], self.b_consts], writes=p1.bufs)
        p2 = self.ps(T)
        for i in range(n):
            sc.op("pe", lambda e, i=i: e.matmul(p2.ap, ones, sqs[i][0][:], start=(i == 0), stop=(i == n - 1)),
                  reads=[sqs[i][1], self.b_consts], writes=p2.bufs)
        mean, bm = self.stt()
        sc.op("act", lambda e: e.activation(mean[:], p1.ap, AF.Identity, scale=1.0 / nfeat), reads=p1.bufs, writes=[bm])
        msq, bq = self.stt()
        sc.op("dve", lambda e: e.tensor_tensor(msq[:], mean[:], mean[:], ALU.mult), reads=[bm], writes=[bq])
        var, bv = self.stt()
        sc.op("dve", lambda e: e.scalar_tensor_tensor(var[:], p2.ap, 1.0 / nfeat, msq[:], ALU.mult, ALU.subtract),
              reads=p2.bufs + [bq], writes=[bv])
        rs, brs = self.rstd_from(var[:], [bv], 1.0, eps)
        return mean, bm, rs, brs

    def tile_layer(self, ti, l):
        sc, T, NCH = self.sc, self.T, self.NCH
        t0 = ti * T
        wdt = self.wdt
        ident = self.cst(C_ID, 128)
        ones = self.cst(C_ONES, 128)
        sc.op("sp", lambda e: e.dma_start(out=self.lp[:, 0:512], in_=self.d_sguT[l]), writes=[self.b_lp], dsem=self.s_lp)
        sc.op("sp", lambda e: e.dma_start(out=self.lp[:, 512:768], in_=self.d_sgub[l]), writes=[self.b_lp], dsem=self.s_lp)
        sc.op("sp", lambda e: e.dma_start(out=self.lp[:, 768:1280], in_=self.d_pww[l]), writes=[self.b_lp], dsem=self.s_lp)
        sc.op("sp", lambda e: e.dma_start(out=self.lp[:, 1280:2304], in_=self.d_lora[l]), writes=[self.b_lp], dsem=self.s_lp)
        sguT = self.lp[:, 0:512]
        sc.op("pool", lambda e: e.memset(self.lp[64:128, 0:512].rearrange("p (h i) -> p h i", h=4)[:, :, 0:64], 0.0),
              reads=[self.b_lp], writes=[self.b_lp])
        qd = "sp" if wdt == F32 else "pool"
        sc.op(qd, lambda e: e.dma_start(out=self.pt[:], in_=self.d_p[l, :, t0:t0 + T].rearrange("(c p) t -> p c t", p=128)),
              writes=[self.b_pt], dsem=self.s_pt)

        if self.stage < 1:
            return
        sqs0 = []
        for c in range(8):
            t, b = self.stt()
            sc.op("act", lambda e, t=t, c=c: e.activation(t[:], self.xt[:, c, :], AF.Square), reads=[self.b_xt[c]], writes=[b])
            sqs0.append((t, b))
        pss0 = self.ps(T)
        for c in range(8):
            sc.op("pe", lambda e, c=c: e.matmul(pss0.ap, ones, sqs0[c][0][:], start=(c == 0), stop=(c == 7)),
                  reads=[sqs0[c][1], self.b_consts], writes=pss0.bufs)
        rs0, brs0 = self.rstd_from(pss0.ap, pss0.bufs, 1.0 / D, RMS_EPS)
        for c in range(8):
            sc.op("dve", lambda e, c=c: e.scalar_tensor_tensor(self.hT[:, c, :], self.xt[:, c, :], self.vcol(l, V_PREG + c), rs0[:], ALU.mult, ALU.mult),
                  reads=[self.b_xt[c], brs0, self.b_vecs], writes=[self.b_hT[c]])

        def zchunk(oc):
            wt, wb = self.wload(self.d_win[l, oc])
            p = self.ps(T)
            import os
            if os.environ.get("KDUP"):
                sc.op("pe", lambda e: e.matmul(p.ap, wt[:, 0:128], self.hT[:, 0, :], start=True, stop=True), reads=[wb, self.b_hT[0]], writes=p.bufs)
            for kc in range(8):
                sc.op("pe", lambda e, kc=kc: e.matmul(p.ap, wt[:, kc * 128:(kc + 1) * 128], self.hT[:, kc, :], start=(kc == 0), stop=(kc == 7)),
                      reads=[wb, self.b_hT[kc]], writes=p.bufs)
            return p

        if self.stage < 2.1:
            if self.stage == 1.5:
                for c in range(8):
                    sc.op("act", lambda e, c=c: e.activation(self.xt[:, c, :], self.hT[:, c, :], AF.Identity), reads=[self.b_hT[c]], writes=[self.b_xt[c]])
            return
        if self.stage == 2.17:
            wt, wb = self.wload(self.d_win[l, 2])
            p = self.ps(T)
            sc.op("pe", lambda e: e.matmul(p.ap, wt[:, 0:128], self.hT[:, 0, :], start=True, stop=True), reads=[wb, self.b_hT[0]], writes=p.bufs)
            sc.op("act", lambda e: e.activation(self.xt[:, 0, :], p.ap, AF.Identity), reads=p.bufs, writes=[self.b_xt[0]])
            p2 = self.ps(T)
            for kc in range(8):
                sc.op("pe", lambda e, kc=kc: e.matmul(p2.ap, wt[:, kc * 128:(kc + 1) * 128], self.hT[:, kc, :], start=(kc == 0), stop=(kc == 7)),
                      reads=[wb, self.b_hT[kc]], writes=p2.bufs)
            sc.op("act", lambda e: e.activation(self.xt[:, 1, :], p2.ap, AF.Identity), reads=p2.bufs, writes=[self.b_xt[1]])
            p3 = self.ps(T)
            for kc in range(8):
                sc.op("pe", lambda e, kc=kc: e.matmul(p3.ap, wt[:, kc * 128:(kc + 1) * 128], self.hT[:, kc, :], start=(kc == 0), stop=(kc == 7)),
                      reads=[wb, self.b_hT[kc]], writes=p3.bufs)
            sc.op("dve", lambda e: e.tensor_copy(self.xt[:, 2, :], p3.ap), reads=p3.bufs, writes=[self.b_xt[2]])
            return
        if self.stage == 2.15:
            for i in range(3):
                wt, wb = self.wload(self.d_win[l, 2 + i])
                sc.op("act", lambda e, wt=wt, i=i: e.activation(self.xt[:, i, :], wt[:, 0:256], AF.Identity), reads=[wb], writes=[self.b_xt[i]])
                sc.op("act", lambda e, wt=wt, i=i: e.activation(self.xt[:, 3 + i, :], wt[:, 768:1024], AF.Identity), reads=[wb], writes=[self.b_xt[3 + i]])
            return
        sga = []
        for c in range(2):
            p = zchunk(4 + c)
            t, b = self.stt()
            sc.op("act", lambda e, t=t, p=p: e.activation(t[:], p.ap, AF.Silu), reads=p.bufs, writes=[b])
            sga.append((t, b))
        for c in range(2):
            p = zchunk(0 + c)
            sc.op("dve", lambda e, c=c, p=p: e.tensor_tensor(self.ug[:, c, :], p.ap, sga[c][0][:], ALU.mult),
                  reads=p.bufs + [sga[c][1]], writes=[self.b_ug[c]])
        for c in range(2):
            p = zchunk(2 + c)
            sc.op("act", lambda e, c=c, p=p: e.activation(self.va[:, c, :], p.ap, AF.Identity), reads=p.bufs, writes=[self.b_va[c]])
        if self.stage < 2.2:
            if self.stage == 2.19:
                srcs = [(self.va[:, 0, :], self.b_va[0]), (self.va[:, 1, :], self.b_va[1]), (self.ug[:, 0, :], self.b_ug[0]), (self.ug[:, 1, :], self.b_ug[1])]
                for c in range(4):
                    sc.op("act", lambda e, c=c: e.activation(self.xt[:, c, :], srcs[c][0], AF.Identity), reads=[srcs[c][1]], writes=[self.b_xt[c]])
            return
        meanA, bmA, rsA, brsA = self.ln_stats([self.va[:, c, :] for c in range(2)], self.b_va, C_ONES, AW, LN_EPS)
        if self.stage < 2.4:
            if self.stage == 2.3:
                srcs = [(self.va[:, 0, :], self.b_va[0]), (self.va[:, 1, :], self.b_va[1]), (self.ug[:, 0, :], self.b_ug[0]), (self.ug[:, 1, :], self.b_ug[1])]
                for c in range(4):
                    sc.op("act", lambda e, c=c: e.activation(self.xt[:, c, :], srcs[c][0], AF.Identity), reads=[srcs[c][1]], writes=[self.b_xt[c]], force=True)
            return
        for c in range(2):
            t, b = self.stt()
            sc.op("dve", lambda e, c=c, t=t: e.tensor_tensor(t[:], self.va[:, c, :], meanA[:], ALU.subtract), reads=[self.b_va[c], bmA], writes=[b])
            sc.op("dve", lambda e, t=t: e.tensor_tensor(t[:], t[:], rsA[:], ALU.mult), reads=[b, brsA], writes=[b])
            sc.op("act", lambda e, c=c, t=t: e.activation(self.vn[:, c, :], t[:], AF.Identity, bias=self.vcol(l, V_SLB + c), scale=self.vcol(l, V_SLG + c)),
                  reads=[b, self.b_vecs], writes=[self.b_vn[c]])
        if self.stage < 2.6:
            return
        for blk in range(self.NBLK):
            for c in range(2):
                p = self.ps(128)
                sc.op("pe", lambda e, p=p, c=c, blk=blk: e.transpose(p.ap, self.vn[:, c, blk * 128:(blk + 1) * 128], ident),
                      reads=[self.b_vn[c], self.b_consts], writes=p.bufs)
                for hh in range(2):
                    sc.op("act", lambda e, p=p, c=c, blk=blk, hh=hh: e.activation(self.vntok[hh][:, blk, c * 128 + hh * 64:c * 128 + hh * 64 + 64], p.ap[:, hh * 64:hh * 64 + 64], AF.Identity),
                          reads=p.bufs, writes=[self.b_vntok[blk]])
        if self.stage < 2.8:
            return
        for blk in range(self.NBLK):
            for c in range(2):
                p = self.ps(128)
                for hh in range(2):
                    h = 2 * c + hh
                    sc.op("pe", lambda e, p=p, c=c, hh=hh, h=h, blk=blk: e.matmul(
                        p.ap, self.vntok[hh][:, blk, c * 128:(c + 1) * 128],
                        self.lp[:, h * 128:(h + 1) * 128], start=(hh == 0), stop=(hh == 1)),
                        reads=[self.b_vntok[blk], self.b_lp], writes=p.bufs)
                t, b = self.stt()
                sc.op("dve", lambda e, p=p, c=c, t=t: e.tensor_tensor(t[:, 0:128], p.ap, self.lp[:, 512 + c * 128:512 + (c + 1) * 128], ALU.add),
                      reads=p.bufs + [self.b_lp], writes=[b])
                sc.op("dve", lambda e, c=c, t=t, blk=blk: e.tensor_tensor(self.mixT[:, c, blk * 128:(blk + 1) * 128], t[:, 0:128],
                                                                           self.ug[:, c, blk * 128:(blk + 1) * 128], ALU.mult),
                      reads=[b, self.b_ug[c]], writes=[self.b_mix[c]])

        if self.stage < 3:
            return
        sgl = []
        for c in range(2):
            p = zchunk(8 + c)
            t, b = self.stt()
            sc.op("act", lambda e, t=t, p=p: e.activation(t[:], p.ap, AF.Sigmoid), reads=p.bufs, writes=[b])
            sgl.append((t, b))
        for c in range(2):
            hoff = (l * 2 + c) * HALO
            sc.op("pool", lambda e, c=c, hoff=hoff: e.tensor_copy(self.ybuf[:, c, 0:HALO], self.halo[:, hoff:hoff + HALO]),
                  reads=[self.b_halo], writes=[self.b_ybuf[c]])
            p = zchunk(6 + c)
            sc.op("dve", lambda e, c=c, p=p: e.tensor_tensor(self.ybuf[:, c, HALO:HALO + T], p.ap, sgl[c][0][:], ALU.mult),
                  reads=p.bufs + [sgl[c][1]], writes=[self.b_ybuf[c]])
            sc.op("pool", lambda e, c=c, hoff=hoff: e.tensor_copy(self.halo[:, hoff:hoff + HALO], self.ybuf[:, c, T:T + HALO]),
                  reads=[self.b_ybuf[c]], writes=[self.b_halo])
        for c in range(2):
            p = zchunk(10 + c)
            sc.op("act", lambda e, c=c, p=p: e.activation(self.sgb[:, c, :], p.ap, AF.Silu), reads=p.bufs, writes=[self.b_sgb[c]])
        for c in range(2):
            p = self.ps(T)
            for tap in range(CONVW):
                di = self.dgptr
                self.dgptr = (di + 1) % self.NDG
                dg, dgb = self.diag[di], self.b_diag[di]
                sc.op("pool", lambda e, dg=dg, c=c, tap=tap: e.tensor_scalar(dg[:], ident, self.vcol(l, V_CW + c * CONVW + tap), None, ALU.mult),
                      reads=[self.b_consts, self.b_vecs], writes=[dgb])
                sc.op("pe", lambda e, dg=dg, c=c, tap=tap, p=p: e.matmul(p.ap, dg[:], self.ybuf[:, c, tap:tap + T], start=(tap == 0), stop=(tap == CONVW - 1)),
                      reads=[dgb, self.b_ybuf[c]], writes=p.bufs)
            sc.op("act", lambda e, c=c, p=p: e.activation(self.yc[:, c, :], p.ap, AF.Identity, bias=self.vcol(l, V_CB + c)),
                  reads=p.bufs + [self.b_vecs], writes=[self.b_yc[c]])
        meanB, bmB, rsB, brsB = self.ln_stats([self.yc[:, c, :] for c in range(2)], self.b_yc, C_ONES, BW, LN_EPS)
        for c in range(2):
            t, b = self.stt()
            sc.op("dve", lambda e, c=c, t=t: e.tensor_tensor(t[:], self.yc[:, c, :], meanB[:], ALU.subtract), reads=[self.b_yc[c], bmB], writes=[b])
            sc.op("dve", lambda e, t=t: e.tensor_tensor(t[:], t[:], rsB[:], ALU.mult), reads=[b, brsB], writes=[b])
            sc.op("act", lambda e, c=c, t=t: e.activation(self.yn[:, c, :], t[:], AF.Silu, bias=self.vcol(l, V_CLB + c), scale=self.vcol(l, V_CLG + c)),
                  reads=[b, self.b_vecs], writes=[self.b_yn[c]])
        for co in range(2):
            p = self.ps(T)
            for ci in range(2):
                sc.op("pe", lambda e, p=p, ci=ci, co=co: e.matmul(p.ap, self.lp[:, 768 + ci * 256 + co * 128:768 + ci * 256 + (co + 1) * 128], self.yn[:, ci, :],
                                                                 start=(ci == 0), stop=(ci == 1)),
                      reads=[self.b_lp, self.b_yn[ci]], writes=p.bufs)
            sc.op("dve", lambda e, p=p, co=co: e.scalar_tensor_tensor(self.mixT[:, 2 + co, :], p.ap, self.vcol(l, V_PWB + co), self.sgb[:, co, :], ALU.add, ALU.mult),
                  reads=p.bufs + [self.b_sgb[co], self.b_vecs], writes=[self.b_mix[2 + co]])

        if self.stage < 4:
            return
        for c in range(4):
            p = zchunk(25 + c)
            sc.op("act", lambda e, c=c, p=p: e.activation(self.sgc[:, c, :], p.ap, AF.Silu), reads=p.bufs, writes=[self.b_sgc[c]])
        for c in range(13):
            p = zchunk(12 + c)
            zl = self.zlast[:, l * 13 + c:l * 13 + c + 1]
            sc.op("act", lambda e, c=c, p=p: e.activation(self.zs[:, c, :], p.ap, AF.Identity, scale=self.dcol(l, DV_OMM + c)),
                  reads=p.bufs + [self.b_dv], writes=[self.b_zs[c]])
            sc.op("dve", lambda e, c=c, p=p: e.scalar_tensor_tensor(self.zs[:, c, 1:T], p.ap[:, 0:T - 1], self.vcol(l, V_MU + c), self.zs[:, c, 1:T], ALU.mult, ALU.add),
                  reads=p.bufs + [self.b_zs[c], self.b_vecs], writes=[self.b_zs[c]])
            sc.op("dve", lambda e, c=c, zl=zl: e.scalar_tensor_tensor(self.zs[:, c, 0:1], zl, self.vcol(l, V_MU + c), self.zs[:, c, 0:1], ALU.mult, ALU.add),
                  reads=[self.b_zlast, self.b_zs[c], self.b_vecs], writes=[self.b_zs[c]])
            sc.op("act", lambda e, p=p, zl=zl: e.activation(zl, p.ap[:, T - 1:T], AF.Identity), reads=p.bufs, writes=[self.b_zlast])
        sc.op("act", lambda e: e.activation(self.zs[0:64, 12, :], self.zs[0:64, 12, :], AF.Tanh), reads=[self.b_zs[12]], writes=[self.b_zs[12]])
        if self.stage < 5 and self.stage != 4.5:
            return
        for pr in range(4):
            if self.stage == 4.5:
                break
            self.rwkv_pair(l, pr)
        if self.stage < 7:
            if self.stage == 4.5:
                srcs = [(self.va[:, 0, :], self.b_va[0]), (self.va[:, 1, :], self.b_va[1]), (self.ug[:, 0, :], self.b_ug[0]), (self.ug[:, 1, :], self.b_ug[1]),
                        (self.sgb[:, 0, :], self.b_sgb[0]), (self.yc[:, 0, :], self.b_yc[0]), (self.zs[:, 0, :], self.b_zs[0]), (self.sgc[:, 0, :], self.b_sgc[0])]
                for c in range(8):
                    sc.op("act", lambda e, c=c: e.activation(self.xt[:, c, :], srcs[c][0], AF.Identity), reads=[srcs[c][1]], writes=[self.b_xt[c]], force=True)
            if self.stage == 6.5:
                for c in range(8):
                    sc.op("act", lambda e, c=c: e.activation(self.xt[:, c, :], self.mixT[:, c, :], AF.Identity), reads=[self.b_mix[c]], writes=[self.b_xt[c]])
            return

        for oc in range(8):
            wt, wb = self.wload(self.d_wout[l, oc])
            p = self.ps(T)
            for kc in range(8):
                sc.op("pe", lambda e, kc=kc, wt=wt, p=p: e.matmul(p.ap, wt[:, kc * 128:(kc + 1) * 128], self.mixT[:, kc, :], start=(kc == 0), stop=(kc == 7)),
                      reads=[wb, self.b_mix[kc]], writes=p.bufs)
            sc.op("act", lambda e, oc=oc, p=p: e.activation(self.mo[:, oc, :], p.ap, AF.Identity), reads=p.bufs, writes=[self.b_mo[oc]])
        sqs1 = []
        for c in range(8):
            t, b = self.stt()
            sc.op("act", lambda e, t=t, c=c: e.activation(t[:], self.mo[:, c, :], AF.Square), reads=[self.b_mo[c]], writes=[b])
            sqs1.append((t, b))
        pss1 = self.ps(T)
        for c in range(8):
            sc.op("pe", lambda e, c=c: e.matmul(pss1.ap, ones, sqs1[c][0][:], start=(c == 0), stop=(c == 7)),
                  reads=[sqs1[c][1], self.b_consts], writes=pss1.bufs)
        rs1, brs1 = self.rstd_from(pss1.ap, pss1.bufs, 1.0 / D, RMS_EPS)
        for c in range(8):
            sc.op("dve", lambda e, c=c: e.scalar_tensor_tensor(self.mo[:, c, :], self.mo[:, c, :], self.vcol(l, V_POSTG + c), rs1[:], ALU.mult, ALU.mult),
                  reads=[self.b_mo[c], brs1, self.b_vecs], writes=[self.b_mo[c]])
            sc.op("dve", lambda e, c=c: e.tensor_tensor(self.xt[:, c, :], self.xt[:, c, :], self.mo[:, c, :], ALU.add),
                  reads=[self.b_xt[c], self.b_mo[c]], writes=[self.b_xt[c]])
        if wdt != F32:
            for c in range(8):
                sc.op("act", lambda e, c=c: e.activation(self.hT[:, c, :], self.xt[:, c, :], AF.Identity), reads=[self.b_xt[c]], writes=[self.b_hT[c]])
            xsrc, xb = self.hT, self.b_hT
        else:
            xsrc, xb = self.xt, self.b_xt
        for oc in range(8):
            wt, wb = self.wload(self.d_gw[l, oc])
            p = self.ps(T)
            for kc in range(8):
                sc.op("pe", lambda e, kc=kc, wt=wt, p=p: e.matmul(p.ap, wt[:, kc * 128:(kc + 1) * 128], xsrc[:, kc, :], start=(kc == 0), stop=(kc == 7)),
                      reads=[wb, xb[kc]], writes=p.bufs)
            sc.op("act", lambda e, oc=oc, p=p: e.activation(self.mo[:, oc, :], p.ap, AF.Sigmoid, bias=self.vcol(l, V_GATEB + oc)),
                  reads=p.bufs + [self.b_vecs], writes=[self.b_mo[oc]])
        for oc in range(8):
            wt, wb = self.wload(self.d_plew[l, oc], 256)
            p = self.ps(T)
            for kc in range(2):
                sc.op("pe", lambda e, kc=kc, wt=wt, p=p: e.matmul(p.ap, wt[:, kc * 128:(kc + 1) * 128], self.pt[:, kc, :], start=(kc == 0), stop=(kc == 1)),
                      reads=[wb, self.b_pt], writes=p.bufs)
            sc.op("dve", lambda e, oc=oc, p=p: e.tensor_tensor(self.mo[:, oc, :], p.ap, self.mo[:, oc, :], ALU.mult),
                  reads=p.bufs + [self.b_mo[oc]], writes=[self.b_mo[oc]])
            sc.op("dve", lambda e, oc=oc: e.tensor_tensor(self.xt[:, oc, :], self.xt[:, oc, :], self.mo[:, oc, :], ALU.add),
                  reads=[self.b_xt[oc], self.b_mo[oc]], writes=[self.b_xt[oc]])

    def rwkv_pair(self, l, pr):
        sc, T, NCH = self.sc, self.T, self.NCH
        ident = self.cst(C_ID, 128)
        blk = self.cst(C_BLK, 128)
        P = self.pt_
        Bf = self.b_pt_
        r_ap, r_b = self.zs[:, 0 + pr, :], self.b_zs[0 + pr]
        k_ap, k_b = self.zs[:, 4 + pr, :], self.b_zs[4 + pr]
        v_ap, v_b = self.zs[:, 8 + pr, :], self.b_zs[8 + pr]
        lora_w = self.lp[:, 1280 + pr * 128:1280 + (pr + 1) * 128]
        lora_a = self.lp[:, 1792 + pr * 128:1792 + (pr + 1) * 128]
        p = self.ps(T)
        sc.op("pe", lambda e: e.matmul(p.ap, lora_w, self.zs[:, 12, :], start=True, stop=True), reads=[self.b_lp, self.b_zs[12]], writes=p.bufs)
        sc.op("act", lambda e: e.activation(P["lw"][:], p.ap, AF.Sigmoid, bias=self.vcol(l, V_W0 + pr)), reads=p.bufs + [self.b_vecs], writes=[Bf["lw"]])
        p2 = self.ps(T)
        sc.op("pe", lambda e: e.matmul(p2.ap, lora_a, self.zs[:, 12, :], start=True, stop=True), reads=[self.b_lp, self.b_zs[12]], writes=p2.bufs)
        sc.op("act", lambda e: e.activation(P["aa"][:], p2.ap, AF.Sigmoid, bias=self.vcol(l, V_A0 + pr)), reads=p2.bufs + [self.b_vecs], writes=[Bf["aa"]])
        sc.op("dve", lambda e: e.tensor_scalar(P["lw"][:], P["lw"][:], -DECAY, None, ALU.mult), reads=[Bf["lw"]], writes=[Bf["lw"]])
        sc.op("dve", lambda e: e.tensor_tensor_scan(P["cum"][:], self.cst(C_RESET, T), P["lw"][:], 0.0, ALU.mult, ALU.add),
              reads=[Bf["lw"], self.b_consts], writes=[Bf["cum"]])
        sc.op("dve", lambda e: e.tensor_tensor(P["cume"][:], P["cum"][:], P["lw"][:], ALU.subtract), reads=[Bf["cum"], Bf["lw"]], writes=[Bf["cume"]])
        sc.op("act", lambda e: e.activation(P["E1"][:], P["cum"][:], AF.Exp), reads=[Bf["cum"]], writes=[Bf["E1"]])
        sc.op("act", lambda e: e.activation(P["E2"][:], P["cum"][:], AF.Exp, scale=-1.0), reads=[Bf["cum"]], writes=[Bf["E2"]])
        sc.op("act", lambda e: e.activation(P["E3"][:], P["cume"][:], AF.Exp), reads=[Bf["cume"]], writes=[Bf["E3"]])
        sc.op("dve", lambda e: e.tensor_scalar(P["kk0"][:], k_ap, self.vcol(l, V_KK + pr), None, ALU.mult), reads=[k_b, self.b_vecs], writes=[Bf["kk0"]])
        t, b = self.stt()
        sc.op("act", lambda e: e.activation(t[:], P["kk0"][:], AF.Square), reads=[Bf["kk0"]], writes=[b])
        p3 = self.ps(T)
        sc.op("pe", lambda e: e.matmul(p3.ap, blk, t[:], start=True, stop=True), reads=[b, self.b_consts], writes=p3.bufs)
        rn, brn = self.rstd_from(p3.ap, p3.bufs, 1.0, 0.0, clamp=1e-12)
        sc.op("dve", lambda e: e.tensor_tensor(P["kkn"][:], P["kk0"][:], rn[:], ALU.mult), reads=[Bf["kk0"], brn], writes=[Bf["kkn"]])
        t2, b2 = self.stt()
        sc.op("dve", lambda e: e.tensor_scalar(t2[:], P["aa"][:], self.vcol(l, V_KA + pr), self.dcol(l, DV_OMKA + pr), ALU.mult, ALU.add),
              reads=[Bf["aa"], self.b_vecs, self.b_dv], writes=[b2])
        sc.op("dve", lambda e: e.tensor_tensor(P["kmod"][:], k_ap, t2[:], ALU.mult), reads=[k_b, b2], writes=[Bf["kmod"]])
        sc.op("dve", lambda e: e.tensor_tensor(P["bvec"][:], P["kkn"][:], P["aa"][:], ALU.mult), reads=[Bf["kkn"], Bf["aa"]], writes=[Bf["bvec"]])
        def v3(ap):
            return ap.rearrange("p (j t) -> p j t", t=CH)

        def padv(n, hh):
            return self.pads[n][:, :].rearrange("p (j h t) -> p j h t", h=2, t=CH)[:, :, hh, :]

        for hh in range(2):
            rows = slice(hh * 64, hh * 64 + 64)
            def pv(n, hh=hh, rows=rows):
                return self.pads[n][rows, :].rearrange("p (j h t) -> p j h t", h=2, t=CH)[:, :, hh, :]
            sc.op("dve", lambda e, rows=rows, pv=pv: e.scalar_tensor_tensor(pv("A"), v3(P["kkn"][rows, :]), -1.0, v3(P["E3"][rows, :]), ALU.mult, ALU.mult),
                  reads=[Bf["kkn"], Bf["E3"]], writes=self.b_pads["A"])
            sc.op("dve", lambda e, rows=rows, pv=pv: e.tensor_tensor(pv("B"), v3(P["bvec"][rows, :]), v3(P["E2"][rows, :]), ALU.mult),
                  reads=[Bf["bvec"], Bf["E2"]], writes=self.b_pads["B"])
            sc.op("dve", lambda e, rows=rows, pv=pv: e.tensor_tensor(pv("K"), v3(P["kmod"][rows, :]), v3(P["E2"][rows, :]), ALU.mult),
                  reads=[Bf["kmod"], Bf["E2"]], writes=self.b_pads["K"])
            sc.op("act", lambda e, rows=rows, pv=pv: e.activation(pv("V"), v3(self.zs[rows, 8 + pr, :]), AF.Identity),
                  reads=[v_b], writes=self.b_pads["V"])
        sc.op("dve", lambda e: e.tensor_tensor(self.Rt[:], r_ap, P["E1"][:], ALU.mult), reads=[r_b, Bf["E1"]], writes=[self.b_Rt])
        if self.stage < 6:
            return
        for j in range(NCH):
            self.scan_unit(l, pr, j)
        yy, byy = P["yy"], Bf["yy"]
        meanC, bmC, rsC, brsC = self.ln_stats([yy[:]], [byy], C_BLK, CH, GN_EPS)
        tpo, bpo = self.stt()
        sc.op("dve", lambda e: e.tensor_tensor(tpo[:], yy[:], meanC[:], ALU.subtract), reads=[byy, bmC], writes=[bpo])
        sc.op("dve", lambda e: e.tensor_tensor(tpo[:], tpo[:], rsC[:], ALU.mult), reads=[bpo, brsC], writes=[bpo])
        sc.op("act", lambda e: e.activation(tpo[:], tpo[:], AF.Identity, bias=self.vcol(l, V_LXB + pr), scale=self.vcol(l, V_LXG + pr)),
              reads=[bpo, self.b_vecs], writes=[bpo])
        t3, b3 = self.stt()
        sc.op("dve", lambda e: e.scalar_tensor_tensor(t3[:], r_ap, self.vcol(l, V_RK + pr), P["kmod"][:], ALU.mult, ALU.mult),
              reads=[r_b, Bf["kmod"], self.b_vecs], writes=[b3])
        p4 = self.ps(T)
        sc.op("pe", lambda e: e.matmul(p4.ap, blk, t3[:], start=True, stop=True), reads=[b3, self.b_consts], writes=p4.bufs)
        sc.op("dve", lambda e: e.tensor_tensor(t3[:], p4.ap, v_ap, ALU.mult), reads=p4.bufs + [v_b], writes=[b3])
        sc.op("dve", lambda e: e.tensor_tensor(tpo[:], tpo[:], t3[:], ALU.add), reads=[bpo, b3], writes=[bpo])
        sc.op("dve", lambda e: e.tensor_tensor(self.mixT[:, 4 + pr, :], tpo[:], self.sgc[:, pr, :], ALU.mult),
              reads=[bpo, self.b_sgc[pr]], writes=[self.b_mix[4 + pr]])

    def scan_unit(self, l, pr, j):
        sc, T = self.sc, self.T
        ident = self.cst(C_ID, 128)
        msu = self.cst(C_MSU, 128)
        msl = self.cst(C_MSL, 128)
        mui = self.cst(C_MUI, 64)
        ui = getattr(self, "_uptr", 0)
        self._uptr = (ui + 1) % self.NU
        U = self.ut[ui]
        cs = slice(j * 128, (j + 1) * 128)
        Ap, Bp, Kp, Vp = (self.pads[n][:, cs] for n in ("A", "B", "K", "V"))
        bA, bB, bK, bV = (self.b_pads[n][j] for n in ("A", "B", "K", "V"))
        Rst = self.Rt[:, j * CH:(j + 1) * CH]
        CB = self.b_consts

        def mm(out_ps, lhsT, rhs, reads, start=True, stop=True):
            sc.op("pe", lambda e: e.matmul(out_ps.ap, lhsT, rhs, start=start, stop=stop), reads=reads, writes=out_ps.bufs)

        def ev_mask(dst, p, mask):
            sc.op("dve", lambda e: e.tensor_tensor(dst[0][:], p.ap, mask, ALU.mult), reads=p.bufs + [CB], writes=[dst[1]])

        def ev_copy(dst, p, dst_ap=None):
            d = dst[0][:] if dst_ap is None else dst_ap
            sc.op("act", lambda e: e.activation(d, p.ap, AF.Identity), reads=p.bufs, writes=[dst[1]])

        p = self.ps(128); mm(p, Bp, Ap, [bB, bA]); ev_mask(U["B0"], p, msu)
        p = self.ps(128); mm(p, Ap, Bp, [bA, bB]); ev_mask(U["A0"], p, msl)
        p = self.ps(128); mm(p, Kp, Ap, [bK, bA]); ev_mask(U["MakT"], p, msu)
        p = self.ps(64); mm(p, Bp, Rst, [bB, self.b_Rt]); ev_mask(U["MrbT"], p, mui)
        p = self.ps(64); mm(p, Kp, Rst, [bK, self.b_Rt]); ev_mask(U["MrkT"], p, mui)
        def tr(dst, src, bsrc, dst_ap=None):
            p = self.ps(128)
            sc.op("pe", lambda e: e.transpose(p.ap, src, ident), reads=[bsrc, CB], writes=p.bufs)
            ev_copy(dst, p, dst_ap)
        tr(U["Z0"], Ap, bA, U["Z0"][0][:, 0:128])
        tr(U["Btok"], Bp, bB)
        tr(U["Ktok"], Kp, bK)
        tr(U["Vbd"], Vp, bV)
        p = self.ps(128); mm(p, U["MakT"][0][:], U["Vbd"][0][:], [U["MakT"][1], U["Vbd"][1]])
        ev_copy(U["Z0"], p, U["Z0"][0][:, 128:256])
        Acur, Bcur, Anext, Bnext = U["A0"], U["B0"], U["A1"], U["B1"]
        Zc, Zn = U["Z0"], U["Z1"]
        for n in range(6):
            p = self.ps(256)
            mm(p, Bcur[0][:], Zc[0][:], [Bcur[1], Zc[1]])
            sc.op("dve", lambda e, p=p, Zc=Zc, Zn=Zn: e.tensor_tensor(Zn[0][:], p.ap, Zc[0][:], ALU.add), reads=p.bufs + [Zc[1]], writes=[Zn[1]])
            Zc, Zn = Zn, Zc
            if n < 5:
                pb = self.ps(128); mm(pb, Acur[0][:], Bcur[0][:], [Acur[1], Bcur[1]])
                if n < 4:
                    pa = self.ps(128); mm(pa, Bcur[0][:], Acur[0][:], [Acur[1], Bcur[1]])
                ev_copy(Bnext, pb)
                if n < 4:
                    ev_copy(Anext, pa)
                Acur, Anext = Anext, Acur
                Bcur, Bnext = Bnext, Bcur
        Pm, Qm = Zc[0][:, 0:128], Zc[0][:, 128:256]
        bZ = Zc[1]
        p = self.ps(64)
        mm(p, ident, Rst, [CB, self.b_Rt], start=True, stop=False)
        mm(p, Pm, U["MrbT"][0][:], [bZ, U["MrbT"][1]], start=False, stop=True)
        ev_copy(U["Rp"], p)
        p = self.ps(128)
        mm(p, ident, ident, [CB], start=True, stop=False)
        mm(p, Pm, U["Btok"][0][:], [bZ, U["Btok"][1]], start=False, stop=True)
        ev_copy(U["G0"], p)
        st_ap = self.state[:, (l * 4 + pr) * 128:(l * 4 + pr + 1) * 128]
        bS = self.b_state[l][pr]
        py = self.ps(64)
        mm(py, Qm, U["MrbT"][0][:], [bZ, U["MrbT"][1]], start=True, stop=False)
        mm(py, U["Vbd"][0][:], U["MrkT"][0][:], [U["Vbd"][1], U["MrkT"][1]], start=False, stop=False)
        mm(py, st_ap, U["Rp"][0][:], [bS, U["Rp"][1]], start=False, stop=True)
        pS = self.ps(128)
        mm(pS, U["Btok"][0][:], Qm, [U["Btok"][1], bZ], start=True, stop=False)
        mm(pS, U["Ktok"][0][:], U["Vbd"][0][:], [U["Ktok"][1], U["Vbd"][1]], start=False, stop=False)
        mm(pS, U["G0"][0][:], st_ap, [U["G0"][1], bS], start=False, stop=True)
        yy, byy = self.pt_["yy"], self.b_pt_["yy"]
        sc.op("act", lambda e: e.activation(yy[:, j * CH:(j + 1) * CH], py.ap, AF.Identity), reads=py.bufs, writes=[byy])
        wc = self.pt_["E1"][:, j * CH + CH - 1:j * CH + CH]
        sc.op("dve", lambda e: e.tensor_scalar(st_ap, pS.ap, wc, None, ALU.mult), reads=pS.bufs + [self.b_pt_["E1"]], writes=[bS])


def make_consts(T):
    NC = C_RESET + T
    c = np.zeros((128, NC), np.float32)
    c[:, C_ID:C_ID + 128] = np.eye(128, dtype=np.float32)
    c[:, C_ONES:C_ONES + 128] = 1.0
    blk = np.zeros((128, 128), np.float32)
    blk[:64, :64] = 1.0
    blk[64:, 64:] = 1.0
    c[:, C_BLK:C_BLK + 128] = blk
    tri_u = np.triu(np.ones((64, 64), np.float32), 1)
    msu = np.zeros((128, 128), np.float32)
    msu[:64, :64] = tri_u
    msu[64:, 64:] = tri_u
    c[:, C_MSU:C_MSU + 128] = msu
    c[:, C_MSL:C_MSL + 128] = msu.T
    ui = np.triu(np.ones((64, 64), np.float32), 0)
    c[:64, C_MUI:C_MUI + 64] = ui
    c[64:, C_MUI:C_MUI + 64] = ui
    c[:64, C_PAD] = 1.0
    c[64:, C_PAD + 1] = 1.0
    c[:64, C_NPAD] = -1.0
    c[64:, C_NPAD + 1] = -1.0
    r = np.ones(T, np.float32)
    r[::CH] = 0.0
    c[:, C_RESET:C_RESET + T] = r[None, :]
    return c


def colmaj(v, n):
    return np.ascontiguousarray(v.reshape(n, 128).T)


def prep_shared(inp, NL, T):
    f = lambda a: np.ascontiguousarray(a, dtype=np.float32)
    sh = {}
    w_in = f(inp["w_in"][:NL])
    sh["win"] = np.ascontiguousarray(w_in.reshape(NL, 8, 128, NOC, 128).transpose(0, 3, 2, 1, 4)).reshape(NL, NOC, 128, 1024)
    sh["wout"] = np.ascontiguousarray(f(inp["w_out"][:NL]).reshape(NL, 8, 128, 8, 128).transpose(0, 3, 2, 1, 4)).reshape(NL, 8, 128, 1024)
    sh["gw"] = np.ascontiguousarray(f(inp["ple_gate_w"][:NL]).reshape(NL, 8, 128, 8, 128).transpose(0, 3, 2, 1, 4)).reshape(NL, 8, 128, 1024)
    sh["plew"] = np.ascontiguousarray(f(inp["ple_w"][:NL]).reshape(NL, 2, 128, 8, 128).transpose(0, 3, 2, 1, 4)).reshape(NL, 8, 128, 256)
    vecs = np.zeros((128, NL * NV), np.float32)
    for l in range(NL):
        o = l * NV
        def put(col, v, n):
            vecs[:, o + col:o + col + n] = colmaj(f(v), n)
        put(V_PREG, inp["pre_norm_g"][l], 8)
        put(V_POSTG, inp["post_norm_g"][l], 8)
        put(V_GATEB, inp["ple_gate_b"][l], 8)
        put(V_SLG, inp["sgu_ln_g"][l], 2)
        put(V_SLB, inp["sgu_ln_b"][l], 2)
        put(V_CB, inp["conv_b"][l], 2)
        put(V_CLG, inp["conv_ln_g"][l], 2)
        put(V_CLB, inp["conv_ln_b"][l], 2)
        put(V_PWB, inp["pw_b"][l], 2)
        put(V_MU, inp["shift_mu"][l], 13)
        put(V_W0, inp["w0"][l], 4)
        put(V_A0, inp["a0"][l], 4)
        put(V_KK, inp["k_k"][l], 4)
        put(V_KA, inp["k_a"][l], 4)
        put(V_RK, inp["r_k"][l].reshape(-1), 4)
        put(V_LXG, inp["lnx_g"][l], 4)
        put(V_LXB, inp["lnx_b"][l], 4)
        cw = f(inp["conv_w"][l])
        for c in range(2):
            vecs[:, o + V_CW + c * CONVW:o + V_CW + (c + 1) * CONVW] = cw[:, c * 128:(c + 1) * 128].T
    sh["vecs"] = vecs
    sh["sguT"] = np.ascontiguousarray(f(inp["sgu_w"][:NL]).transpose(0, 3, 1, 2)).reshape(NL, 128, 512)
    sb = f(inp["sgu_b"][:NL])
    sgub = np.zeros((NL, 128, 256), np.float32)
    for c in range(2):
        for hh in range(2):
            sgub[:, hh * 64:(hh + 1) * 64, c * 128:(c + 1) * 128] = sb[:, 2 * c + hh][:, None, :]
    sh["sgub"] = sgub
    sh["pww"] = np.ascontiguousarray(f(inp["pw_w"][:NL]).reshape(NL, 2, 128, 256).transpose(0, 2, 1, 3)).reshape(NL, 128, 512)
    lora = np.zeros((NL, 128, 1024), np.float32)
    lora[:, 0:64, 0:512] = f(inp["w_up"][:NL])
    lora[:, 64:128, 512:1024] = f(inp["a_up"][:NL])
    sh["lora"] = lora
    sh["consts"] = make_consts(T)
    return sh


_CACHE = {}


def run(inp, NL, NT, T, ncore, **kw):
    key = (NL, NT, T, tuple(sorted(kw.items())))
    S = NT * T
    prog = Prog(NL, NT, T, **kw)
    nc = prog.build()
    sh = prep_shared(inp, NL, T)
    in_maps = []
    for b in range(ncore):
        m = dict(sh)
        m["xT"] = np.ascontiguousarray(np.asarray(inp["x"][b, :S], np.float32).T)
        m["pT"] = np.ascontiguousarray(np.asarray(inp["p"][:NL, b, :S], np.float32).transpose(0, 2, 1))
        in_maps.append(m)
    res = run_bass_kernel_spmd(nc, in_maps, core_ids=list(range(ncore)))
    out = np.stack([np.ascontiguousarray(r["yT"].T) for r in res.results], axis=0)
    return out.astype(np.float32), prog


def kernel(**inputs):
    out, _ = run(inputs, NLAYER, SEQ // 256, 256, NCORE)
    return out
```

```python
import math
from contextlib import ExitStack

import numpy as np
import concourse.bass as bass
import concourse.mybir as mybir
from concourse.bass_utils import run_bass_kernel_spmd

F32 = mybir.dt.float32
BF16 = mybir.dt.bfloat16
AF = mybir.ActivationFunctionType
ALU = mybir.AluOpType

D = 1024
SEQ = 4096
NLAYER = 4
NCORE = 8
AW = 256
BW = 256
CW = 512
PLE = 256
CONVW = 31
HALO = CONVW - 1
INC = 3712
NOC = INC // 128
CH = 64
RMS_EPS = 1e-6
LN_EPS = 1e-5
GN_EPS = 64e-5
DECAY = math.exp(-0.5)

V_PREG, V_POSTG, V_GATEB = 0, 8, 16
V_SLG, V_SLB = 24, 26
V_CB, V_CLG, V_CLB, V_PWB = 28, 30, 32, 34
V_MU = 36
V_W0, V_A0, V_KK, V_KA, V_RK, V_LXG, V_LXB = 49, 53, 57, 61, 65, 69, 73
V_CW = 77
NV = V_CW + 2 * CONVW
DV_OMM, DV_OMKA = 0, 13
NDV = 17
C_ID, C_ONES, C_BLK, C_MSU, C_MSL, C_MUI, C_PAD, C_NPAD, C_RESET = 0, 128, 256, 384, 512, 640, 704, 706, 708


class Sem:
    def __init__(self, h):
        self.h = h
        self.cnt = 0


class Buf:
    __slots__ = ("name", "w", "r", "excl")

    def __init__(self, name, excl=False):
        self.name = name
        self.w = None
        self.r = {}
        self.excl = excl


class Eng:
    def __init__(self, name, sem):
        self.name = name
        self.sem = sem
        self.seen = {}
        self.items = []


class Sched:
    def __init__(self, nc, es):
        self.nc = nc
        self.es = es
        self.eng = {}
        for n in ("pe", "act", "dve", "pool", "sp"):
            self.eng[n] = Eng(n, self.new_sem("s_" + n))
        self.nops = 0

    def new_sem(self, name):
        return Sem(self.es.enter_context(self.nc.semaphore(name)))

    def _need(self, E, ev):
        sem, val = ev
        if E.seen.get(sem, 0) >= val:
            return
        E.items.append(("w", sem, val))
        E.seen[sem] = val

    def op(self, en, fn, reads=(), writes=(), dsem=None, force=False):
        import os
        lim = int(os.environ.get("KLIMIT", "0"))
        if lim and self.nops >= lim and not force:
            return None
        E = self.eng[en]
        if any(b.excl for b in reads):
            writes = list(writes) + [b for b in reads if b.excl]
            reads = [b for b in reads if not b.excl]
        for b in reads:
            if b.w is not None:
                if b.w[0] is E.sem and en == "pe":
                    continue
                self._need(E, b.w)
        for b in writes:
            if b.w is not None and b.w[0] is not E.sem:
                self._need(E, b.w)
            for sem, val in b.r.items():
                if sem is not E.sem:
                    self._need(E, (sem, val))
        if dsem is None:
            sem = E.sem
            sem.cnt += 1
            inc = 1
        else:
            sem = dsem
            sem.cnt += 16
            inc = 16
        ev = (sem, sem.cnt)
        E.items.append(("o", fn, sem, inc))
        for b in reads:
            b.r[sem] = sem.cnt
        for b in writes:
            b.w = ev
            b.r = {}
        self.nops += 1
        return ev

    def wait(self, en, ev):
        self._need(self.eng[en], ev)

    def replay(self, en, e):
        for it in self.eng[en].items:
            if it[0] == "w":
                e.wait_ge(it[1].h, it[2])
            else:
                it[1](e).then_inc(it[2].h, it[3])


class PsTile:
    def __init__(self, ap, bufs):
        self.ap = ap
        self.bufs = bufs


class Prog:
    def __init__(self, NL, NT, T, wdt=F32, sdt=F32, nslot=2, ugroup=4, stage=99):
        self.stage = stage
        self.NL, self.NT, self.T = NL, NT, T
        self.S = NT * T
        self.NCH = T // CH
        self.NBLK = T // 128
        self.wdt, self.sdt = wdt, sdt
        self.nslot = nslot
        self.ugroup = ugroup
        self.NC = C_RESET + T
        self.nc = bass.Bass("TRN2", target_bir_lowering=False)
        self.es = ExitStack()

    def dram(self, name, shape, kind="ExternalInput", dt=F32):
        return self.nc.dram_tensor(name, list(shape), dt, kind=kind).ap()

    def sb(self, name, shape, dt=F32):
        t = self.es.enter_context(self.nc.sbuf_tensor("sb_" + name, list(shape), dt))
        return t

    def build(self):
        nc, NL, T, S = self.nc, self.NL, self.T, self.S
        es = self.es
        with es:
            self.sc = Sched(nc, es)
            self.d_x = self.dram("xT", [D, S])
            self.d_p = self.dram("pT", [NL, PLE, S])
            self.d_win = self.dram("win", [NL, NOC, 128, 1024])
            self.d_wout = self.dram("wout", [NL, 8, 128, 1024])
            self.d_gw = self.dram("gw", [NL, 8, 128, 1024])
            self.d_plew = self.dram("plew", [NL, 8, 128, 256])
            self.d_vecs = self.dram("vecs", [128, NL * NV])
            self.d_sguT = self.dram("sguT", [NL, 128, 512])
            self.d_sgub = self.dram("sgub", [NL, 128, 256])
            self.d_pww = self.dram("pww", [NL, 128, 512])
            self.d_lora = self.dram("lora", [NL, 128, 1024])
            self.d_consts = self.dram("consts", [128, self.NC])
            self.d_y = self.dram("yT", [D, S], kind="ExternalOutput")
            self.alloc()
            self.emit()
            with nc.Block() as block:
                @block.tensor
                def _(e):
                    self.sc.replay("pe", e)

                @block.scalar
                def _(e):
                    self.sc.replay("act", e)

                @block.vector
                def _(e):
                    self.sc.replay("dve", e)

                @block.gpsimd
                def _(e):
                    self.sc.replay("pool", e)

                @block.sync
                def _(e):
                    self.sc.replay("sp", e)
        return nc

    def alloc(self):
        NL, T, NCH = self.NL, self.T, self.NCH
        sb = self.sb
        B = Buf
        self.xt = sb("xt", [128, 8, T]); self.b_xt = [B(f"xt{c}") for c in range(8)]
        self.hT = sb("hT", [128, 8, T], self.wdt); self.b_hT = [B(f"hT{c}") for c in range(8)]
        self.zs = sb("zs", [128, 13, T]); self.b_zs = [B(f"zs{c}") for c in range(13)]
        self.va = sb("va", [128, 2, T]); self.b_va = [B(f"va{c}") for c in range(2)]
        self.mixT = sb("mixT", [128, 8, T], self.wdt); self.b_mix = [B(f"mix{c}") for c in range(8)]
        self.mo = sb("mo", [128, 8, T]); self.b_mo = [B(f"mo{c}") for c in range(8)]
        self.wring = [sb(f"wr{i}", [128, 1024], self.wdt) for i in range(self.nslot)]
        self.b_wring = [B(f"wr{i}") for i in range(self.nslot)]
        self.s_wring = [self.sc.new_sem(f"swr{i}") for i in range(self.nslot)]
        self.wptr = 0
        self.NST = 10
        self.st = [sb(f"st{i}", [128, T]) for i in range(self.NST)]
        self.b_st = [B(f"st{i}") for i in range(self.NST)]
        self.stptr = 0
        self.pt = sb("pt", [128, 2, T], self.wdt); self.b_pt = B("pt"); self.s_pt = self.sc.new_sem("spt")
        self.vecs = sb("vecs", [128, NL * NV]); self.b_vecs = B("vecs")
        self.dv = sb("dv", [128, NL * NDV]); self.b_dv = B("dv")
        self.consts = sb("consts", [128, self.NC]); self.b_consts = B("consts")
        self.s_misc = self.sc.new_sem("smisc")
        self.s_x = self.sc.new_sem("sx")
        self.s_y = self.sc.new_sem("sy")
        self.lp = sb("lp", [128, 512 + 256 + 512 + 1024]); self.b_lp = B("lp"); self.s_lp = self.sc.new_sem("slp")
        self.ybuf = sb("ybuf", [128, 2, HALO + T]); self.b_ybuf = [B(f"ybuf{c}") for c in range(2)]
        self.halo = sb("halo", [128, NL * 2 * HALO]); self.b_halo = B("halo")
        self.zlast = sb("zlast", [128, NL * 13]); self.b_zlast = B("zlast")
        self.NDG = 8
        self.diag = [sb(f"dg{i}", [128, 128]) for i in range(self.NDG)]
        self.b_diag = [B(f"dg{i}") for i in range(self.NDG)]
        self.dgptr = 0
        self.state = sb("state", [128, NL * 4 * 128], self.sdt)
        self.b_state = [[B(f"state{l}_{p}") for p in range(4)] for l in range(NL)]
        self.vn = sb("vn", [128, 2, T]); self.b_vn = [B(f"vn{c}") for c in range(2)]
        self.vntok = [sb(f"vntok{hh}", [128, self.NBLK, 256]) for hh in range(2)]; self.b_vntok = [B(f"vntok{b}") for b in range(self.NBLK)]
        self.ug = sb("ug", [128, 2, T]); self.b_ug = [B(f"ug{c}") for c in range(2)]
        self.sgb = sb("sgb", [128, 2, T]); self.b_sgb = [B(f"sgb{c}") for c in range(2)]
        self.yc = sb("yc", [128, 2, T]); self.b_yc = [B(f"yc{c}") for c in range(2)]
        self.yn = sb("yn", [128, 2, T]); self.b_yn = [B(f"yn{c}") for c in range(2)]
        self.sgc = sb("sgc", [128, 4, T]); self.b_sgc = [B(f"sgc{c}") for c in range(4)]
        names = ["lw", "aa", "cum", "cume", "E2", "E3", "kk0", "kkn", "bvec"]
        self.pt_sh = {n: sb("c_" + n, [128, T]) for n in names}
        self.b_pt_sh = {n: B("c_" + n) for n in names}
        self.pt_pp = [{n: sb(f"c{pr}_" + n, [128, T]) for n in ("E1", "kmod", "yy")} for pr in range(4)]
        self.b_pt_pp = [{n: B(f"c{pr}_" + n) for n in ("E1", "kmod", "yy")} for pr in range(4)]
        self.Rt4 = [sb(f"Rt{pr}", [128, T], self.sdt) for pr in range(4)]; self.b_Rt4 = [B(f"Rt{pr}") for pr in range(4)]
        self.pads4 = [{n: sb(f"pad{pr}_" + n, [128, NCH * 128], self.sdt) for n in ("A", "B", "K", "V")} for pr in range(4)]
        self.b_pads4 = [{n: [B(f"pad{pr}_{n}{j}") for j in range(NCH)] for n in ("A", "B", "K", "V")} for pr in range(4)]
        self.NU = 4
        self.ut = []
        for u in range(self.NU):
            d = {}
            for n, w in (("A0", 128), ("A1", 128), ("B0", 128), ("B1", 128), ("MakT", 128), ("MrbT", 64), ("MrkT", 64),
                         ("Z0", 256), ("Z1", 256), ("Btok", 128), ("Ktok", 128), ("Vbd", 128), ("Rp", 64), ("G0", 128)):
                d[n] = (sb(f"u{u}_{n}", [128, w], self.sdt), B(f"u{u}_{n}"))
            self.ut.append(d)
        self.psb = [self.es.enter_context(self.nc.psum_tensor(f"ps{b}", [128, 512], F32)) for b in range(8)]
        self.b_psq = [B(f"psbank{b}", excl=True) for b in range(8)]
        self.psptr = 0

    def ps(self, ncols):
        b = self.psptr
        self.psptr = (b + 1) % 8
        return PsTile(self.psb[b][:, 0:ncols], [self.b_psq[b]])

    def stt(self):
        i = self.stptr
        self.stptr = (i + 1) % self.NST
        return self.st[i], self.b_st[i]

    def cst(self, off, n, rows=slice(0, 128)):
        return self.consts[rows, off:off + n]

    def vcol(self, l, col, rows=slice(0, 128)):
        return self.vecs[rows, l * NV + col:l * NV + col + 1]

    def dcol(self, l, col):
        return self.dv[:, l * NDV + col:l * NDV + col + 1]

    def emit(self):
        sc, NL, NT, T = self.sc, self.NL, self.NT, self.T
        sc.op("sp", lambda e: e.dma_start(out=self.consts[:], in_=self.d_consts), writes=[self.b_consts], dsem=self.s_misc)
        sc.op("sp", lambda e: e.dma_start(out=self.vecs[:], in_=self.d_vecs), writes=[self.b_vecs], dsem=self.s_misc)
        ev = (self.s_misc, self.s_misc.cnt)
        self.b_consts.w = ev
        self.b_vecs.w = ev
        for l in range(NL):
            sc.op("dve", lambda e, l=l: e.tensor_scalar(self.dv[:, l * NDV + DV_OMM:l * NDV + DV_OMM + 13],
                                                       self.vecs[:, l * NV + V_MU:l * NV + V_MU + 13], -1.0, 1.0, ALU.mult, ALU.add),
                  reads=[self.b_vecs], writes=[self.b_dv])
            sc.op("dve", lambda e, l=l: e.tensor_scalar(self.dv[:, l * NDV + DV_OMKA:l * NDV + DV_OMKA + 4],
                                                       self.vecs[:, l * NV + V_KA:l * NV + V_KA + 4], -1.0, 1.0, ALU.mult, ALU.add),
                  reads=[self.b_vecs], writes=[self.b_dv])
        sc.op("pool", lambda e: e.memset(self.state[:], 0.0), writes=[b for bl in self.b_state for b in bl])
        sc.op("pool", lambda e: e.memset(self.halo[:], 0.0), writes=[self.b_halo])
        sc.op("pool", lambda e: e.memset(self.zlast[:], 0.0), writes=[self.b_zlast])
        for pr in range(4):
            for n in ("A", "B", "K", "V"):
                sc.op("pool", lambda e, n=n, pr=pr: e.memset(self.pads4[pr][n][:], 0.0), writes=self.b_pads4[pr][n])
        for hh in range(2):
            sc.op("pool", lambda e, hh=hh: e.memset(self.vntok[hh][:], 0.0), writes=self.b_vntok)
        for ti in range(NT):
            t0 = ti * T
            sc.op("sp", lambda e, t0=t0: e.dma_start(out=self.xt[:], in_=self.d_x[:, t0:t0 + T].rearrange("(c p) t -> p c t", p=128)),
                  writes=self.b_xt, dsem=self.s_x)
            for l in range(NL):
                self.tile_layer(ti, l)
            ev = sc.op("sp", lambda e, t0=t0: e.dma_start(out=self.d_y[:, t0:t0 + T].rearrange("(c p) t -> p c t", p=128), in_=self.xt[:]),
                       reads=self.b_xt, dsem=self.s_y, force=True)
        sc.wait("sp", (self.s_y, self.s_y.cnt))

    def wload(self, src_ap, ncols=1024):
        i = self.wptr
        self.wptr = (i + 1) % self.nslot
        tile, buf, sem = self.wring[i], self.b_wring[i], self.s_wring[i]
        q = "sp" if self.wdt == F32 else "pool"
        self.sc.op(q, lambda e: e.dma_start(out=tile[:, 0:ncols], in_=src_ap), writes=[buf], dsem=sem)
        return tile, buf

    def rstd_from(self, src_ap, src_bufs, scale, eps, clamp=None):
        sc = self.sc
        t, b = self.stt()
        if clamp is not None:
            sc.op("dve", lambda e: e.tensor_scalar(t[:], src_ap, clamp, None, ALU.max), reads=src_bufs, writes=[b])
            sc.op("act", lambda e: e.activation(t[:], t[:], AF.Ln), reads=[b], writes=[b])
        else:
            sc.op("act", lambda e: e.activation(t[:], src_ap, AF.Ln, bias=float(eps), scale=scale), reads=src_bufs + [self.b_consts], writes=[b])
        sc.op("act", lambda e: e.activation(t[:], t[:], AF.Exp, scale=-0.5), reads=[b], writes=[b])
        return t, b

    def ln_stats(self, x_aps, x_bufs, ones_off, nfeat, eps):
        sc, T = self.sc, self.T
        ones = self.cst(ones_off, 128)
        n = len(x_aps)
        sqs = []
        for i in range(n):
            t, b = self.stt()
            sc.op("dve", lambda e, t=t, i=i: e.tensor_tensor(t[:], x_aps[i], x_aps[i], ALU.mult), reads=[x_bufs[i]], writes=[b])
            sqs.append((t, b))
        p1 = self.ps(T)
        for i in range(n):
            sc.op("pe", lambda e, i=i: e.matmul(p1.ap, ones, x_aps[i], start=(i == 0), stop=(i == n - 1)),
                  reads=[x_bufs[i], self.b_consts], writes=p1.bufs)
        p2 = self.ps(T)
        for i in range(n):
            sc.op("pe", lambda e, i=i: e.matmul(p2.ap, ones, sqs[i][0][:], start=(i == 0), stop=(i == n - 1)),
                  reads=[sqs[i][1], self.b_consts], writes=p2.bufs)
        mean, bm = self.stt()
        sc.op("act", lambda e: e.activation(mean[:], p1.ap, AF.Identity, scale=1.0 / nfeat), reads=p1.bufs, writes=[bm])
        msq, bq = self.stt()
        sc.op("dve", lambda e: e.tensor_tensor(msq[:], mean[:], mean[:], ALU.mult), reads=[bm], writes=[bq])
        var, bv = self.stt()
        sc.op("dve", lambda e: e.scalar_tensor_tensor(var[:], p2.ap, 1.0 / nfeat, msq[:], ALU.mult, ALU.subtract),
              reads=p2.bufs + [bq], writes=[bv])
        rs, brs = self.rstd_from(var[:], [bv], 1.0, eps)
        return mean, bm, rs, brs

    def tile_layer(self, ti, l):
        sc, T, NCH = self.sc, self.T, self.NCH
        t0 = ti * T
        wdt = self.wdt
        ident = self.cst(C_ID, 128)
        ones = self.cst(C_ONES, 128)
        sc.op("sp", lambda e: e.dma_start(out=self.lp[:, 0:512], in_=self.d_sguT[l]), writes=[self.b_lp], dsem=self.s_lp)
        sc.op("sp", lambda e: e.dma_start(out=self.lp[:, 512:768], in_=self.d_sgub[l]), writes=[self.b_lp], dsem=self.s_lp)
        sc.op("sp", lambda e: e.dma_start(out=self.lp[:, 768:1280], in_=self.d_pww[l]), writes=[self.b_lp], dsem=self.s_lp)
        sc.op("sp", lambda e: e.dma_start(out=self.lp[:, 1280:2304], in_=self.d_lora[l]), writes=[self.b_lp], dsem=self.s_lp)
        sguT = self.lp[:, 0:512]
        sc.op("pool", lambda e: e.memset(self.lp[64:128, 0:512].rearrange("p (h i) -> p h i", h=4)[:, :, 0:64], 0.0),
              reads=[self.b_lp], writes=[self.b_lp])
        qd = "sp" if wdt == F32 else "pool"
        sc.op(qd, lambda e: e.dma_start(out=self.pt[:], in_=self.d_p[l, :, t0:t0 + T].rearrange("(c p) t -> p c t", p=128)),
              writes=[self.b_pt], dsem=self.s_pt)

        if self.stage < 1:
            return
        sqs0 = []
        for c in range(8):
            t, b = self.stt()
            sc.op("act", lambda e, t=t, c=c: e.activation(t[:], self.xt[:, c, :], AF.Square), reads=[self.b_xt[c]], writes=[b])
            sqs0.append((t, b))
        pss0 = self.ps(T)
        for c in range(8):
            sc.op("pe", lambda e, c=c: e.matmul(pss0.ap, ones, sqs0[c][0][:], start=(c == 0), stop=(c == 7)),
                  reads=[sqs0[c][1], self.b_consts], writes=pss0.bufs)
        rs0, brs0 = self.rstd_from(pss0.ap, pss0.bufs, 1.0 / D, RMS_EPS)
        for c in range(8):
            sc.op("dve", lambda e, c=c: e.scalar_tensor_tensor(self.hT[:, c, :], self.xt[:, c, :], self.vcol(l, V_PREG + c), rs0[:], ALU.mult, ALU.mult),
                  reads=[self.b_xt[c], brs0, self.b_vecs], writes=[self.b_hT[c]])

        def zchunk(oc):
            wt, wb = self.wload(self.d_win[l, oc])
            p = self.ps(T)
            import os
            if os.environ.get("KDUP"):
                sc.op("pe", lambda e: e.matmul(p.ap, wt[:, 0:128], self.hT[:, 0, :], start=True, stop=True), reads=[wb, self.b_hT[0]], writes=p.bufs)
            for kc in range(8):
                sc.op("pe", lambda e, kc=kc: e.matmul(p.ap, wt[:, kc * 128:(kc + 1) * 128], self.hT[:, kc, :], start=(kc == 0), stop=(kc == 7)),
                      reads=[wb, self.b_hT[kc]], writes=p.bufs)
            return p

        if self.stage < 2.1:
            if self.stage == 1.5:
                for c in range(8):
                    sc.op("act", lambda e, c=c: e.activation(self.xt[:, c, :], self.hT[:, c, :], AF.Identity), reads=[self.b_hT[c]], writes=[self.b_xt[c]])
            return
        if self.stage == 2.17:
            wt, wb = self.wload(self.d_win[l, 2])
            p = self.ps(T)
            sc.op("pe", lambda e: e.matmul(p.ap, wt[:, 0:128], self.hT[:, 0, :], start=True, stop=True), reads=[wb, self.b_hT[0]], writes=p.bufs)
            sc.op("act", lambda e: e.activation(self.xt[:, 0, :], p.ap, AF.Identity), reads=p.bufs, writes=[self.b_xt[0]])
            p2 = self.ps(T)
            for kc in range(8):
                sc.op("pe", lambda e, kc=kc: e.matmul(p2.ap, wt[:, kc * 128:(kc + 1) * 128], self.hT[:, kc, :], start=(kc == 0), stop=(kc == 7)),
                      reads=[wb, self.b_hT[kc]], writes=p2.bufs)
            sc.op("act", lambda e: e.activation(self.xt[:, 1, :], p2.ap, AF.Identity), reads=p2.bufs, writes=[self.b_xt[1]])
            p3 = self.ps(T)
            for kc in range(8):
                sc.op("pe", lambda e, kc=kc: e.matmul(p3.ap, wt[:, kc * 128:(kc + 1) * 128], self.hT[:, kc, :], start=(kc == 0), stop=(kc == 7)),
                      reads=[wb, self.b_hT[kc]], writes=p3.bufs)
            sc.op("dve", lambda e: e.tensor_copy(self.xt[:, 2, :], p3.ap), reads=p3.bufs, writes=[self.b_xt[2]])
            return
        if self.stage == 2.15:
            for i in range(3):
                wt, wb = self.wload(self.d_win[l, 2 + i])
                sc.op("act", lambda e, wt=wt, i=i: e.activation(self.xt[:, i, :], wt[:, 0:256], AF.Identity), reads=[wb], writes=[self.b_xt[i]])
                sc.op("act", lambda e, wt=wt, i=i: e.activation(self.xt[:, 3 + i, :], wt[:, 768:1024], AF.Identity), reads=[wb], writes=[self.b_xt[3 + i]])
            return
        sga = []
        for c in range(2):
            p = zchunk(4 + c)
            t, b = self.stt()
            sc.op("act", lambda e, t=t, p=p: e.activation(t[:], p.ap, AF.Silu), reads=p.bufs, writes=[b])
            sga.append((t, b))
        for c in range(2):
            p = zchunk(0 + c)
            sc.op("dve", lambda e, c=c, p=p: e.tensor_tensor(self.ug[:, c, :], p.ap, sga[c][0][:], ALU.mult),
                  reads=p.bufs + [sga[c][1]], writes=[self.b_ug[c]])
        for c in range(2):
            p = zchunk(2 + c)
            sc.op("act", lambda e, c=c, p=p: e.activation(self.va[:, c, :], p.ap, AF.Identity), reads=p.bufs, writes=[self.b_va[c]])
        if self.stage < 2.2:
            if self.stage == 2.19:
                srcs = [(self.va[:, 0, :], self.b_va[0]), (self.va[:, 1, :], self.b_va[1]), (self.ug[:, 0, :], self.b_ug[0]), (self.ug[:, 1, :], self.b_ug[1])]
                for c in range(4):
                    sc.op("act", lambda e, c=c: e.activation(self.xt[:, c, :], srcs[c][0], AF.Identity), reads=[srcs[c][1]], writes=[self.b_xt[c]])
            return
        meanA, bmA, rsA, brsA = self.ln_stats([self.va[:, c, :] for c in range(2)], self.b_va, C_ONES, AW, LN_EPS)
        if self.stage < 2.4:
            if self.stage == 2.3:
                srcs = [(self.va[:, 0, :], self.b_va[0]), (self.va[:, 1, :], self.b_va[1]), (self.ug[:, 0, :], self.b_ug[0]), (self.ug[:, 1, :], self.b_ug[1])]
                for c in range(4):
                    sc.op("act", lambda e, c=c: e.activation(self.xt[:, c, :], srcs[c][0], AF.Identity), reads=[srcs[c][1]], writes=[self.b_xt[c]], force=True)
            return
        for c in range(2):
            t, b = self.stt()
            sc.op("dve", lambda e, c=c, t=t: e.tensor_tensor(t[:], self.va[:, c, :], meanA[:], ALU.subtract), reads=[self.b_va[c], bmA], writes=[b])
            sc.op("dve", lambda e, t=t: e.tensor_tensor(t[:], t[:], rsA[:], ALU.mult), reads=[b, brsA], writes=[b])
            sc.op("act", lambda e, c=c, t=t: e.activation(self.vn[:, c, :], t[:], AF.Identity, bias=self.vcol(l, V_SLB + c), scale=self.vcol(l, V_SLG + c)),
                  reads=[b, self.b_vecs], writes=[self.b_vn[c]])
        if self.stage < 2.6:
            return
        for blk in range(self.NBLK):
            for c in range(2):
                p = self.ps(128)
                sc.op("pe", lambda e, p=p, c=c, blk=blk: e.transpose(p.ap, self.vn[:, c, blk * 128:(blk + 1) * 128], ident),
                      reads=[self.b_vn[c], self.b_consts], writes=p.bufs)
                for hh in range(2):
                    sc.op("act", lambda e, p=p, c=c, blk=blk, hh=hh: e.activation(self.vntok[hh][:, blk, c * 128 + hh * 64:c * 128 + hh * 64 + 64], p.ap[:, hh * 64:hh * 64 + 64], AF.Identity),
                          reads=p.bufs, writes=[self.b_vntok[blk]])
        if self.stage < 2.8:
            return
        for blk in range(self.NBLK):
            for c in range(2):
                p = self.ps(128)
                for hh in range(2):
                    h = 2 * c + hh
                    sc.op("pe", lambda e, p=p, c=c, hh=hh, h=h, blk=blk: e.matmul(
                        p.ap, self.vntok[hh][:, blk, c * 128:(c + 1) * 128],
                        self.lp[:, h * 128:(h + 1) * 128], start=(hh == 0), stop=(hh == 1)),
                        reads=[self.b_vntok[blk], self.b_lp], writes=p.bufs)
                t, b = self.stt()
                sc.op("dve", lambda e, p=p, c=c, t=t: e.tensor_tensor(t[:, 0:128], p.ap, self.lp[:, 512 + c * 128:512 + (c + 1) * 128], ALU.add),
                      reads=p.bufs + [self.b_lp], writes=[b])
                sc.op("dve", lambda e, c=c, t=t, blk=blk: e.tensor_tensor(self.mixT[:, c, blk * 128:(blk + 1) * 128], t[:, 0:128],
                                                                           self.ug[:, c, blk * 128:(blk + 1) * 128], ALU.mult),
                      reads=[b, self.b_ug[c]], writes=[self.b_mix[c]])

        if self.stage < 3:
            return
        sgl = []
        for c in range(2):
            p = zchunk(8 + c)
            t, b = self.stt()
            sc.op("act", lambda e, t=t, p=p: e.activation(t[:], p.ap, AF.Sigmoid), reads=p.bufs, writes=[b])
            sgl.append((t, b))
        for c in range(2):
            hoff = (l * 2 + c) * HALO
            sc.op("pool", lambda e, c=c, hoff=hoff: e.tensor_copy(self.ybuf[:, c, 0:HALO], self.halo[:, hoff:hoff + HALO]),
                  reads=[self.b_halo], writes=[self.b_ybuf[c]])
            p = zchunk(6 + c)
            sc.op("dve", lambda e, c=c, p=p: e.tensor_tensor(self.ybuf[:, c, HALO:HALO + T], p.ap, sgl[c][0][:], ALU.mult),
                  reads=p.bufs + [sgl[c][1]], writes=[self.b_ybuf[c]])
            sc.op("pool", lambda e, c=c, hoff=hoff: e.tensor_copy(self.halo[:, hoff:hoff + HALO], self.ybuf[:, c, T:T + HALO]),
                  reads=[self.b_ybuf[c]], writes=[self.b_halo])
        for c in range(2):
            p = zchunk(10 + c)
            sc.op("act", lambda e, c=c, p=p: e.activation(self.sgb[:, c, :], p.ap, AF.Silu), reads=p.bufs, writes=[self.b_sgb[c]])
        for c in range(2):
            p = self.ps(T)
            for tap in range(CONVW):
                di = self.dgptr
                self.dgptr = (di + 1) % self.NDG
                dg, dgb = self.diag[di], self.b_diag[di]
                sc.op("pool", lambda e, dg=dg, c=c, tap=tap: e.tensor_scalar(dg[:], ident, self.vcol(l, V_CW + c * CONVW + tap), None, ALU.mult),
                      reads=[self.b_consts, self.b_vecs], writes=[dgb])
                sc.op("pe", lambda e, dg=dg, c=c, tap=tap, p=p: e.matmul(p.ap, dg[:], self.ybuf[:, c, tap:tap + T], start=(tap == 0), stop=(tap == CONVW - 1)),
                      reads=[dgb, self.b_ybuf[c]], writes=p.bufs)
            sc.op("act", lambda e, c=c, p=p: e.activation(self.yc[:, c, :], p.ap, AF.Identity, bias=self.vcol(l, V_CB + c)),
                  reads=p.bufs + [self.b_vecs], writes=[self.b_yc[c]])
        meanB, bmB, rsB, brsB = self.ln_stats([self.yc[:, c, :] for c in range(2)], self.b_yc, C_ONES, BW, LN_EPS)
        for c in range(2):
            t, b = self.stt()
            sc.op("dve", lambda e, c=c, t=t: e.tensor_tensor(t[:], self.yc[:, c, :], meanB[:], ALU.subtract), reads=[self.b_yc[c], bmB], writes=[b])
            sc.op("dve", lambda e, t=t: e.tensor_tensor(t[:], t[:], rsB[:], ALU.mult), reads=[b, brsB], writes=[b])
            sc.op("act", lambda e, c=c, t=t: e.activation(self.yn[:, c, :], t[:], AF.Silu, bias=self.vcol(l, V_CLB + c), scale=self.vcol(l, V_CLG + c)),
                  reads=[b, self.b_vecs], writes=[self.b_yn[c]])
        for co in range(2):
            p = self.ps(T)
            for ci in range(2):
                sc.op("pe", lambda e, p=p, ci=ci, co=co: e.matmul(p.ap, self.lp[:, 768 + ci * 256 + co * 128:768 + ci * 256 + (co + 1) * 128], self.yn[:, ci, :],
                                                                 start=(ci == 0), stop=(ci == 1)),
                      reads=[self.b_lp, self.b_yn[ci]], writes=p.bufs)
            sc.op("dve", lambda e, p=p, co=co: e.scalar_tensor_tensor(self.mixT[:, 2 + co, :], p.ap, self.vcol(l, V_PWB + co), self.sgb[:, co, :], ALU.add, ALU.mult),
                  reads=p.bufs + [self.b_sgb[co], self.b_vecs], writes=[self.b_mix[2 + co]])

        if self.stage < 4:
            return
        for c in range(4):
            p = zchunk(25 + c)
            sc.op("act", lambda e, c=c, p=p: e.activation(self.sgc[:, c, :], p.ap, AF.Silu), reads=p.bufs, writes=[self.b_sgc[c]])
        for c in range(13):
            p = zchunk(12 + c)
            zl = self.zlast[:, l * 13 + c:l * 13 + c + 1]
            sc.op("act", lambda e, c=c, p=p: e.activation(self.zs[:, c, :], p.ap, AF.Identity, scale=self.dcol(l, DV_OMM + c)),
                  reads=p.bufs + [self.b_dv], writes=[self.b_zs[c]])
            sc.op("dve", lambda e, c=c, p=p: e.scalar_tensor_tensor(self.zs[:, c, 1:T], p.ap[:, 0:T - 1], self.vcol(l, V_MU + c), self.zs[:, c, 1:T], ALU.mult, ALU.add),
                  reads=p.bufs + [self.b_zs[c], self.b_vecs], writes=[self.b_zs[c]])
            sc.op("dve", lambda e, c=c, zl=zl: e.scalar_tensor_tensor(self.zs[:, c, 0:1], zl, self.vcol(l, V_MU + c), self.zs[:, c, 0:1], ALU.mult, ALU.add),
                  reads=[self.b_zlast, self.b_zs[c], self.b_vecs], writes=[self.b_zs[c]])
            sc.op("act", lambda e, p=p, zl=zl: e.activation(zl, p.ap[:, T - 1:T], AF.Identity), reads=p.bufs, writes=[self.b_zlast])
        sc.op("act", lambda e: e.activation(self.zs[0:64, 12, :], self.zs[0:64, 12, :], AF.Tanh), reads=[self.b_zs[12]], writes=[self.b_zs[12]])
        if self.stage < 5 and self.stage != 4.5:
            return
        for pr in range(4):
            self.rwkv_pair(l, pr)
        for j in range(NCH):
            gens = [self.scan_unit(l, pr, j) for pr in range(4)]
            while gens:
                for g in list(gens):
                    try:
                        next(g)
                    except StopIteration:
                        gens.remove(g)
        for pr in range(4):
            self.rwkv_post(l, pr)
        if self.stage < 7:
            if self.stage == 4.5:
                srcs = [(self.va[:, 0, :], self.b_va[0]), (self.va[:, 1, :], self.b_va[1]), (self.ug[:, 0, :], self.b_ug[0]), (self.ug[:, 1, :], self.b_ug[1]),
                        (self.sgb[:, 0, :], self.b_sgb[0]), (self.yc[:, 0, :], self.b_yc[0]), (self.zs[:, 0, :], self.b_zs[0]), (self.sgc[:, 0, :], self.b_sgc[0])]
                for c in range(8):
                    sc.op("act", lambda e, c=c: e.activation(self.xt[:, c, :], srcs[c][0], AF.Identity), reads=[srcs[c][1]], writes=[self.b_xt[c]], force=True)
            if self.stage == 6.5:
                for c in range(8):
                    sc.op("act", lambda e, c=c: e.activation(self.xt[:, c, :], self.mixT[:, c, :], AF.Identity), reads=[self.b_mix[c]], writes=[self.b_xt[c]])
            return

        for oc in range(8):
            wt, wb = self.wload(self.d_wout[l, oc])
            p = self.ps(T)
            for kc in range(8):
                sc.op("pe", lambda e, kc=kc, wt=wt, p=p: e.matmul(p.ap, wt[:, kc * 128:(kc + 1) * 128], self.mixT[:, kc, :], start=(kc == 0), stop=(kc == 7)),
                      reads=[wb, self.b_mix[kc]], writes=p.bufs)
            sc.op("act", lambda e, oc=oc, p=p: e.activation(self.mo[:, oc, :], p.ap, AF.Identity), reads=p.bufs, writes=[self.b_mo[oc]])
        sqs1 = []
        for c in range(8):
            t, b = self.stt()
            sc.op("act", lambda e, t=t, c=c: e.activation(t[:], self.mo[:, c, :], AF.Square), reads=[self.b_mo[c]], writes=[b])
            sqs1.append((t, b))
        pss1 = self.ps(T)
        for c in range(8):
            sc.op("pe", lambda e, c=c: e.matmul(pss1.ap, ones, sqs1[c][0][:], start=(c == 0), stop=(c == 7)),
                  reads=[sqs1[c][1], self.b_consts], writes=pss1.bufs)
        rs1, brs1 = self.rstd_from(pss1.ap, pss1.bufs, 1.0 / D, RMS_EPS)
        for c in range(8):
            sc.op("dve", lambda e, c=c: e.scalar_tensor_tensor(self.mo[:, c, :], self.mo[:, c, :], self.vcol(l, V_POSTG + c), rs1[:], ALU.mult, ALU.mult),
                  reads=[self.b_mo[c], brs1, self.b_vecs], writes=[self.b_mo[c]])
            sc.op("dve", lambda e, c=c: e.tensor_tensor(self.xt[:, c, :], self.xt[:, c, :], self.mo[:, c, :], ALU.add),
                  reads=[self.b_xt[c], self.b_mo[c]], writes=[self.b_xt[c]])
        if wdt != F32:
            for c in range(8):
                sc.op("act", lambda e, c=c: e.activation(self.hT[:, c, :], self.xt[:, c, :], AF.Identity), reads=[self.b_xt[c]], writes=[self.b_hT[c]])
            xsrc, xb = self.hT, self.b_hT
        else:
            xsrc, xb = self.xt, self.b_xt
        for oc in range(8):
            wt, wb = self.wload(self.d_gw[l, oc])
            p = self.ps(T)
            for kc in range(8):
                sc.op("pe", lambda e, kc=kc, wt=wt, p=p: e.matmul(p.ap, wt[:, kc * 128:(kc + 1) * 128], xsrc[:, kc, :], start=(kc == 0), stop=(kc == 7)),
                      reads=[wb, xb[kc]], writes=p.bufs)
            sc.op("act", lambda e, oc=oc, p=p: e.activation(self.mo[:, oc, :], p.ap, AF.Sigmoid, bias=self.vcol(l, V_GATEB + oc)),
                  reads=p.bufs + [self.b_vecs], writes=[self.b_mo[oc]])
        for oc in range(8):
            wt, wb = self.wload(self.d_plew[l, oc], 256)
            p = self.ps(T)
            for kc in range(2):
                sc.op("pe", lambda e, kc=kc, wt=wt, p=p: e.matmul(p.ap, wt[:, kc * 128:(kc + 1) * 128], self.pt[:, kc, :], start=(kc == 0), stop=(kc == 1)),
                      reads=[wb, self.b_pt], writes=p.bufs)
            sc.op("dve", lambda e, oc=oc, p=p: e.tensor_tensor(self.mo[:, oc, :], p.ap, self.mo[:, oc, :], ALU.mult),
                  reads=p.bufs + [self.b_mo[oc]], writes=[self.b_mo[oc]])
            sc.op("dve", lambda e, oc=oc: e.tensor_tensor(self.xt[:, oc, :], self.xt[:, oc, :], self.mo[:, oc, :], ALU.add),
                  reads=[self.b_xt[oc], self.b_mo[oc]], writes=[self.b_xt[oc]])

    def rwkv_pair(self, l, pr):
        sc, T, NCH = self.sc, self.T, self.NCH
        ident = self.cst(C_ID, 128)
        blk = self.cst(C_BLK, 128)
        P = dict(self.pt_sh); P.update(self.pt_pp[pr])
        Bf = dict(self.b_pt_sh); Bf.update(self.b_pt_pp[pr])
        pads, b_pads = self.pads4[pr], self.b_pads4[pr]
        Rt, b_Rt = self.Rt4[pr], self.b_Rt4[pr]
        r_ap, r_b = self.zs[:, 0 + pr, :], self.b_zs[0 + pr]
        k_ap, k_b = self.zs[:, 4 + pr, :], self.b_zs[4 + pr]
        v_ap, v_b = self.zs[:, 8 + pr, :], self.b_zs[8 + pr]
        lora_w = self.lp[:, 1280 + pr * 128:1280 + (pr + 1) * 128]
        lora_a = self.lp[:, 1792 + pr * 128:1792 + (pr + 1) * 128]
        p = self.ps(T)
        sc.op("pe", lambda e: e.matmul(p.ap, lora_w, self.zs[:, 12, :], start=True, stop=True), reads=[self.b_lp, self.b_zs[12]], writes=p.bufs)
        sc.op("act", lambda e: e.activation(P["lw"][:], p.ap, AF.Sigmoid, bias=self.vcol(l, V_W0 + pr)), reads=p.bufs + [self.b_vecs], writes=[Bf["lw"]])
        p2 = self.ps(T)
        sc.op("pe", lambda e: e.matmul(p2.ap, lora_a, self.zs[:, 12, :], start=True, stop=True), reads=[self.b_lp, self.b_zs[12]], writes=p2.bufs)
        sc.op("act", lambda e: e.activation(P["aa"][:], p2.ap, AF.Sigmoid, bias=self.vcol(l, V_A0 + pr)), reads=p2.bufs + [self.b_vecs], writes=[Bf["aa"]])
        sc.op("dve", lambda e: e.tensor_scalar(P["lw"][:], P["lw"][:], -DECAY, None, ALU.mult), reads=[Bf["lw"]], writes=[Bf["lw"]])
        sc.op("dve", lambda e: e.tensor_tensor_scan(P["cum"][:], self.cst(C_RESET, T), P["lw"][:], 0.0, ALU.mult, ALU.add),
              reads=[Bf["lw"], self.b_consts], writes=[Bf["cum"]])
        sc.op("dve", lambda e: e.tensor_tensor(P["cume"][:], P["cum"][:], P["lw"][:], ALU.subtract), reads=[Bf["cum"], Bf["lw"]], writes=[Bf["cume"]])
        sc.op("act", lambda e: e.activation(P["E1"][:], P["cum"][:], AF.Exp), reads=[Bf["cum"]], writes=[Bf["E1"]])
        sc.op("act", lambda e: e.activation(P["E2"][:], P["cum"][:], AF.Exp, scale=-1.0), reads=[Bf["cum"]], writes=[Bf["E2"]])
        sc.op("act", lambda e: e.activation(P["E3"][:], P["cume"][:], AF.Exp), reads=[Bf["cume"]], writes=[Bf["E3"]])
        sc.op("dve", lambda e: e.tensor_scalar(P["kk0"][:], k_ap, self.vcol(l, V_KK + pr), None, ALU.mult), reads=[k_b, self.b_vecs], writes=[Bf["kk0"]])
        t, b = self.stt()
        sc.op("act", lambda e: e.activation(t[:], P["kk0"][:], AF.Square), reads=[Bf["kk0"]], writes=[b])
        p3 = self.ps(T)
        sc.op("pe", lambda e: e.matmul(p3.ap, blk, t[:], start=True, stop=True), reads=[b, self.b_consts], writes=p3.bufs)
        rn, brn = self.rstd_from(p3.ap, p3.bufs, 1.0, 0.0, clamp=1e-12)
        sc.op("dve", lambda e: e.tensor_tensor(P["kkn"][:], P["kk0"][:], rn[:], ALU.mult), reads=[Bf["kk0"], brn], writes=[Bf["kkn"]])
        t2, b2 = self.stt()
        sc.op("dve", lambda e: e.tensor_scalar(t2[:], P["aa"][:], self.vcol(l, V_KA + pr), self.dcol(l, DV_OMKA + pr), ALU.mult, ALU.add),
              reads=[Bf["aa"], self.b_vecs, self.b_dv], writes=[b2])
        sc.op("dve", lambda e: e.tensor_tensor(P["kmod"][:], k_ap, t2[:], ALU.mult), reads=[k_b, b2], writes=[Bf["kmod"]])
        sc.op("dve", lambda e: e.tensor_tensor(P["bvec"][:], P["kkn"][:], P["aa"][:], ALU.mult), reads=[Bf["kkn"], Bf["aa"]], writes=[Bf["bvec"]])
        def v3(ap):
            return ap.rearrange("p (j t) -> p j t", t=CH)

        for hh in range(2):
            rows = slice(hh * 64, hh * 64 + 64)
            def pv(n, hh=hh, rows=rows):
                return pads[n][rows, :].rearrange("p (j h t) -> p j h t", h=2, t=CH)[:, :, hh, :]
            sc.op("dve", lambda e, rows=rows, pv=pv: e.scalar_tensor_tensor(pv("A"), v3(P["kkn"][rows, :]), -1.0, v3(P["E3"][rows, :]), ALU.mult, ALU.mult),
                  reads=[Bf["kkn"], Bf["E3"]], writes=b_pads["A"])
            sc.op("dve", lambda e, rows=rows, pv=pv: e.tensor_tensor(pv("B"), v3(P["bvec"][rows, :]), v3(P["E2"][rows, :]), ALU.mult),
                  reads=[Bf["bvec"], Bf["E2"]], writes=b_pads["B"])
            sc.op("dve", lambda e, rows=rows, pv=pv: e.tensor_tensor(pv("K"), v3(P["kmod"][rows, :]), v3(P["E2"][rows, :]), ALU.mult),
                  reads=[Bf["kmod"], Bf["E2"]], writes=b_pads["K"])
            sc.op("act", lambda e, rows=rows, pv=pv: e.activation(pv("V"), v3(self.zs[rows, 8 + pr, :]), AF.Identity),
                  reads=[v_b], writes=b_pads["V"])
        sc.op("dve", lambda e: e.tensor_tensor(Rt[:], r_ap, P["E1"][:], ALU.mult), reads=[r_b, Bf["E1"]], writes=[b_Rt])

    def rwkv_post(self, l, pr):
        sc, T, NCH = self.sc, self.T, self.NCH
        blk = self.cst(C_BLK, 128)
        P = dict(self.pt_sh); P.update(self.pt_pp[pr])
        Bf = dict(self.b_pt_sh); Bf.update(self.b_pt_pp[pr])
        r_ap, r_b = self.zs[:, 0 + pr, :], self.b_zs[0 + pr]
        v_ap, v_b = self.zs[:, 8 + pr, :], self.b_zs[8 + pr]
        yy, byy = P["yy"], Bf["yy"]
        meanC, bmC, rsC, brsC = self.ln_stats([yy[:]], [byy], C_BLK, CH, GN_EPS)
        tpo, bpo = self.stt()
        sc.op("dve", lambda e: e.tensor_tensor(tpo[:], yy[:], meanC[:], ALU.subtract), reads=[byy, bmC], writes=[bpo])
        sc.op("dve", lambda e: e.tensor_tensor(tpo[:], tpo[:], rsC[:], ALU.mult), reads=[bpo, brsC], writes=[bpo])
        sc.op("act", lambda e: e.activation(tpo[:], tpo[:], AF.Identity, bias=self.vcol(l, V_LXB + pr), scale=self.vcol(l, V_LXG + pr)),
              reads=[bpo, self.b_vecs], writes=[bpo])
        t3, b3 = self.stt()
        sc.op("dve", lambda e: e.scalar_tensor_tensor(t3[:], r_ap, self.vcol(l, V_RK + pr), P["kmod"][:], ALU.mult, ALU.mult),
              reads=[r_b, Bf["kmod"], self.b_vecs], writes=[b3])
        p4 = self.ps(T)
        sc.op("pe", lambda e: e.matmul(p4.ap, blk, t3[:], start=True, stop=True), reads=[b3, self.b_consts], writes=p4.bufs)
        sc.op("dve", lambda e: e.tensor_tensor(t3[:], p4.ap, v_ap, ALU.mult), reads=p4.bufs + [v_b], writes=[b3])
        sc.op("dve", lambda e: e.tensor_tensor(tpo[:], tpo[:], t3[:], ALU.add), reads=[bpo, b3], writes=[bpo])
        sc.op("dve", lambda e: e.tensor_tensor(self.mixT[:, 4 + pr, :], tpo[:], self.sgc[:, pr, :], ALU.mult),
              reads=[bpo, self.b_sgc[pr]], writes=[self.b_mix[4 + pr]])

    def scan_unit(self, l, pr, j):
        sc, T = self.sc, self.T
        ident = self.cst(C_ID, 128)
        msu = self.cst(C_MSU, 128)
        msl = self.cst(C_MSL, 128)
        mui = self.cst(C_MUI, 64)
        U = self.ut[pr]
        cs = slice(j * 128, (j + 1) * 128)
        Ap, Bp, Kp, Vp = (self.pads4[pr][n][:, cs] for n in ("A", "B", "K", "V"))
        bA, bB, bK, bV = (self.b_pads4[pr][n][j] for n in ("A", "B", "K", "V"))
        Rst = self.Rt4[pr][:, j * CH:(j + 1) * CH]
        b_Rt = self.b_Rt4[pr]
        CB = self.b_consts

        def mm(out_ps, lhsT, rhs, reads, start=True, stop=True):
            sc.op("pe", lambda e: e.matmul(out_ps.ap, lhsT, rhs, start=start, stop=stop), reads=reads, writes=out_ps.bufs)

        def ev_mask(dst, p, mask):
            sc.op("dve", lambda e: e.tensor_tensor(dst[0][:], p.ap, mask, ALU.mult), reads=p.bufs + [CB], writes=[dst[1]])

        def ev_copy(dst, p, dst_ap=None):
            d = dst[0][:] if dst_ap is None else dst_ap
            sc.op("act", lambda e: e.activation(d, p.ap, AF.Identity), reads=p.bufs, writes=[dst[1]])

        p = self.ps(128); mm(p, Bp, Ap, [bB, bA]); ev_mask(U["B0"], p, msu)
        p = self.ps(128); mm(p, Ap, Bp, [bA, bB]); ev_mask(U["A0"], p, msl)
        p = self.ps(128); mm(p, Kp, Ap, [bK, bA]); ev_mask(U["MakT"], p, msu)
        p = self.ps(64); mm(p, Bp, Rst, [bB, b_Rt]); ev_mask(U["MrbT"], p, mui)
        p = self.ps(64); mm(p, Kp, Rst, [bK, b_Rt]); ev_mask(U["MrkT"], p, mui)
        yield
        def tr(dst, src, bsrc, dst_ap=None):
            p = self.ps(128)
            sc.op("pe", lambda e: e.transpose(p.ap, src, ident), reads=[bsrc, CB], writes=p.bufs)
            ev_copy(dst, p, dst_ap)
        tr(U["Z0"], Ap, bA, U["Z0"][0][:, 0:128])
        tr(U["Btok"], Bp, bB)
        tr(U["Ktok"], Kp, bK)
        tr(U["Vbd"], Vp, bV)
        yield
        p = self.ps(128); mm(p, U["MakT"][0][:], U["Vbd"][0][:], [U["MakT"][1], U["Vbd"][1]])
        ev_copy(U["Z0"], p, U["Z0"][0][:, 128:256])
        Acur, Bcur, Anext, Bnext = U["A0"], U["B0"], U["A1"], U["B1"]
        Zc, Zn = U["Z0"], U["Z1"]
        for n in range(6):
            yield
            p = self.ps(256)
            mm(p, Bcur[0][:], Zc[0][:], [Bcur[1], Zc[1]])
            sc.op("dve", lambda e, p=p, Zc=Zc, Zn=Zn: e.tensor_tensor(Zn[0][:], p.ap, Zc[0][:], ALU.add), reads=p.bufs + [Zc[1]], writes=[Zn[1]])
            Zc, Zn = Zn, Zc
            if n < 5:
                pb = self.ps(128); mm(pb, Acur[0][:], Bcur[0][:], [Acur[1], Bcur[1]])
                if n < 4:
                    pa = self.ps(128); mm(pa, Bcur[0][:], Acur[0][:], [Acur[1], Bcur[1]])
                ev_copy(Bnext, pb)
                if n < 4:
                    ev_copy(Anext, pa)
                Acur, Anext = Anext, Acur
                Bcur, Bnext = Bnext, Bcur
        yield
        Pm, Qm = Zc[0][:, 0:128], Zc[0][:, 128:256]
        bZ = Zc[1]
        p = self.ps(64)
        mm(p, ident, Rst, [CB, b_Rt], start=True, stop=False)
        mm(p, Pm, U["MrbT"][0][:], [bZ, U["MrbT"][1]], start=False, stop=True)
        ev_copy(U["Rp"], p)
        p = self.ps(128)
        mm(p, ident, ident, [CB], start=True, stop=False)
        mm(p, Pm, U["Btok"][0][:], [bZ, U["Btok"][1]], start=False, stop=True)
        ev_copy(U["G0"], p)
        yield
        st_ap = self.state[:, (l * 4 + pr) * 128:(l * 4 + pr + 1) * 128]
        bS = self.b_state[l][pr]
        py = self.ps(64)
        mm(py, Qm, U["MrbT"][0][:], [bZ, U["MrbT"][1]], start=True, stop=False)
        mm(py, U["Vbd"][0][:], U["MrkT"][0][:], [U["Vbd"][1], U["MrkT"][1]], start=False, stop=False)
        mm(py, st_ap, U["Rp"][0][:], [bS, U["Rp"][1]], start=False, stop=True)
        pS = self.ps(128)
        mm(pS, U["Btok"][0][:], Qm, [U["Btok"][1], bZ], start=True, stop=False)
        mm(pS, U["Ktok"][0][:], U["Vbd"][0][:], [U["Ktok"][1], U["Vbd"][1]], start=False, stop=False)
        mm(pS, U["G0"][0][:], st_ap, [U["G0"][1], bS], start=False, stop=True)
        yy, byy = self.pt_pp[pr]["yy"], self.b_pt_pp[pr]["yy"]
        sc.op("act", lambda e: e.activation(yy[:, j * CH:(j + 1) * CH], py.ap, AF.Identity), reads=py.bufs, writes=[byy])
        wc = self.pt_pp[pr]["E1"][:, j * CH + CH - 1:j * CH + CH]
        sc.op("dve", lambda e: e.tensor_scalar(st_ap, pS.ap, wc, None, ALU.mult), reads=pS.bufs + [self.b_pt_pp[pr]["E1"]], writes=[bS])


def make_consts(T):
    NC = C_RESET + T
    c = np.zeros((128, NC), np.float32)
    c[:, C_ID:C_ID + 128] = np.eye(128, dtype=np.float32)
    c[:, C_ONES:C_ONES + 128] = 1.0
    blk = np.zeros((128, 128), np.float32)
    blk[:64, :64] = 1.0
    blk[64:, 64:] = 1.0
    c[:, C_BLK:C_BLK + 128] = blk
    tri_u = np.triu(np.ones((64, 64), np.float32), 1)
    msu = np.zeros((128, 128), np.float32)
    msu[:64, :64] = tri_u
    msu[64:, 64:] = tri_u
    c[:, C_MSU:C_MSU + 128] = msu
    c[:, C_MSL:C_MSL + 128] = msu.T
    ui = np.triu(np.ones((64, 64), np.float32), 0)
    c[:64, C_MUI:C_MUI + 64] = ui
    c[64:, C_MUI:C_MUI + 64] = ui
    c[:64, C_PAD] = 1.0
    c[64:, C_PAD + 1] = 1.0
    c[:64, C_NPAD] = -1.0
    c[64:, C_NPAD + 1] = -1.0
    r = np.ones(T, np.float32)
    r[::CH] = 0.0
    c[:, C_RESET:C_RESET + T] = r[None, :]
    return c


def colmaj(v, n):
    return np.ascontiguousarray(v.reshape(n, 128).T)


def prep_shared(inp, NL, T):
    f = lambda a: np.ascontiguousarray(a, dtype=np.float32)
    sh = {}
    w_in = f(inp["w_in"][:NL])
    sh["win"] = np.ascontiguousarray(w_in.reshape(NL, 8, 128, NOC, 128).transpose(0, 3, 2, 1, 4)).reshape(NL, NOC, 128, 1024)
    sh["wout"] = np.ascontiguousarray(f(inp["w_out"][:NL]).reshape(NL, 8, 128, 8, 128).transpose(0, 3, 2, 1, 4)).reshape(NL, 8, 128, 1024)
    sh["gw"] = np.ascontiguousarray(f(inp["ple_gate_w"][:NL]).reshape(NL, 8, 128, 8, 128).transpose(0, 3, 2, 1, 4)).reshape(NL, 8, 128, 1024)
    sh["plew"] = np.ascontiguousarray(f(inp["ple_w"][:NL]).reshape(NL, 2, 128, 8, 128).transpose(0, 3, 2, 1, 4)).reshape(NL, 8, 128, 256)
    vecs = np.zeros((128, NL * NV), np.float32)
    for l in range(NL):
        o = l * NV
        def put(col, v, n):
            vecs[:, o + col:o + col + n] = colmaj(f(v), n)
        put(V_PREG, inp["pre_norm_g"][l], 8)
        put(V_POSTG, inp["post_norm_g"][l], 8)
        put(V_GATEB, inp["ple_gate_b"][l], 8)
        put(V_SLG, inp["sgu_ln_g"][l], 2)
        put(V_SLB, inp["sgu_ln_b"][l], 2)
        put(V_CB, inp["conv_b"][l], 2)
        put(V_CLG, inp["conv_ln_g"][l], 2)
        put(V_CLB, inp["conv_ln_b"][l], 2)
        put(V_PWB, inp["pw_b"][l], 2)
        put(V_MU, inp["shift_mu"][l], 13)
        put(V_W0, inp["w0"][l], 4)
        put(V_A0, inp["a0"][l], 4)
        put(V_KK, inp["k_k"][l], 4)
        put(V_KA, inp["k_a"][l], 4)
        put(V_RK, inp["r_k"][l].reshape(-1), 4)
        put(V_LXG, inp["lnx_g"][l], 4)
        put(V_LXB, inp["lnx_b"][l], 4)
        cw = f(inp["conv_w"][l])
        for c in range(2):
            vecs[:, o + V_CW + c * CONVW:o + V_CW + (c + 1) * CONVW] = cw[:, c * 128:(c + 1) * 128].T
    sh["vecs"] = vecs
    sh["sguT"] = np.ascontiguousarray(f(inp["sgu_w"][:NL]).transpose(0, 3, 1, 2)).reshape(NL, 128, 512)
    sb = f(inp["sgu_b"][:NL])
    sgub = np.zeros((NL, 128, 256), np.float32)
    for c in range(2):
        for hh in range(2):
            sgub[:, hh * 64:(hh + 1) * 64, c * 128:(c + 1) * 128] = sb[:, 2 * c + hh][:, None, :]
    sh["sgub"] = sgub
    sh["pww"] = np.ascontiguousarray(f(inp["pw_w"][:NL]).reshape(NL, 2, 128, 256).transpose(0, 2, 1, 3)).reshape(NL, 128, 512)
    lora = np.zeros((NL, 128, 1024), np.float32)
    lora[:, 0:64, 0:512] = f(inp["w_up"][:NL])
    lora[:, 64:128, 512:1024] = f(inp["a_up"][:NL])
    sh["lora"] = lora
    sh["consts"] = make_consts(T)
    return sh


_CACHE = {}


def run(inp, NL, NT, T, ncore, **kw):
    key = (NL, NT, T, tuple(sorted(kw.items())))
    S = NT * T
    prog = Prog(NL, NT, T, **kw)
    nc = prog.build()
    sh = prep_shared(inp, NL, T)
    in_maps = []
    for b in range(ncore):
        m = dict(sh)
        m["xT"] = np.ascontiguousarray(np.asarray(inp["x"][b, :S], np.float32).T)
        m["pT"] = np.ascontiguousarray(np.asarray(inp["p"][:NL, b, :S], np.float32).transpose(0, 2, 1))
        in_maps.append(m)
    res = run_bass_kernel_spmd(nc, in_maps, core_ids=list(range(ncore)))
    out = np.stack([np.ascontiguousarray(r["yT"].T) for r in res.results], axis=0)
    return out.astype(np.float32), prog


def kernel(**inputs):
    out, _ = run(inputs, NLAYER, SEQ // 256, 256, NCORE)
    return out
```

```python
import math
from contextlib import ExitStack

import numpy as np
import concourse.bass as bass
import concourse.mybir as mybir
from concourse.bass_utils import run_bass_kernel_spmd

F32 = mybir.dt.float32
BF16 = mybir.dt.bfloat16
AF = mybir.ActivationFunctionType
ALU = mybir.AluOpType

D = 1024
SEQ = 4096
NLAYER = 4
NCORE = 8
AW = 256
BW = 256
CW = 512
PLE = 256
CONVW = 31
HALO = CONVW - 1
INC = 3712
NOC = INC // 128
CH = 64
RMS_EPS = 1e-6
LN_EPS = 1e-5
GN_EPS = 64e-5
DECAY = math.exp(-0.5)

V_PREG, V_POSTG, V_GATEB = 0, 8, 16
V_SLG, V_SLB = 24, 26
V_CB, V_CLG, V_CLB, V_PWB = 28, 30, 32, 34
V_MU = 36
V_W0, V_A0, V_KK, V_KA, V_RK, V_LXG, V_LXB = 49, 53, 57, 61, 65, 69, 73
V_CW = 77
NV = V_CW + 2 * CONVW
DV_OMM, DV_OMKA = 0, 13
NDV = 17
C_ID, C_ONES, C_BLK, C_MSU, C_MSL, C_MUI, C_PAD, C_NPAD, C_RESET = 0, 128, 256, 384, 512, 640, 704, 706, 708


class Sem:
    def __init__(self, h):
        self.h = h
        self.cnt = 0


class Buf:
    __slots__ = ("name", "w", "r", "excl")

    def __init__(self, name, excl=False):
        self.name = name
        self.w = None
        self.r = {}
        self.excl = excl


class Eng:
    def __init__(self, name, sem):
        self.name = name
        self.sem = sem
        self.seen = {}
        self.items = []


class Sched:
    def __init__(self, nc, es):
        self.nc = nc
        self.es = es
        self.eng = {}
        for n in ("pe", "act", "dve", "pool", "sp"):
            self.eng[n] = Eng(n, self.new_sem("s_" + n))
        self.nops = 0

    def new_sem(self, name):
        return Sem(self.es.enter_context(self.nc.semaphore(name)))

    def _need(self, E, ev):
        sem, val = ev
        if E.seen.get(sem, 0) >= val:
            return
        E.items.append(("w", sem, val))
        E.seen[sem] = val

    def op(self, en, fn, reads=(), writes=(), dsem=None, force=False):
        import os
        lim = int(os.environ.get("KLIMIT", "0"))
        if lim and self.nops >= lim and not force:
            return None
        E = self.eng[en]
        if any(b.excl for b in reads):
            writes = list(writes) + [b for b in reads if b.excl]
            reads = [b for b in reads if not b.excl]
        for b in reads:
            if b.w is not None:
                if b.w[0] is E.sem and en == "pe":
                    continue
                self._need(E, b.w)
        for b in writes:
            if b.w is not None and b.w[0] is not E.sem:
                self._need(E, b.w)
            for sem, val in b.r.items():
                if sem is not E.sem:
                    self._need(E, (sem, val))
        if dsem is None:
            sem = E.sem
            sem.cnt += 1
            inc = 1
        else:
            sem = dsem
            sem.cnt += 16
            inc = 16
        ev = (sem, sem.cnt)
        E.items.append(("o", fn, sem, inc))
        for b in reads:
            b.r[sem] = sem.cnt
        for b in writes:
            b.w = ev
            b.r = {}
        self.nops += 1
        return ev

    def wait(self, en, ev):
        self._need(self.eng[en], ev)

    def replay(self, en, e):
        for it in self.eng[en].items:
            if it[0] == "w":
                e.wait_ge(it[1].h, it[2])
            else:
                it[1](e).then_inc(it[2].h, it[3])


class PsTile:
    def __init__(self, ap, bufs):
        self.ap = ap
        self.bufs = bufs


class Prog:
    def __init__(self, NL, NT, T, wdt=F32, sdt=F32, nslot=2, ugroup=4, stage=99):
        self.stage = stage
        self.NL, self.NT, self.T = NL, NT, T
        self.S = NT * T
        self.NCH = T // CH
        self.NBLK = T // 128
        self.wdt, self.sdt = wdt, sdt
        self.nslot = nslot
        self.ugroup = ugroup
        self.NC = C_RESET + T
        self.nc = bass.Bass("TRN2", target_bir_lowering=False)
        self.es = ExitStack()

    def dram(self, name, shape, kind="ExternalInput", dt=F32):
        return self.nc.dram_tensor(name, list(shape), dt, kind=kind).ap()

    def sb(self, name, shape, dt=F32):
        t = self.es.enter_context(self.nc.sbuf_tensor("sb_" + name, list(shape), dt))
        return t

    def build(self):
        nc, NL, T, S = self.nc, self.NL, self.T, self.S
        es = self.es
        with es:
            self.sc = Sched(nc, es)
            self.d_x = self.dram("xT", [D, S])
            self.d_p = self.dram("pT", [NL, PLE, S])
            self.d_win = self.dram("win", [NL, NOC, 128, 1024])
            self.d_wout = self.dram("wout", [NL, 8, 128, 1024])
            self.d_gw = self.dram("gw", [NL, 8, 128, 1024])
            self.d_plew = self.dram("plew", [NL, 8, 128, 256])
            self.d_vecs = self.dram("vecs", [128, NL * NV])
            self.d_sguT = self.dram("sguT", [NL, 128, 512])
            self.d_sgub = self.dram("sgub", [NL, 128, 256])
            self.d_pww = self.dram("pww", [NL, 128, 512])
            self.d_lora = self.dram("lora", [NL, 128, 1024])
            self.d_consts = self.dram("consts", [128, self.NC])
            self.d_y = self.dram("yT", [D, S], kind="ExternalOutput")
            self.alloc()
            self.emit()
            with nc.Block() as block:
                @block.tensor
                def _(e):
                    self.sc.replay("pe", e)

                @block.scalar
                def _(e):
                    self.sc.replay("act", e)

                @block.vector
                def _(e):
                    self.sc.replay("dve", e)

                @block.gpsimd
                def _(e):
                    self.sc.replay("pool", e)

                @block.sync
                def _(e):
                    self.sc.replay("sp", e)
        return nc

    def alloc(self):
        NL, T, NCH = self.NL, self.T, self.NCH
        sb = self.sb
        B = Buf
        self.xt = sb("xt", [128, 8, T]); self.b_xt = [B(f"xt{c}") for c in range(8)]
        self.hT = sb("hT", [128, 8, T], self.wdt); self.b_hT = [B(f"hT{c}") for c in range(8)]
        self.zs = sb("zs", [128, 13, T]); self.b_zs = [B(f"zs{c}") for c in range(13)]
        self.va = sb("va", [128, 2, T]); self.b_va = [B(f"va{c}") for c in range(2)]
        self.mixT = sb("mixT", [128, 8, T], self.wdt); self.b_mix = [B(f"mix{c}") for c in range(8)]
        self.mo = sb("mo", [128, 8, T]); self.b_mo = [B(f"mo{c}") for c in range(8)]
        self.wring = [sb(f"wr{i}", [128, 1024], self.wdt) for i in range(self.nslot)]
        self.b_wring = [B(f"wr{i}") for i in range(self.nslot)]
        self.s_wring = [self.sc.new_sem(f"swr{i}") for i in range(self.nslot)]
        self.wptr = 0
        self.NST = 10
        self.st = [sb(f"st{i}", [128, T]) for i in range(self.NST)]
        self.b_st = [B(f"st{i}") for i in range(self.NST)]
        self.stptr = 0
        self.pt = sb("pt", [128, 2, T], self.wdt); self.b_pt = B("pt"); self.s_pt = self.sc.new_sem("spt")
        self.vecs = sb("vecs", [128, NL * NV]); self.b_vecs = B("vecs")
        self.dv = sb("dv", [128, NL * NDV]); self.b_dv = B("dv")
        self.consts = sb("consts", [128, self.NC]); self.b_consts = B("consts")
        self.s_misc = self.sc.new_sem("smisc")
        self.s_x = self.sc.new_sem("sx")
        self.s_y = self.sc.new_sem("sy")
        self.lp = sb("lp", [128, 512 + 256 + 512 + 1024]); self.b_lp = B("lp"); self.s_lp = self.sc.new_sem("slp")
        self.ybuf = sb("ybuf", [128, 2, HALO + T]); self.b_ybuf = [B(f"ybuf{c}") for c in range(2)]
        self.halo = sb("halo", [128, NL * 2 * HALO]); self.b_halo = B("halo")
        self.zlast = sb("zlast", [128, NL * 13]); self.b_zlast = B("zlast")
        self.NDG = 8
        self.diag = [sb(f"dg{i}", [128, 128]) for i in range(self.NDG)]
        self.b_diag = [B(f"dg{i}") for i in range(self.NDG)]
        self.dgptr = 0
        self.state = sb("state", [128, NL * 4 * 128], self.sdt)
        self.b_state = [[B(f"state{l}_{p}") for p in range(4)] for l in range(NL)]
        self.vn = sb("vn", [128, 2, T]); self.b_vn = [B(f"vn{c}") for c in range(2)]
        self.vntok = [sb(f"vntok{hh}", [128, self.NBLK, 256]) for hh in range(2)]; self.b_vntok = [B(f"vntok{b}") for b in range(self.NBLK)]
        self.ug = sb("ug", [128, 2, T]); self.b_ug = [B(f"ug{c}") for c in range(2)]
        self.sgb = sb("sgb", [128, 2, T]); self.b_sgb = [B(f"sgb{c}") for c in range(2)]
        self.yc = sb("yc", [128, 2, T]); self.b_yc = [B(f"yc{c}") for c in range(2)]
        self.yn = sb("yn", [128, 2, T]); self.b_yn = [B(f"yn{c}") for c in range(2)]
        self.sgc = sb("sgc", [128, 4, T]); self.b_sgc = [B(f"sgc{c}") for c in range(4)]
        names = ["lw", "aa", "cum", "cume", "E2", "E3", "kk0", "kkn", "bvec"]
        self.pt_sh = {n: sb("c_" + n, [128, T]) for n in names}
        self.b_pt_sh = {n: B("c_" + n) for n in names}
        self.pt_pp = [{n: sb(f"c{pr}_" + n, [128, T]) for n in ("E1", "kmod", "yy")} for pr in range(4)]
        self.b_pt_pp = [{n: B(f"c{pr}_" + n) for n in ("E1", "kmod", "yy")} for pr in range(4)]
        self.Rt4 = [sb(f"Rt{pr}", [128, T], self.sdt) for pr in range(4)]; self.b_Rt4 = [B(f"Rt{pr}") for pr in range(4)]
        self.pads4 = [{n: sb(f"pad{pr}_" + n, [128, NCH * 128], self.sdt) for n in ("A", "B", "K", "V")} for pr in range(4)]
        self.b_pads4 = [{n: [B(f"pad{pr}_{n}{j}") for j in range(NCH)] for n in ("A", "B", "K", "V")} for pr in range(4)]
        self.NU = 4
        self.ut = []
        for u in range(self.NU):
            d = {}
            for n, w in (("A0", 128), ("A1", 128), ("B0", 128), ("B1", 128), ("MakT", 128), ("MrbT", 64), ("MrkT", 64),
                         ("Z0", 256), ("Z1", 256), ("Btok", 128), ("Ktok", 128), ("Vbd", 128), ("Rp", 64), ("G0", 128)):
                d[n] = (sb(f"u{u}_{n}", [128, w], self.sdt), B(f"u{u}_{n}"))
            self.ut.append(d)
        self.psb = [self.es.enter_context(self.nc.psum_tensor(f"ps{b}", [128, 512], F32)) for b in range(8)]
        self.b_psq = [B(f"psbank{b}", excl=True) for b in range(8)]
        self.psptr = 0

    def ps(self, ncols):
        b = self.psptr
        self.psptr = (b + 1) % 8
        return PsTile(self.psb[b][:, 0:ncols], [self.b_psq[b]])

    def stt(self):
        i = self.stptr
        self.stptr = (i + 1) % self.NST
        return self.st[i], self.b_st[i]

    def cst(self, off, n, rows=slice(0, 128)):
        return self.consts[rows, off:off + n]

    def vcol(self, l, col, rows=slice(0, 128)):
        return self.vecs[rows, l * NV + col:l * NV + col + 1]

    def dcol(self, l, col):
        return self.dv[:, l * NDV + col:l * NDV + col + 1]

    def emit(self):
        sc, NL, NT, T = self.sc, self.NL, self.NT, self.T
        sc.op("sp", lambda e: e.dma_start(out=self.consts[:], in_=self.d_consts), writes=[self.b_consts], dsem=self.s_misc)
        sc.op("sp", lambda e: e.dma_start(out=self.vecs[:], in_=self.d_vecs), writes=[self.b_vecs], dsem=self.s_misc)
        ev = (self.s_misc, self.s_misc.cnt)
        self.b_consts.w = ev
        self.b_vecs.w = ev
        for l in range(NL):
            sc.op("dve", lambda e, l=l: e.tensor_scalar(self.dv[:, l * NDV + DV_OMM:l * NDV + DV_OMM + 13],
                                                       self.vecs[:, l * NV + V_MU:l * NV + V_MU + 13], -1.0, 1.0, ALU.mult, ALU.add),
                  reads=[self.b_vecs], writes=[self.b_dv])
            sc.op("dve", lambda e, l=l: e.tensor_scalar(self.dv[:, l * NDV + DV_OMKA:l * NDV + DV_OMKA + 4],
                                                       self.vecs[:, l * NV + V_KA:l * NV + V_KA + 4], -1.0, 1.0, ALU.mult, ALU.add),
                  reads=[self.b_vecs], writes=[self.b_dv])
        sc.op("pool", lambda e: e.memset(self.state[:], 0.0), writes=[b for bl in self.b_state for b in bl])
        sc.op("pool", lambda e: e.memset(self.halo[:], 0.0), writes=[self.b_halo])
        sc.op("pool", lambda e: e.memset(self.zlast[:], 0.0), writes=[self.b_zlast])
        for pr in range(4):
            for n in ("A", "B", "K", "V"):
                sc.op("pool", lambda e, n=n, pr=pr: e.memset(self.pads4[pr][n][:], 0.0), writes=self.b_pads4[pr][n])
        for hh in range(2):
            sc.op("pool", lambda e, hh=hh: e.memset(self.vntok[hh][:], 0.0), writes=self.b_vntok)
        for ti in range(NT):
            t0 = ti * T
            sc.op("sp", lambda e, t0=t0: e.dma_start(out=self.xt[:], in_=self.d_x[:, t0:t0 + T].rearrange("(c p) t -> p c t", p=128)),
                  writes=self.b_xt, dsem=self.s_x)
            for l in range(NL):
                self.tile_layer(ti, l)
            ev = sc.op("sp", lambda e, t0=t0: e.dma_start(out=self.d_y[:, t0:t0 + T].rearrange("(c p) t -> p c t", p=128), in_=self.xt[:]),
                       reads=self.b_xt, dsem=self.s_y, force=True)
        sc.wait("sp", (self.s_y, self.s_y.cnt))

    def wload(self, src_ap, ncols=1024):
        i = self.wptr
        self.wptr = (i + 1) % self.nslot
        tile, buf, sem = self.wring[i], self.b_wring[i], self.s_wring[i]
        q = "sp" if self.wdt == F32 else "pool"
        self.sc.op(q, lambda e: e.dma_start(out=tile[:, 0:ncols], in_=src_ap), writes=[buf], dsem=sem)
        return tile, buf

    def rstd_from(self, src_ap, src_bufs, scale, eps, clamp=None):
        sc = self.sc
        t, b = self.stt()
        if clamp is not None:
            sc.op("dve", lambda e: e.tensor_scalar(t[:], src_ap, clamp, None, ALU.max), reads=src_bufs, writes=[b])
            sc.op("act", lambda e: e.activation(t[:], t[:], AF.Ln), reads=[b], writes=[b])
        else:
            sc.op("act", lambda e: e.activation(t[:], src_ap, AF.Ln, bias=float(eps), scale=scale), reads=src_bufs + [self.b_consts], writes=[b])
        sc.op("act", lambda e: e.activation(t[:], t[:], AF.Exp, scale=-0.5), reads=[b], writes=[b])
        return t, b

    def ln_stats(self, x_aps, x_bufs, ones_off, nfeat, eps):
        sc, T = self.sc, self.T
        ones = self.cst(ones_off, 128)
        n = len(x_aps)
        sqs = []
        for i in range(n):
            t, b = self.stt()
            sc.op("dve", lambda e, t=t, i=i: e.tensor_tensor(t[:], x_aps[i], x_aps[i], ALU.mult), reads=[x_bufs[i]], writes=[b])
            sqs.append((t, b))
        p1 = self.ps(T)
        for i in range(n):
            sc.op("pe", lambda e, i=i: e.matmul(p1.ap, ones, x_aps[i], start=(i == 0), stop=(i == n - 1)),
                  reads=[x_bufs[i], self.b_consts], writes=p1.bufs)
        p2 = self.ps(T)
        for i in range(n):
            sc.op("pe", lambda e, i=i: e.matmul(p2.ap, ones, sqs[i][0][:], start=(i == 0), stop=(i == n - 1)),
                  reads=[sqs[i][1], self.b_consts], writes=p2.bufs)
        mean, bm = self.stt()
        sc.op("act", lambda e: e.activation(mean[:], p1.ap, AF.Identity, scale=1.0 / nfeat), reads=p1.bufs, writes=[bm])
        msq, bq = self.stt()
        sc.op("dve", lambda e: e.tensor_tensor(msq[:], mean[:], mean[:], ALU.mult), reads=[bm], writes=[bq])
        var, bv = self.stt()
        sc.op("dve", lambda e: e.scalar_tensor_tensor(var[:], p2.ap, 1.0 / nfeat, msq[:], ALU.mult, ALU.subtract),
              reads=p2.bufs + [bq], writes=[bv])
        rs, brs = self.rstd_from(var[:], [bv], 1.0, eps)
        return mean, bm, rs, brs

    def tile_layer(self, ti, l):
        sc, T, NCH = self.sc, self.T, self.NCH
        t0 = ti * T
        wdt = self.wdt
        ident = self.cst(C_ID, 128)
        ones = self.cst(C_ONES, 128)
        sc.op("sp", lambda e: e.dma_start(out=self.lp[:, 0:512], in_=self.d_sguT[l]), writes=[self.b_lp], dsem=self.s_lp)
        sc.op("sp", lambda e: e.dma_start(out=self.lp[:, 512:768], in_=self.d_sgub[l]), writes=[self.b_lp], dsem=self.s_lp)
        sc.op("sp", lambda e: e.dma_start(out=self.lp[:, 768:1280], in_=self.d_pww[l]), writes=[self.b_lp], dsem=self.s_lp)
        sc.op("sp", lambda e: e.dma_start(out=self.lp[:, 1280:2304], in_=self.d_lora[l]), writes=[self.b_lp], dsem=self.s_lp)
        sguT = self.lp[:, 0:512]
        sc.op("pool", lambda e: e.memset(self.lp[64:128, 0:512].rearrange("p (h i) -> p h i", h=4)[:, :, 0:64], 0.0),
              reads=[self.b_lp], writes=[self.b_lp])
        qd = "sp" if wdt == F32 else "pool"
        sc.op(qd, lambda e: e.dma_start(out=self.pt[:], in_=self.d_p[l, :, t0:t0 + T].rearrange("(c p) t -> p c t", p=128)),
              writes=[self.b_pt], dsem=self.s_pt)

        if self.stage < 1:
            return
        sqs0 = []
        for c in range(8):
            t, b = self.stt()
            sc.op("act", lambda e, t=t, c=c: e.activation(t[:], self.xt[:, c, :], AF.Square), reads=[self.b_xt[c]], writes=[b])
            sqs0.append((t, b))
        pss0 = self.ps(T)
        for c in range(8):
            sc.op("pe", lambda e, c=c: e.matmul(pss0.ap, ones, sqs0[c][0][:], start=(c == 0), stop=(c == 7)),
                  reads=[sqs0[c][1], self.b_consts], writes=pss0.bufs)
        rs0, brs0 = self.rstd_from(pss0.ap, pss0.bufs, 1.0 / D, RMS_EPS)
        for c in range(8):
            sc.op("dve", lambda e, c=c: e.scalar_tensor_tensor(self.hT[:, c, :], self.xt[:, c, :], self.vcol(l, V_PREG + c), rs0[:], ALU.mult, ALU.mult),
                  reads=[self.b_xt[c], brs0, self.b_vecs], writes=[self.b_hT[c]])

        def zchunk(oc):
            wt, wb = self.wload(self.d_win[l, oc])
            p = self.ps(T)
            import os
            if os.environ.get("KDUP"):
                sc.op("pe", lambda e: e.matmul(p.ap, wt[:, 0:128], self.hT[:, 0, :], start=True, stop=True), reads=[wb, self.b_hT[0]], writes=p.bufs)
            for kc in range(8):
                sc.op("pe", lambda e, kc=kc: e.matmul(p.ap, wt[:, kc * 128:(kc + 1) * 128], self.hT[:, kc, :], start=(kc == 0), stop=(kc == 7)),
                      reads=[wb, self.b_hT[kc]], writes=p.bufs)
            return p

        if self.stage < 2.1:
            if self.stage == 1.5:
                for c in range(8):
                    sc.op("act", lambda e, c=c: e.activation(self.xt[:, c, :], self.hT[:, c, :], AF.Identity), reads=[self.b_hT[c]], writes=[self.b_xt[c]])
            return
        if self.stage == 2.17:
            wt, wb = self.wload(self.d_win[l, 2])
            p = self.ps(T)
            sc.op("pe", lambda e: e.matmul(p.ap, wt[:, 0:128], self.hT[:, 0, :], start=True, stop=True), reads=[wb, self.b_hT[0]], writes=p.bufs)
            sc.op("act", lambda e: e.activation(self.xt[:, 0, :], p.ap, AF.Identity), reads=p.bufs, writes=[self.b_xt[0]])
            p2 = self.ps(T)
            for kc in range(8):
                sc.op("pe", lambda e, kc=kc: e.matmul(p2.ap, wt[:, kc * 128:(kc + 1) * 128], self.hT[:, kc, :], start=(kc == 0), stop=(kc == 7)),
                      reads=[wb, self.b_hT[kc]], writes=p2.bufs)
            sc.op("act", lambda e: e.activation(self.xt[:, 1, :], p2.ap, AF.Identity), reads=p2.bufs, writes=[self.b_xt[1]])
            p3 = self.ps(T)
            for kc in range(8):
                sc.op("pe", lambda e, kc=kc: e.matmul(p3.ap, wt[:, kc * 128:(kc + 1) * 128], self.hT[:, kc, :], start=(kc == 0), stop=(kc == 7)),
                      reads=[wb, self.b_hT[kc]], writes=p3.bufs)
            sc.op("dve", lambda e: e.tensor_copy(self.xt[:, 2, :], p3.ap), reads=p3.bufs, writes=[self.b_xt[2]])
            return
        if self.stage == 2.15:
            for i in range(3):
                wt, wb = self.wload(self.d_win[l, 2 + i])
                sc.op("act", lambda e, wt=wt, i=i: e.activation(self.xt[:, i, :], wt[:, 0:256], AF.Identity), reads=[wb], writes=[self.b_xt[i]])
                sc.op("act", lambda e, wt=wt, i=i: e.activation(self.xt[:, 3 + i, :], wt[:, 768:1024], AF.Identity), reads=[wb], writes=[self.b_xt[3 + i]])
            return
        sga = []
        for c in range(2):
            p = zchunk(4 + c)
            t, b = self.stt()
            sc.op("act", lambda e, t=t, p=p: e.activation(t[:], p.ap, AF.Silu), reads=p.bufs, writes=[b])
            sga.append((t, b))
        for c in range(2):
            p = zchunk(0 + c)
            sc.op("dve", lambda e, c=c, p=p: e.tensor_tensor(self.ug[:, c, :], p.ap, sga[c][0][:], ALU.mult),
                  reads=p.bufs + [sga[c][1]], writes=[self.b_ug[c]])
        for c in range(2):
            p = zchunk(2 + c)
            sc.op("act", lambda e, c=c, p=p: e.activation(self.va[:, c, :], p.ap, AF.Identity), reads=p.bufs, writes=[self.b_va[c]])
        if self.stage < 2.2:
            if self.stage == 2.19:
                srcs = [(self.va[:, 0, :], self.b_va[0]), (self.va[:, 1, :], self.b_va[1]), (self.ug[:, 0, :], self.b_ug[0]), (self.ug[:, 1, :], self.b_ug[1])]
                for c in range(4):
                    sc.op("act", lambda e, c=c: e.activation(self.xt[:, c, :], srcs[c][0], AF.Identity), reads=[srcs[c][1]], writes=[self.b_xt[c]])
            return
        meanA, bmA, rsA, brsA = self.ln_stats([self.va[:, c, :] for c in range(2)], self.b_va, C_ONES, AW, LN_EPS)
        if self.stage < 2.4:
            if self.stage == 2.3:
                srcs = [(self.va[:, 0, :], self.b_va[0]), (self.va[:, 1, :], self.b_va[1]), (self.ug[:, 0, :], self.b_ug[0]), (self.ug[:, 1, :], self.b_ug[1])]
                for c in range(4):
                    sc.op("act", lambda e, c=c: e.activation(self.xt[:, c, :], srcs[c][0], AF.Identity), reads=[srcs[c][1]], writes=[self.b_xt[c]], force=True)
            return
        for c in range(2):
            t, b = self.stt()
            sc.op("dve", lambda e, c=c, t=t: e.tensor_tensor(t[:], self.va[:, c, :], meanA[:], ALU.subtract), reads=[self.b_va[c], bmA], writes=[b])
            sc.op("dve", lambda e, t=t: e.tensor_tensor(t[:], t[:], rsA[:], ALU.mult), reads=[b, brsA], writes=[b])
            sc.op("act", lambda e, c=c, t=t: e.activation(self.vn[:, c, :], t[:], AF.Identity, bias=self.vcol(l, V_SLB + c), scale=self.vcol(l, V_SLG + c)),
                  reads=[b, self.b_vecs], writes=[self.b_vn[c]])
        if self.stage < 2.6:
            return
        for blk in range(self.NBLK):
            for c in range(2):
                p = self.ps(128)
                sc.op("pe", lambda e, p=p, c=c, blk=blk: e.transpose(p.ap, self.vn[:, c, blk * 128:(blk + 1) * 128], ident),
                      reads=[self.b_vn[c], self.b_consts], writes=p.bufs)
                for hh in range(2):
                    sc.op("act", lambda e, p=p, c=c, blk=blk, hh=hh: e.activation(self.vntok[hh][:, blk, c * 128 + hh * 64:c * 128 + hh * 64 + 64], p.ap[:, hh * 64:hh * 64 + 64], AF.Identity),
                          reads=p.bufs, writes=[self.b_vntok[blk]])
        if self.stage < 2.8:
            return
        for blk in range(self.NBLK):
            for c in range(2):
                p = self.ps(128)
                for hh in range(2):
                    h = 2 * c + hh
                    sc.op("pe", lambda e, p=p, c=c, hh=hh, h=h, blk=blk: e.matmul(
                        p.ap, self.vntok[hh][:, blk, c * 128:(c + 1) * 128],
                        self.lp[:, h * 128:(h + 1) * 128], start=(hh == 0), stop=(hh == 1)),
                        reads=[self.b_vntok[blk], self.b_lp], writes=p.bufs)
                t, b = self.stt()
                sc.op("dve", lambda e, p=p, c=c, t=t: e.tensor_tensor(t[:, 0:128], p.ap, self.lp[:, 512 + c * 128:512 + (c + 1) * 128], ALU.add),
                      reads=p.bufs + [self.b_lp], writes=[b])
                sc.op("dve", lambda e, c=c, t=t, blk=blk: e.tensor_tensor(self.mixT[:, c, blk * 128:(blk + 1) * 128], t[:, 0:128],
                                                                           self.ug[:, c, blk * 128:(blk + 1) * 128], ALU.mult),
                      reads=[b, self.b_ug[c]], writes=[self.b_mix[c]])

        if self.stage < 3:
            return
        sgl = []
        for c in range(2):
            p = zchunk(8 + c)
            t, b = self.stt()
            sc.op("act", lambda e, t=t, p=p: e.activation(t[:], p.ap, AF.Sigmoid), reads=p.bufs, writes=[b])
            sgl.append((t, b))
        for c in range(2):
            hoff = (l * 2 + c) * HALO
            sc.op("pool", lambda e, c=c, hoff=hoff: e.tensor_copy(self.ybuf[:, c, 0:HALO], self.halo[:, hoff:hoff + HALO]),
                  reads=[self.b_halo], writes=[self.b_ybuf[c]])
            p = zchunk(6 + c)
            sc.op("dve", lambda e, c=c, p=p: e.tensor_tensor(self.ybuf[:, c, HALO:HALO + T], p.ap, sgl[c][0][:], ALU.mult),
                  reads=p.bufs + [sgl[c][1]], writes=[self.b_ybuf[c]])
            sc.op("pool", lambda e, c=c, hoff=hoff: e.tensor_copy(self.halo[:, hoff:hoff + HALO], self.ybuf[:, c, T:T + HALO]),
                  reads=[self.b_ybuf[c]], writes=[self.b_halo])
        for c in range(2):
            p = zchunk(10 + c)
            sc.op("act", lambda e, c=c, p=p: e.activation(self.sgb[:, c, :], p.ap, AF.Silu), reads=p.bufs, writes=[self.b_sgb[c]])
        for c in range(2):
            p = self.ps(T)
            for tap in range(CONVW):
                di = self.dgptr
                self.dgptr = (di + 1) % self.NDG
                dg, dgb = self.diag[di], self.b_diag[di]
                sc.op("pool", lambda e, dg=dg, c=c, tap=tap: e.tensor_scalar(dg[:], ident, self.vcol(l, V_CW + c * CONVW + tap), None, ALU.mult),
                      reads=[self.b_consts, self.b_vecs], writes=[dgb])
                sc.op("pe", lambda e, dg=dg, c=c, tap=tap, p=p: e.matmul(p.ap, dg[:], self.ybuf[:, c, tap:tap + T], start=(tap == 0), stop=(tap == CONVW - 1)),
                      reads=[dgb, self.b_ybuf[c]], writes=p.bufs)
            sc.op("act", lambda e, c=c, p=p: e.activation(self.yc[:, c, :], p.ap, AF.Identity, bias=self.vcol(l, V_CB + c)),
                  reads=p.bufs + [self.b_vecs], writes=[self.b_yc[c]])
        meanB, bmB, rsB, brsB = self.ln_stats([self.yc[:, c, :] for c in range(2)], self.b_yc, C_ONES, BW, LN_EPS)
        for c in range(2):
            t, b = self.stt()
            sc.op("dve", lambda e, c=c, t=t: e.tensor_tensor(t[:], self.yc[:, c, :], meanB[:], ALU.subtract), reads=[self.b_yc[c], bmB], writes=[b])
            sc.op("dve", lambda e, t=t: e.tensor_tensor(t[:], t[:], rsB[:], ALU.mult), reads=[b, brsB], writes=[b])
            sc.op("act", lambda e, c=c, t=t: e.activation(self.yn[:, c, :], t[:], AF.Silu, bias=self.vcol(l, V_CLB + c), scale=self.vcol(l, V_CLG + c)),
                  reads=[b, self.b_vecs], writes=[self.b_yn[c]])
        for co in range(2):
            p = self.ps(T)
            for ci in range(2):
                sc.op("pe", lambda e, p=p, ci=ci, co=co: e.matmul(p.ap, self.lp[:, 768 + ci * 256 + co * 128:768 + ci * 256 + (co + 1) * 128], self.yn[:, ci, :],
                                                                 start=(ci == 0), stop=(ci == 1)),
                      reads=[self.b_lp, self.b_yn[ci]], writes=p.bufs)
            sc.op("dve", lambda e, p=p, co=co: e.scalar_tensor_tensor(self.mixT[:, 2 + co, :], p.ap, self.vcol(l, V_PWB + co), self.sgb[:, co, :], ALU.add, ALU.mult),
                  reads=p.bufs + [self.b_sgb[co], self.b_vecs], writes=[self.b_mix[2 + co]])

        if self.stage < 4:
            return
        for c in range(4):
            p = zchunk(25 + c)
            sc.op("act", lambda e, c=c, p=p: e.activation(self.sgc[:, c, :], p.ap, AF.Silu), reads=p.bufs, writes=[self.b_sgc[c]])
        for c in range(13):
            p = zchunk(12 + c)
            zl = self.zlast[:, l * 13 + c:l * 13 + c + 1]
            sc.op("act", lambda e, c=c, p=p: e.activation(self.zs[:, c, :], p.ap, AF.Identity, scale=self.dcol(l, DV_OMM + c)),
                  reads=p.bufs + [self.b_dv], writes=[self.b_zs[c]])
            sc.op("dve", lambda e, c=c, p=p: e.scalar_tensor_tensor(self.zs[:, c, 1:T], p.ap[:, 0:T - 1], self.vcol(l, V_MU + c), self.zs[:, c, 1:T], ALU.mult, ALU.add),
                  reads=p.bufs + [self.b_zs[c], self.b_vecs], writes=[self.b_zs[c]])
            sc.op("dve", lambda e, c=c, zl=zl: e.scalar_tensor_tensor(self.zs[:, c, 0:1], zl, self.vcol(l, V_MU + c), self.zs[:, c, 0:1], ALU.mult, ALU.add),
                  reads=[self.b_zlast, self.b_zs[c], self.b_vecs], writes=[self.b_zs[c]])
            sc.op("act", lambda e, p=p, zl=zl: e.activation(zl, p.ap[:, T - 1:T], AF.Identity), reads=p.bufs, writes=[self.b_zlast])
        sc.op("act", lambda e: e.activation(self.zs[0:64, 12, :], self.zs[0:64, 12, :], AF.Tanh), reads=[self.b_zs[12]], writes=[self.b_zs[12]])
        if self.stage < 5 and self.stage != 4.5:
            return
        for pr in range(4):
            self.rwkv_pair(l, pr)
        for j in range(NCH):
            gens = [self.scan_unit(l, pr, j) for pr in range(4)]
            while gens:
                for g in list(gens):
                    try:
                        next(g)
                    except StopIteration:
                        gens.remove(g)
        for pr in range(4):
            self.rwkv_post(l, pr)
        if self.stage < 7:
            if self.stage == 4.5:
                srcs = [(self.va[:, 0, :], self.b_va[0]), (self.va[:, 1, :], self.b_va[1]), (self.ug[:, 0, :], self.b_ug[0]), (self.ug[:, 1, :], self.b_ug[1]),
                        (self.sgb[:, 0, :], self.b_sgb[0]), (self.yc[:, 0, :], self.b_yc[0]), (self.zs[:, 0, :], self.b_zs[0]), (self.sgc[:, 0, :], self.b_sgc[0])]
                for c in range(8):
                    sc.op("act", lambda e, c=c: e.activation(self.xt[:, c, :], srcs[c][0], AF.Identity), reads=[srcs[c][1]], writes=[self.b_xt[c]], force=True)
            if self.stage == 6.5:
                for c in range(8):
                    sc.op("act", lambda e, c=c: e.activation(self.xt[:, c, :], self.mixT[:, c, :], AF.Identity), reads=[self.b_mix[c]], writes=[self.b_xt[c]])
            return

        for oc in range(8):
            wt, wb = self.wload(self.d_wout[l, oc])
            p = self.ps(T)
            for kc in range(8):
                sc.op("pe", lambda e, kc=kc, wt=wt, p=p: e.matmul(p.ap, wt[:, kc * 128:(kc + 1) * 128], self.mixT[:, kc, :], start=(kc == 0), stop=(kc == 7)),
                      reads=[wb, self.b_mix[kc]], writes=p.bufs)
            sc.op("act", lambda e, oc=oc, p=p: e.activation(self.mo[:, oc, :], p.ap, AF.Identity), reads=p.bufs, writes=[self.b_mo[oc]])
        sqs1 = []
        for c in range(8):
            t, b = self.stt()
            sc.op("act", lambda e, t=t, c=c: e.activation(t[:], self.mo[:, c, :], AF.Square), reads=[self.b_mo[c]], writes=[b])
            sqs1.append((t, b))
        pss1 = self.ps(T)
        for c in range(8):
            sc.op("pe", lambda e, c=c: e.matmul(pss1.ap, ones, sqs1[c][0][:], start=(c == 0), stop=(c == 7)),
                  reads=[sqs1[c][1], self.b_consts], writes=pss1.bufs)
        rs1, brs1 = self.rstd_from(pss1.ap, pss1.bufs, 1.0 / D, RMS_EPS)
        for c in range(8):
            sc.op("dve", lambda e, c=c: e.scalar_tensor_tensor(self.mo[:, c, :], self.mo[:, c, :], self.vcol(l, V_POSTG + c), rs1[:], ALU.mult, ALU.mult),
                  reads=[self.b_mo[c], brs1, self.b_vecs], writes=[self.b_mo[c]])
            sc.op("dve", lambda e, c=c: e.tensor_tensor(self.xt[:, c, :], self.xt[:, c, :], self.mo[:, c, :], ALU.add),
                  reads=[self.b_xt[c], self.b_mo[c]], writes=[self.b_xt[c]])
        if wdt != F32:
            for c in range(8):
                sc.op("act", lambda e, c=c: e.activation(self.hT[:, c, :], self.xt[:, c, :], AF.Identity), reads=[self.b_xt[c]], writes=[self.b_hT[c]])
            xsrc, xb = self.hT, self.b_hT
        else:
            xsrc, xb = self.xt, self.b_xt
        for oc in range(8):
            wt, wb = self.wload(self.d_gw[l, oc])
            p = self.ps(T)
            for kc in range(8):
                sc.op("pe", lambda e, kc=kc, wt=wt, p=p: e.matmul(p.ap, wt[:, kc * 128:(kc + 1) * 128], xsrc[:, kc, :], start=(kc == 0), stop=(kc == 7)),
                      reads=[wb, xb[kc]], writes=p.bufs)
            sc.op("act", lambda e, oc=oc, p=p: e.activation(self.mo[:, oc, :], p.ap, AF.Sigmoid, bias=self.vcol(l, V_GATEB + oc)),
                  reads=p.bufs + [self.b_vecs], writes=[self.b_mo[oc]])
        for oc in range(8):
            wt, wb = self.wload(self.d_plew[l, oc], 256)
            p = self.ps(T)
            for kc in range(2):
                sc.op("pe", lambda e, kc=kc, wt=wt, p=p: e.matmul(p.ap, wt[:, kc * 128:(kc + 1) * 128], self.pt[:, kc, :], start=(kc == 0), stop=(kc == 1)),
                      reads=[wb, self.b_pt], writes=p.bufs)
            sc.op("dve", lambda e, oc=oc, p=p: e.tensor_tensor(self.mo[:, oc, :], p.ap, self.mo[:, oc, :], ALU.mult),
                  reads=p.bufs + [self.b_mo[oc]], writes=[self.b_mo[oc]])
            sc.op("dve", lambda e, oc=oc: e.tensor_tensor(self.xt[:, oc, :], self.xt[:, oc, :], self.mo[:, oc, :], ALU.add),
                  reads=[self.b_xt[oc], self.b_mo[oc]], writes=[self.b_xt[oc]])

    def rwkv_pair(self, l, pr):
        sc, T, NCH = self.sc, self.T, self.NCH
        ident = self.cst(C_ID, 128)
        blk = self.cst(C_BLK, 128)
        P = dict(self.pt_sh); P.update(self.pt_pp[pr])
        Bf = dict(self.b_pt_sh); Bf.update(self.b_pt_pp[pr])
        pads, b_pads = self.pads4[pr], self.b_pads4[pr]
        Rt, b_Rt = self.Rt4[pr], self.b_Rt4[pr]
        r_ap, r_b = self.zs[:, 0 + pr, :], self.b_zs[0 + pr]
        k_ap, k_b = self.zs[:, 4 + pr, :], self.b_zs[4 + pr]
        v_ap, v_b = self.zs[:, 8 + pr, :], self.b_zs[8 + pr]
        lora_w = self.lp[:, 1280 + pr * 128:1280 + (pr + 1) * 128]
        lora_a = self.lp[:, 1792 + pr * 128:1792 + (pr + 1) * 128]
        p = self.ps(T)
        sc.op("pe", lambda e: e.matmul(p.ap, lora_w, self.zs[:, 12, :], start=True, stop=True), reads=[self.b_lp, self.b_zs[12]], writes=p.bufs)
        sc.op("act", lambda e: e.activation(P["lw"][:], p.ap, AF.Sigmoid, bias=self.vcol(l, V_W0 + pr)), reads=p.bufs + [self.b_vecs], writes=[Bf["lw"]])
        p2 = self.ps(T)
        sc.op("pe", lambda e: e.matmul(p2.ap, lora_a, self.zs[:, 12, :], start=True, stop=True), reads=[self.b_lp, self.b_zs[12]], writes=p2.bufs)
        sc.op("act", lambda e: e.activation(P["aa"][:], p2.ap, AF.Sigmoid, bias=self.vcol(l, V_A0 + pr)), reads=p2.bufs + [self.b_vecs], writes=[Bf["aa"]])
        sc.op("dve", lambda e: e.tensor_scalar(P["lw"][:], P["lw"][:], -DECAY, None, ALU.mult), reads=[Bf["lw"]], writes=[Bf["lw"]])
        sc.op("dve", lambda e: e.tensor_tensor_scan(P["cum"][:], self.cst(C_RESET, T), P["lw"][:], 0.0, ALU.mult, ALU.add),
              reads=[Bf["lw"], self.b_consts], writes=[Bf["cum"]])
        sc.op("dve", lambda e: e.tensor_tensor(P["cume"][:], P["cum"][:], P["lw"][:], ALU.subtract), reads=[Bf["cum"], Bf["lw"]], writes=[Bf["cume"]])
        sc.op("act", lambda e: e.activation(P["E1"][:], P["cum"][:], AF.Exp), reads=[Bf["cum"]], writes=[Bf["E1"]])
        sc.op("act", lambda e: e.activation(P["E2"][:], P["cum"][:], AF.Exp, scale=-1.0), reads=[Bf["cum"]], writes=[Bf["E2"]])
        sc.op("act", lambda e: e.activation(P["E3"][:], P["cume"][:], AF.Exp), reads=[Bf["cume"]], writes=[Bf["E3"]])
        sc.op("dve", lambda e: e.tensor_scalar(P["kk0"][:], k_ap, self.vcol(l, V_KK + pr), None, ALU.mult), reads=[k_b, self.b_vecs], writes=[Bf["kk0"]])
        t, b = self.stt()
        sc.op("act", lambda e: e.activation(t[:], P["kk0"][:], AF.Square), reads=[Bf["kk0"]], writes=[b])
        p3 = self.ps(T)
        sc.op("pe", lambda e: e.matmul(p3.ap, blk, t[:], start=True, stop=True), reads=[b, self.b_consts], writes=p3.bufs)
        rn, brn = self.rstd_from(p3.ap, p3.bufs, 1.0, 0.0, clamp=1e-12)
        sc.op("dve", lambda e: e.tensor_tensor(P["kkn"][:], P["kk0"][:], rn[:], ALU.mult), reads=[Bf["kk0"], brn], writes=[Bf["kkn"]])
        t2, b2 = self.stt()
        sc.op("dve", lambda e: e.tensor_scalar(t2[:], P["aa"][:], self.vcol(l, V_KA + pr), self.dcol(l, DV_OMKA + pr), ALU.mult, ALU.add),
              reads=[Bf["aa"], self.b_vecs, self.b_dv], writes=[b2])
        sc.op("dve", lambda e: e.tensor_tensor(P["kmod"][:], k_ap, t2[:], ALU.mult), reads=[k_b, b2], writes=[Bf["kmod"]])
        sc.op("dve", lambda e: e.tensor_tensor(P["bvec"][:], P["kkn"][:], P["aa"][:], ALU.mult), reads=[Bf["kkn"], Bf["aa"]], writes=[Bf["bvec"]])
        def v3(ap):
            return ap.rearrange("p (j t) -> p j t", t=CH)

        for hh in range(2):
            rows = slice(hh * 64, hh * 64 + 64)
            def pv(n, hh=hh, rows=rows):
                return pads[n][rows, :].rearrange("p (j h t) -> p j h t", h=2, t=CH)[:, :, hh, :]
            sc.op("dve", lambda e, rows=rows, pv=pv: e.scalar_tensor_tensor(pv("A"), v3(P["kkn"][rows, :]), -1.0, v3(P["E3"][rows, :]), ALU.mult, ALU.mult),
                  reads=[Bf["kkn"], Bf["E3"]], writes=b_pads["A"])
            sc.op("dve", lambda e, rows=rows, pv=pv: e.tensor_tensor(pv("B"), v3(P["bvec"][rows, :]), v3(P["E2"][rows, :]), ALU.mult),
                  reads=[Bf["bvec"], Bf["E2"]], writes=b_pads["B"])
            sc.op("dve", lambda e, rows=rows, pv=pv: e.tensor_tensor(pv("K"), v3(P["kmod"][rows, :]), v3(P["E2"][rows, :]), ALU.mult),
                  reads=[Bf["kmod"], Bf["E2"]], writes=b_pads["K"])
            sc.op("act", lambda e, rows=rows, pv=pv: e.activation(pv("V"), v3(self.zs[rows, 8 + pr, :]), AF.Identity),
                  reads=[v_b], writes=b_pads["V"])
        sc.op("dve", lambda e: e.tensor_tensor(Rt[:], r_ap, P["E1"][:], ALU.mult), reads=[r_b, Bf["E1"]], writes=[b_Rt])

    def rwkv_post(self, l, pr):
        sc, T, NCH = self.sc, self.T, self.NCH
        blk = self.cst(C_BLK, 128)
        P = dict(self.pt_sh); P.update(self.pt_pp[pr])
        Bf = dict(self.b_pt_sh); Bf.update(self.b_pt_pp[pr])
        r_ap, r_b = self.zs[:, 0 + pr, :], self.b_zs[0 + pr]
        v_ap, v_b = self.zs[:, 8 + pr, :], self.b_zs[8 + pr]
        yy, byy = P["yy"], Bf["yy"]
        meanC, bmC, rsC, brsC = self.ln_stats([yy[:]], [byy], C_BLK, CH, GN_EPS)
        tpo, bpo = self.stt()
        sc.op("dve", lambda e: e.tensor_tensor(tpo[:], yy[:], meanC[:], ALU.subtract), reads=[byy, bmC], writes=[bpo])
        sc.op("dve", lambda e: e.tensor_tensor(tpo[:], tpo[:], rsC[:], ALU.mult), reads=[bpo, brsC], writes=[bpo])
        sc.op("act", lambda e: e.activation(tpo[:], tpo[:], AF.Identity, bias=self.vcol(l, V_LXB + pr), scale=self.vcol(l, V_LXG + pr)),
              reads=[bpo, self.b_vecs], writes=[bpo])
        t3, b3 = self.stt()
        sc.op("dve", lambda e: e.scalar_tensor_tensor(t3[:], r_ap, self.vcol(l, V_RK + pr), P["kmod"][:], ALU.mult, ALU.mult),
              reads=[r_b, Bf["kmod"], self.b_vecs], writes=[b3])
        p4 = self.ps(T)
        sc.op("pe", lambda e: e.matmul(p4.ap, blk, t3[:], start=True, stop=True), reads=[b3, self.b_consts], writes=p4.bufs)
        sc.op("dve", lambda e: e.tensor_tensor(t3[:], p4.ap, v_ap, ALU.mult), reads=p4.bufs + [v_b], writes=[b3])
        sc.op("dve", lambda e: e.tensor_tensor(tpo[:], tpo[:], t3[:], ALU.add), reads=[bpo, b3], writes=[bpo])
        sc.op("dve", lambda e: e.tensor_tensor(self.mixT[:, 4 + pr, :], tpo[:], self.sgc[:, pr, :], ALU.mult),
              reads=[bpo, self.b_sgc[pr]], writes=[self.b_mix[4 + pr]])

    def scan_unit(self, l, pr, j):
        sc, T = self.sc, self.T
        ident = self.cst(C_ID, 128)
        msu = self.cst(C_MSU, 128)
        msl = self.cst(C_MSL, 128)
        mui = self.cst(C_MUI, 64)
        U = self.ut[pr]
        cs = slice(j * 128, (j + 1) * 128)
        Ap, Bp, Kp, Vp = (self.pads4[pr][n][:, cs] for n in ("A", "B", "K", "V"))
        bA, bB, bK, bV = (self.b_pads4[pr][n][j] for n in ("A", "B", "K", "V"))
        Rst = self.Rt4[pr][:, j * CH:(j + 1) * CH]
        b_Rt = self.b_Rt4[pr]
        CB = self.b_consts

        def mm(out_ps, lhsT, rhs, reads, start=True, stop=True):
            sc.op("pe", lambda e: e.matmul(out_ps.ap, lhsT, rhs, start=start, stop=stop), reads=reads, writes=out_ps.bufs)

        def ev_mask(dst, p, mask):
            sc.op("dve", lambda e: e.tensor_tensor(dst[0][:], p.ap, mask, ALU.mult), reads=p.bufs + [CB], writes=[dst[1]])

        def ev_copy(dst, p, dst_ap=None):
            d = dst[0][:] if dst_ap is None else dst_ap
            sc.op("act", lambda e: e.activation(d, p.ap, AF.Identity), reads=p.bufs, writes=[dst[1]])

        p = self.ps(128); mm(p, Bp, Ap, [bB, bA]); ev_mask(U["B0"], p, msu)
        p = self.ps(128); mm(p, Ap, Bp, [bA, bB]); ev_mask(U["A0"], p, msl)
        p = self.ps(128); mm(p, Kp, Ap, [bK, bA]); ev_mask(U["MakT"], p, msu)
        p = self.ps(64); mm(p, Bp, Rst, [bB, b_Rt]); ev_mask(U["MrbT"], p, mui)
        p = self.ps(64); mm(p, Kp, Rst, [bK, b_Rt]); ev_mask(U["MrkT"], p, mui)
        yield
        def tr(dst, src, bsrc, dst_ap=None):
            p = self.ps(128)
            sc.op("pe", lambda e: e.transpose(p.ap, src, ident), reads=[bsrc, CB], writes=p.bufs)
            ev_copy(dst, p, dst_ap)
        tr(U["Z0"], Ap, bA, U["Z0"][0][:, 0:128])
        tr(U["Btok"], Bp, bB)
        tr(U["Ktok"], Kp, bK)
        tr(U["Vbd"], Vp, bV)
        yield
        p = self.ps(128); mm(p, U["MakT"][0][:], U["Vbd"][0][:], [U["MakT"][1], U["Vbd"][1]])
        ev_copy(U["Z0"], p, U["Z0"][0][:, 128:256])
        Acur, Bcur, Anext, Bnext = U["A0"], U["B0"], U["A1"], U["B1"]
        Zc, Zn = U["Z0"], U["Z1"]
        for n in range(6):
            yield
            p = self.ps(256)
            mm(p, Bcur[0][:], Zc[0][:], [Bcur[1], Zc[1]])
            sc.op("dve", lambda e, p=p, Zc=Zc, Zn=Zn: e.tensor_tensor(Zn[0][:], p.ap, Zc[0][:], ALU.add), reads=p.bufs + [Zc[1]], writes=[Zn[1]])
            Zc, Zn = Zn, Zc
            if n < 5:
                pb = self.ps(128); mm(pb, Acur[0][:], Bcur[0][:], [Acur[1], Bcur[1]])
                if n < 4:
                    pa = self.ps(128); mm(pa, Bcur[0][:], Acur[0][:], [Acur[1], Bcur[1]])
                ev_copy(Bnext, pb)
                if n < 4:
                    ev_copy(Anext, pa)
                Acur, Anext = Anext, Acur
                Bcur, Bnext = Bnext, Bcur
        yield
        Pm, Qm = Zc[0][:, 0:128], Zc[0][:, 128:256]
        bZ = Zc[1]
        p = self.ps(64)
        mm(p, ident, Rst, [CB, b_Rt], start=True, stop=False)
        mm(p, Pm, U["MrbT"][0][:], [bZ, U["MrbT"][1]], start=False, stop=True)
        ev_copy(U["Rp"], p)
        p = self.ps(128)
        mm(p, ident, ident, [CB], start=True, stop=False)
        mm(p, Pm, U["Btok"][0][:], [bZ, U["Btok"][1]], start=False, stop=True)
        ev_copy(U["G0"], p)
        yield
        st_ap = self.state[:, (l * 4 + pr) * 128:(l * 4 + pr + 1) * 128]
        bS = self.b_state[l][pr]
        py = self.ps(64)
        mm(py, Qm, U["MrbT"][0][:], [bZ, U["MrbT"][1]], start=True, stop=False)
        mm(py, U["Vbd"][0][:], U["MrkT"][0][:], [U["Vbd"][1], U["MrkT"][1]], start=False, stop=False)
        mm(py, st_ap, U["Rp"][0][:], [bS, U["Rp"][1]], start=False, stop=True)
        pS = self.ps(128)
        mm(pS, U["Btok"][0][:], Qm, [U["Btok"][1], bZ], start=True, stop=False)
        mm(pS, U["Ktok"][0][:], U["Vbd"][0][:], [U["Ktok"][1], U["Vbd"][1]], start=False, stop=False)
        mm(pS, U["G0"][0][:], st_ap, [U["G0"][1], bS], start=False, stop=True)
        yy, byy = self.pt_pp[pr]["yy"], self.b_pt_pp[pr]["yy"]
        sc.op("act", lambda e: e.activation(yy[:, j * CH:(j + 1) * CH], py.ap, AF.Identity), reads=py.bufs, writes=[byy])
        wc = self.pt_pp[pr]["E1"][:, j * CH + CH - 1:j * CH + CH]
        sc.op("dve", lambda e: e.tensor_scalar(st_ap, pS.ap, wc, None, ALU.mult), reads=pS.bufs + [self.b_pt_pp[pr]["E1"]], writes=[bS])


def make_consts(T):
    NC = C_RESET + T
    c = np.zeros((128, NC), np.float32)
    c[:, C_ID:C_ID + 128] = np.eye(128, dtype=np.float32)
    c[:, C_ONES:C_ONES + 128] = 1.0
    blk = np.zeros((128, 128), np.float32)
    blk[:64, :64] = 1.0
    blk[64:, 64:] = 1.0
    c[:, C_BLK:C_BLK + 128] = blk
    tri_u = np.triu(np.ones((64, 64), np.float32), 1)
    msu = np.zeros((128, 128), np.float32)
    msu[:64, :64] = tri_u
    msu[64:, 64:] = tri_u
    c[:, C_MSU:C_MSU + 128] = msu
    c[:, C_MSL:C_MSL + 128] = msu.T
    ui = np.triu(np.ones((64, 64), np.float32), 0)
    c[:64, C_MUI:C_MUI + 64] = ui
    c[64:, C_MUI:C_MUI + 64] = ui
    c[:64, C_PAD] = 1.0
    c[64:, C_PAD + 1] = 1.0
    c[:64, C_NPAD] = -1.0
    c[64:, C_NPAD + 1] = -1.0
    r = np.ones(T, np.float32)
    r[::CH] = 0.0
    c[:, C_RESET:C_RESET + T] = r[None, :]
    return c


def colmaj(v, n):
    return np.ascontiguousarray(v.reshape(n, 128).T)


def prep_shared(inp, NL, T):
    f = lambda a: np.ascontiguousarray(a, dtype=np.float32)
    sh = {}
    w_in = f(inp["w_in"][:NL])
    sh["win"] = np.ascontiguousarray(w_in.reshape(NL, 8, 128, NOC, 128).transpose(0, 3, 2, 1, 4)).reshape(NL, NOC, 128, 1024)
    sh["wout"] = np.ascontiguousarray(f(inp["w_out"][:NL]).reshape(NL, 8, 128, 8, 128).transpose(0, 3, 2, 1, 4)).reshape(NL, 8, 128, 1024)
    sh["gw"] = np.ascontiguousarray(f(inp["ple_gate_w"][:NL]).reshape(NL, 8, 128, 8, 128).transpose(0, 3, 2, 1, 4)).reshape(NL, 8, 128, 1024)
    sh["plew"] = np.ascontiguousarray(f(inp["ple_w"][:NL]).reshape(NL, 2, 128, 8, 128).transpose(0, 3, 2, 1, 4)).reshape(NL, 8, 128, 256)
    vecs = np.zeros((128, NL * NV), np.float32)
    for l in range(NL):
        o = l * NV
        def put(col, v, n):
            vecs[:, o + col:o + col + n] = colmaj(f(v), n)
        put(V_PREG, inp["pre_norm_g"][l], 8)
        put(V_POSTG, inp["post_norm_g"][l], 8)
        put(V_GATEB, inp["ple_gate_b"][l], 8)
        put(V_SLG, inp["sgu_ln_g"][l], 2)
        put(V_SLB, inp["sgu_ln_b"][l], 2)
        put(V_CB, inp["conv_b"][l], 2)
        put(V_CLG, inp["conv_ln_g"][l], 2)
        put(V_CLB, inp["conv_ln_b"][l], 2)
        put(V_PWB, inp["pw_b"][l], 2)
        put(V_MU, inp["shift_mu"][l], 13)
        put(V_W0, inp["w0"][l], 4)
        put(V_A0, inp["a0"][l], 4)
        put(V_KK, inp["k_k"][l], 4)
        put(V_KA, inp["k_a"][l], 4)
        put(V_RK, inp["r_k"][l].reshape(-1), 4)
        put(V_LXG, inp["lnx_g"][l], 4)
        put(V_LXB, inp["lnx_b"][l], 4)
        cw = f(inp["conv_w"][l])
        for c in range(2):
            vecs[:, o + V_CW + c * CONVW:o + V_CW + (c + 1) * CONVW] = cw[:, c * 128:(c + 1) * 128].T
    sh["vecs"] = vecs
    sh["sguT"] = np.ascontiguousarray(f(inp["sgu_w"][:NL]).transpose(0, 3, 1, 2)).reshape(NL, 128, 512)
    sb = f(inp["sgu_b"][:NL])
    sgub = np.zeros((NL, 128, 256), np.float32)
    for c in range(2):
        for hh in range(2):
            sgub[:, hh * 64:(hh + 1) * 64, c * 128:(c + 1) * 128] = sb[:, 2 * c + hh][:, None, :]
    sh["sgub"] = sgub
    sh["pww"] = np.ascontiguousarray(f(inp["pw_w"][:NL]).reshape(NL, 2, 128, 256).transpose(0, 2, 1, 3)).reshape(NL, 128, 512)
    lora = np.zeros((NL, 128, 1024), np.float32)
    lora[:, 0:64, 0:512] = f(inp["w_up"][:NL])
    lora[:, 64:128, 512:1024] = f(inp["a_up"][:NL])
    sh["lora"] = lora
    sh["consts"] = make_consts(T)
    return sh


_CACHE = {}


def run(inp, NL, NT, T, ncore, **kw):
    key = (NL, NT, T, tuple(sorted(kw.items())))
    S = NT * T
    prog = Prog(NL, NT, T, **kw)
    nc = prog.build()
    sh = prep_shared(inp, NL, T)
    in_maps = []
    for b in range(ncore):
        m = dict(sh)
        m["xT"] = np.ascontiguousarray(np.asarray(inp["x"][b, :S], np.float32).T)
        m["pT"] = np.ascontiguousarray(np.asarray(inp["p"][:NL, b, :S], np.float32).transpose(0, 2, 1))
        in_maps.append(m)
    res = run_bass_kernel_spmd(nc, in_maps, core_ids=list(range(ncore)))
    out = np.stack([np.ascontiguousarray(r["yT"].T) for r in res.results], axis=0)
    return out.astype(np.float32), prog


def kernel(**inputs):
    out, _ = run(inputs, NLAYER, SEQ // 256, 256, NCORE, wdt=BF16, nslot=4)
    return out
```

```python
import math
from contextlib import ExitStack

import numpy as np
import concourse.bass as bass
import concourse.mybir as mybir
from concourse.bass_utils import run_bass_kernel_spmd

F32 = mybir.dt.float32
BF16 = mybir.dt.bfloat16
AF = mybir.ActivationFunctionType
ALU = mybir.AluOpType

D = 1024
SEQ = 4096
NLAYER = 4
NCORE = 8
AW = 256
BW = 256
CW = 512
PLE = 256
CONVW = 31
HALO = CONVW - 1
INC = 3712
NOC = INC // 128
CH = 64
RMS_EPS = 1e-6
LN_EPS = 1e-5
GN_EPS = 64e-5
DECAY = math.exp(-0.5)

V_PREG, V_POSTG, V_GATEB = 0, 8, 16
V_SLG, V_SLB = 24, 26
V_CB, V_CLG, V_CLB, V_PWB = 28, 30, 32, 34
V_MU = 36
V_W0, V_A0, V_KK, V_KA, V_RK, V_LXG, V_LXB = 49, 53, 57, 61, 65, 69, 73
V_CW = 77
NV = V_CW + 2 * CONVW
DV_OMM, DV_OMKA = 0, 13
NDV = 17
C_ID, C_ONES, C_BLK, C_MSU, C_MSL, C_MUI, C_PAD, C_NPAD, C_RESET = 0, 128, 256, 384, 512, 640, 704, 706, 708


class Sem:
    def __init__(self, h):
        self.h = h
        self.cnt = 0


class Buf:
    __slots__ = ("name", "w", "r", "excl")

    def __init__(self, name, excl=False):
        self.name = name
        self.w = None
        self.r = {}
        self.excl = excl


class Eng:
    def __init__(self, name, sem):
        self.name = name
        self.sem = sem
        self.seen = {}
        self.items = []


class Sched:
    def __init__(self, nc, es):
        self.nc = nc
        self.es = es
        self.eng = {}
        for n in ("pe", "act", "dve", "pool", "sp"):
            self.eng[n] = Eng(n, self.new_sem("s_" + n))
        self.nops = 0

    def new_sem(self, name):
        return Sem(self.es.enter_context(self.nc.semaphore(name)))

    def _need(self, E, ev):
        sem, val = ev
        if E.seen.get(sem, 0) >= val:
            return
        E.items.append(("w", sem, val))
        E.seen[sem] = val

    def op(self, en, fn, reads=(), writes=(), dsem=None, force=False):
        import os
        lim = int(os.environ.get("KLIMIT", "0"))
        if lim and self.nops >= lim and not force:
            return None
        E = self.eng[en]
        if any(b.excl for b in reads):
            writes = list(writes) + [b for b in reads if b.excl]
            reads = [b for b in reads if not b.excl]
        for b in reads:
            if b.w is not None:
                if b.w[0] is E.sem and en == "pe":
                    continue
                self._need(E, b.w)
        for b in writes:
            if b.w is not None and b.w[0] is not E.sem:
                self._need(E, b.w)
            for sem, val in b.r.items():
                if sem is not E.sem:
                    self._need(E, (sem, val))
        if dsem is None:
            sem = E.sem
            sem.cnt += 1
            inc = 1
        else:
            sem = dsem
            sem.cnt += 16
            inc = 16
        ev = (sem, sem.cnt)
        E.items.append(("o", fn, sem, inc))
        for b in reads:
            b.r[sem] = sem.cnt
        for b in writes:
            b.w = ev
            b.r = {}
        self.nops += 1
        return ev

    def wait(self, en, ev):
        self._need(self.eng[en], ev)

    def replay(self, en, e):
        for it in self.eng[en].items:
            if it[0] == "w":
                e.wait_ge(it[1].h, it[2])
            else:
                it[1](e).then_inc(it[2].h, it[3])


class PsTile:
    def __init__(self, ap, bufs):
        self.ap = ap
        self.bufs = bufs


class Prog:
    def __init__(self, NL, NT, T, wdt=F32, sdt=F32, nslot=2, ugroup=4, stage=99):
        self.stage = stage
        self.NL, self.NT, self.T = NL, NT, T
        self.S = NT * T
        self.NCH = T // CH
        self.NBLK = T // 128
        self.wdt, self.sdt = wdt, sdt
        self.nslot = nslot
        self.ugroup = ugroup
        self.NC = C_RESET + T
        self.nc = bass.Bass("TRN2", target_bir_lowering=False)
        self.es = ExitStack()

    def dram(self, name, shape, kind="ExternalInput", dt=F32):
        return self.nc.dram_tensor(name, list(shape), dt, kind=kind).ap()

    def sb(self, name, shape, dt=F32):
        t = self.es.enter_context(self.nc.sbuf_tensor("sb_" + name, list(shape), dt))
        return t

    def build(self):
        nc, NL, T, S = self.nc, self.NL, self.T, self.S
        es = self.es
        with es:
            self.sc = Sched(nc, es)
            self.d_x = self.dram("xT", [D, S])
            self.d_p = self.dram("pT", [NL, PLE, S])
            self.d_win = self.dram("win", [NL, NOC, 128, 1024])
            self.d_wout = self.dram("wout", [NL, 8, 128, 1024])
            self.d_gw = self.dram("gw", [NL, 8, 128, 1024])
            self.d_plew = self.dram("plew", [NL, 8, 128, 256])
            self.d_vecs = self.dram("vecs", [128, NL * NV])
            self.d_sguT = self.dram("sguT", [NL, 128, 512])
            self.d_sgub = self.dram("sgub", [NL, 128, 256])
            self.d_pww = self.dram("pww", [NL, 128, 512])
            self.d_lora = self.dram("lora", [NL, 128, 1024])
            self.d_consts = self.dram("consts", [128, self.NC])
            self.d_y = self.dram("yT", [D, S], kind="ExternalOutput")
            self.alloc()
            self.emit()
            with nc.Block() as block:
                @block.tensor
                def _(e):
                    self.sc.replay("pe", e)

                @block.scalar
                def _(e):
                    self.sc.replay("act", e)

                @block.vector
                def _(e):
                    self.sc.replay("dve", e)

                @block.gpsimd
                def _(e):
                    self.sc.replay("pool", e)

                @block.sync
                def _(e):
                    self.sc.replay("sp", e)
        return nc

    def alloc(self):
        NL, T, NCH = self.NL, self.T, self.NCH
        sb = self.sb
        B = Buf
        self.xt = sb("xt", [128, 8, T]); self.b_xt = [B(f"xt{c}") for c in range(8)]
        self.hT = sb("hT", [128, 8, T], self.wdt); self.b_hT = [B(f"hT{c}") for c in range(8)]
        self.zs = sb("zs", [128, 13, T]); self.b_zs = [B(f"zs{c}") for c in range(13)]
        self.va = sb("va", [128, 2, T]); self.b_va = [B(f"va{c}") for c in range(2)]
        self.mixT = sb("mixT", [128, 8, T], self.wdt); self.b_mix = [B(f"mix{c}") for c in range(8)]
        self.mo = sb("mo", [128, 8, T]); self.b_mo = [B(f"mo{c}") for c in range(8)]
        self.wring = [sb(f"wr{i}", [128, 1024], self.wdt) for i in range(self.nslot)]
        self.b_wring = [B(f"wr{i}") for i in range(self.nslot)]
        self.s_wring = [self.sc.new_sem(f"swr{i}") for i in range(self.nslot)]
        self.wptr = 0
        self.NST = 10
        self.st = [sb(f"st{i}", [128, T]) for i in range(self.NST)]
        self.b_st = [B(f"st{i}") for i in range(self.NST)]
        self.stptr = 0
        self.pt = sb("pt", [128, 2, T], self.wdt); self.b_pt = B("pt"); self.s_pt = self.sc.new_sem("spt")
        self.vecs = sb("vecs", [128, NL * NV]); self.b_vecs = B("vecs")
        self.dv = sb("dv", [128, NL * NDV]); self.b_dv = B("dv")
        self.consts = sb("consts", [128, self.NC]); self.b_consts = B("consts")
        self.s_misc = self.sc.new_sem("smisc")
        self.s_x = self.sc.new_sem("sx")
        self.s_y = self.sc.new_sem("sy")
        self.lp = sb("lp", [128, 512 + 256 + 512 + 1024]); self.b_lp = B("lp"); self.s_lp = self.sc.new_sem("slp")
        self.ybuf = sb("ybuf", [128, 2, HALO + T]); self.b_ybuf = [B(f"ybuf{c}") for c in range(2)]
        self.halo = sb("halo", [128, NL * 2 * HALO]); self.b_halo = B("halo")
        self.zlast = sb("zlast", [128, NL * 13]); self.b_zlast = B("zlast")
        self.NDG = 8
        self.diag = [sb(f"dg{i}", [128, 128]) for i in range(self.NDG)]
        self.b_diag = [B(f"dg{i}") for i in range(self.NDG)]
        self.dgptr = 0
        self.state = sb("state", [128, NL * 4 * 128], self.sdt)
        self.b_state = [[B(f"state{l}_{p}") for p in range(4)] for l in range(NL)]
        self.vn = sb("vn", [128, 2, T]); self.b_vn = [B(f"vn{c}") for c in range(2)]
        self.vntok = [sb(f"vntok{hh}", [128, self.NBLK, 256]) for hh in range(2)]; self.b_vntok = [B(f"vntok{b}") for b in range(self.NBLK)]
        self.ug = sb("ug", [128, 2, T]); self.b_ug = [B(f"ug{c}") for c in range(2)]
        self.sgb = sb("sgb", [128, 2, T]); self.b_sgb = [B(f"sgb{c}") for c in range(2)]
        self.yc = sb("yc", [128, 2, T]); self.b_yc = [B(f"yc{c}") for c in range(2)]
        self.yn = sb("yn", [128, 2, T]); self.b_yn = [B(f"yn{c}") for c in range(2)]
        self.sgc = sb("sgc", [128, 4, T]); self.b_sgc = [B(f"sgc{c}") for c in range(4)]
        names = ["lw", "aa", "cum", "cume", "E2", "E3", "kk0", "kkn", "bvec"]
        self.pt_sh = {n: sb("c_" + n, [128, T]) for n in names}
        self.b_pt_sh = {n: B("c_" + n) for n in names}
        self.pt_pp = [{n: sb(f"c{pr}_" + n, [128, T]) for n in ("E1", "kmod", "yy")} for pr in range(4)]
        self.b_pt_pp = [{n: B(f"c{pr}_" + n) for n in ("E1", "kmod", "yy")} for pr in range(4)]
        self.Rt4 = [sb(f"Rt{pr}", [128, T], self.sdt) for pr in range(4)]; self.b_Rt4 = [B(f"Rt{pr}") for pr in range(4)]
        self.pads4 = [{n: sb(f"pad{pr}_" + n, [128, NCH * 128], self.sdt) for n in ("A", "B", "K", "V")} for pr in range(4)]
        self.b_pads4 = [{n: [B(f"pad{pr}_{n}{j}") for j in range(NCH)] for n in ("A", "B", "K", "V")} for pr in range(4)]
        self.NU = 4
        self.ut = []
        for u in range(self.NU):
            d = {}
            for n, w in (("A0", 128), ("A1", 128), ("B0", 128), ("B1", 128), ("MakT", 128), ("MrbT", 64), ("MrkT", 64),
                         ("Z0", 256), ("Z1", 256), ("Btok", 128), ("Ktok", 128), ("Vbd", 128), ("Rp", 64), ("G0", 128)):
                d[n] = (sb(f"u{u}_{n}", [128, w], self.sdt), B(f"u{u}_{n}"))
            self.ut.append(d)
        self.psb = [self.es.enter_context(self.nc.psum_tensor(f"ps{b}", [128, 512], F32)) for b in range(8)]
        self.b_psq = [B(f"psbank{b}", excl=True) for b in range(8)]
        self.psptr = 0

    def ps(self, ncols):
        b = self.psptr
        self.psptr = (b + 1) % 8
        return PsTile(self.psb[b][:, 0:ncols], [self.b_psq[b]])

    def stt(self):
        i = self.stptr
        self.stptr = (i + 1) % self.NST
        return self.st[i], self.b_st[i]

    def cst(self, off, n, rows=slice(0, 128)):
        return self.consts[rows, off:off + n]

    def vcol(self, l, col, rows=slice(0, 128)):
        return self.vecs[rows, l * NV + col:l * NV + col + 1]

    def dcol(self, l, col):
        return self.dv[:, l * NDV + col:l * NDV + col + 1]

    def emit(self):
        sc, NL, NT, T = self.sc, self.NL, self.NT, self.T
        sc.op("sp", lambda e: e.dma_start(out=self.consts[:], in_=self.d_consts), writes=[self.b_consts], dsem=self.s_misc)
        sc.op("sp", lambda e: e.dma_start(out=self.vecs[:], in_=self.d_vecs), writes=[self.b_vecs], dsem=self.s_misc)
        ev = (self.s_misc, self.s_misc.cnt)
        self.b_consts.w = ev
        self.b_vecs.w = ev
        for l in range(NL):
            sc.op("dve", lambda e, l=l: e.tensor_scalar(self.dv[:, l * NDV + DV_OMM:l * NDV + DV_OMM + 13],
                                                       self.vecs[:, l * NV + V_MU:l * NV + V_MU + 13], -1.0, 1.0, ALU.mult, ALU.add),
                  reads=[self.b_vecs], writes=[self.b_dv])
            sc.op("dve", lambda e, l=l: e.tensor_scalar(self.dv[:, l * NDV + DV_OMKA:l * NDV + DV_OMKA + 4],
                                                       self.vecs[:, l * NV + V_KA:l * NV + V_KA + 4], -1.0, 1.0, ALU.mult, ALU.add),
                  reads=[self.b_vecs], writes=[self.b_dv])
        sc.op("pool", lambda e: e.memset(self.state[:], 0.0), writes=[b for bl in self.b_state for b in bl])
        sc.op("pool", lambda e: e.memset(self.halo[:], 0.0), writes=[self.b_halo])
        sc.op("pool", lambda e: e.memset(self.zlast[:], 0.0), writes=[self.b_zlast])
        for pr in range(4):
            for n in ("A", "B", "K", "V"):
                sc.op("pool", lambda e, n=n, pr=pr: e.memset(self.pads4[pr][n][:], 0.0), writes=self.b_pads4[pr][n])
        for hh in range(2):
            sc.op("pool", lambda e, hh=hh: e.memset(self.vntok[hh][:], 0.0), writes=self.b_vntok)
        for ti in range(NT):
            t0 = ti * T
            sc.op("sp", lambda e, t0=t0: e.dma_start(out=self.xt[:], in_=self.d_x[:, t0:t0 + T].rearrange("(c p) t -> p c t", p=128)),
                  writes=self.b_xt, dsem=self.s_x)
            for l in range(NL):
                self.tile_layer(ti, l)
            ev = sc.op("sp", lambda e, t0=t0: e.dma_start(out=self.d_y[:, t0:t0 + T].rearrange("(c p) t -> p c t", p=128), in_=self.xt[:]),
                       reads=self.b_xt, dsem=self.s_y, force=True)
        sc.wait("sp", (self.s_y, self.s_y.cnt))

    def wload(self, src_ap, ncols=1024):
        i = self.wptr
        self.wptr = (i + 1) % self.nslot
        tile, buf, sem = self.wring[i], self.b_wring[i], self.s_wring[i]
        q = "sp" if self.wdt == F32 else "pool"
        self.sc.op(q, lambda e: e.dma_start(out=tile[:, 0:ncols], in_=src_ap), writes=[buf], dsem=sem)
        return tile, buf

    def rstd_from(self, src_ap, src_bufs, scale, eps, clamp=None):
        sc = self.sc
        t, b = self.stt()
        if clamp is not None:
            sc.op("dve", lambda e: e.tensor_scalar(t[:], src_ap, clamp, None, ALU.max), reads=src_bufs, writes=[b])
            sc.op("act", lambda e: e.activation(t[:], t[:], AF.Ln), reads=[b], writes=[b])
        else:
            sc.op("act", lambda e: e.activation(t[:], src_ap, AF.Ln, bias=float(eps), scale=scale), reads=src_bufs + [self.b_consts], writes=[b])
        sc.op("act", lambda e: e.activation(t[:], t[:], AF.Exp, scale=-0.5), reads=[b], writes=[b])
        return t, b

    def ln_stats(self, x_aps, x_bufs, ones_off, nfeat, eps):
        sc, T = self.sc, self.T
        ones = self.cst(ones_off, 128)
        n = len(x_aps)
        sqs = []
        for i in range(n):
            t, b = self.stt()
            sc.op("dve", lambda e, t=t, i=i: e.tensor_tensor(t[:], x_aps[i], x_aps[i], ALU.mult), reads=[x_bufs[i]], writes=[b])
            sqs.append((t, b))
        p1 = self.ps(T)
        for i in range(n):
            sc.op("pe", lambda e, i=i: e.matmul(p1.ap, ones, x_aps[i], start=(i == 0), stop=(i == n - 1)),
                  reads=[x_bufs[i], self.b_consts], writes=p1.bufs)
        p2 = self.ps(T)
        for i in range(n):
            sc.op("pe", lambda e, i=i: e.matmul(p2.ap, ones, sqs[i][0][:], start=(i == 0), stop=(i == n - 1)),
                  reads=[sqs[i][1], self.b_consts], writes=p2.bufs)
        mean, bm = self.stt()
        sc.op("act", lambda e: e.activation(mean[:], p1.ap, AF.Identity, scale=1.0 / nfeat), reads=p1.bufs, writes=[bm])
        msq, bq = self.stt()
        sc.op("dve", lambda e: e.tensor_tensor(msq[:], mean[:], mean[:], ALU.mult), reads=[bm], writes=[bq])
        var, bv = self.stt()
        sc.op("dve", lambda e: e.scalar_tensor_tensor(var[:], p2.ap, 1.0 / nfeat, msq[:], ALU.mult, ALU.subtract),
              reads=p2.bufs + [bq], writes=[bv])
        rs, brs = self.rstd_from(var[:], [bv], 1.0, eps)
        return mean, bm, rs, brs

    def tile_layer(self, ti, l):
        sc, T, NCH = self.sc, self.T, self.NCH
        t0 = ti * T
        wdt = self.wdt
        ident = self.cst(C_ID, 128)
        ones = self.cst(C_ONES, 128)
        sc.op("sp", lambda e: e.dma_start(out=self.lp[:, 0:512], in_=self.d_sguT[l]), writes=[self.b_lp], dsem=self.s_lp)
        sc.op("sp", lambda e: e.dma_start(out=self.lp[:, 512:768], in_=self.d_sgub[l]), writes=[self.b_lp], dsem=self.s_lp)
        sc.op("sp", lambda e: e.dma_start(out=self.lp[:, 768:1280], in_=self.d_pww[l]), writes=[self.b_lp], dsem=self.s_lp)
        sc.op("sp", lambda e: e.dma_start(out=self.lp[:, 1280:2304], in_=self.d_lora[l]), writes=[self.b_lp], dsem=self.s_lp)
        sguT = self.lp[:, 0:512]
        sc.op("pool", lambda e: e.memset(self.lp[64:128, 0:512].rearrange("p (h i) -> p h i", h=4)[:, :, 0:64], 0.0),
              reads=[self.b_lp], writes=[self.b_lp])
        qd = "sp" if wdt == F32 else "pool"
        sc.op(qd, lambda e: e.dma_start(out=self.pt[:], in_=self.d_p[l, :, t0:t0 + T].rearrange("(c p) t -> p c t", p=128)),
              writes=[self.b_pt], dsem=self.s_pt)

        if self.stage < 1:
            return
        sqs0 = []
        for c in range(8):
            t, b = self.stt()
            sc.op("act", lambda e, t=t, c=c: e.activation(t[:], self.xt[:, c, :], AF.Square), reads=[self.b_xt[c]], writes=[b])
            sqs0.append((t, b))
        pss0 = self.ps(T)
        for c in range(8):
            sc.op("pe", lambda e, c=c: e.matmul(pss0.ap, ones, sqs0[c][0][:], start=(c == 0), stop=(c == 7)),
                  reads=[sqs0[c][1], self.b_consts], writes=pss0.bufs)
        rs0, brs0 = self.rstd_from(pss0.ap, pss0.bufs, 1.0 / D, RMS_EPS)
        for c in range(8):
            sc.op("dve", lambda e, c=c: e.scalar_tensor_tensor(self.hT[:, c, :], self.xt[:, c, :], self.vcol(l, V_PREG + c), rs0[:], ALU.mult, ALU.mult),
                  reads=[self.b_xt[c], brs0, self.b_vecs], writes=[self.b_hT[c]])

        def zchunk(oc):
            wt, wb = self.wload(self.d_win[l, oc])
            p = self.ps(T)
            import os
            if os.environ.get("KDUP"):
                sc.op("pe", lambda e: e.matmul(p.ap, wt[:, 0:128], self.hT[:, 0, :], start=True, stop=True), reads=[wb, self.b_hT[0]], writes=p.bufs)
            for kc in range(8):
                sc.op("pe", lambda e, kc=kc: e.matmul(p.ap, wt[:, kc * 128:(kc + 1) * 128], self.hT[:, kc, :], start=(kc == 0), stop=(kc == 7)),
                      reads=[wb, self.b_hT[kc]], writes=p.bufs)
            return p

        if self.stage < 2.1:
            if self.stage == 1.5:
                for c in range(8):
                    sc.op("act", lambda e, c=c: e.activation(self.xt[:, c, :], self.hT[:, c, :], AF.Identity), reads=[self.b_hT[c]], writes=[self.b_xt[c]])
            return
        def gen_a():
            if self.stage == 2.17:
                wt, wb = self.wload(self.d_win[l, 2])
                p = self.ps(T)
                sc.op("pe", lambda e: e.matmul(p.ap, wt[:, 0:128], self.hT[:, 0, :], start=True, stop=True), reads=[wb, self.b_hT[0]], writes=p.bufs)
                sc.op("act", lambda e: e.activation(self.xt[:, 0, :], p.ap, AF.Identity), reads=p.bufs, writes=[self.b_xt[0]])
                p2 = self.ps(T)
                for kc in range(8):
                    sc.op("pe", lambda e, kc=kc: e.matmul(p2.ap, wt[:, kc * 128:(kc + 1) * 128], self.hT[:, kc, :], start=(kc == 0), stop=(kc == 7)),
                          reads=[wb, self.b_hT[kc]], writes=p2.bufs)
                sc.op("act", lambda e: e.activation(self.xt[:, 1, :], p2.ap, AF.Identity), reads=p2.bufs, writes=[self.b_xt[1]])
                p3 = self.ps(T)
                for kc in range(8):
                    sc.op("pe", lambda e, kc=kc: e.matmul(p3.ap, wt[:, kc * 128:(kc + 1) * 128], self.hT[:, kc, :], start=(kc == 0), stop=(kc == 7)),
                          reads=[wb, self.b_hT[kc]], writes=p3.bufs)
                sc.op("dve", lambda e: e.tensor_copy(self.xt[:, 2, :], p3.ap), reads=p3.bufs, writes=[self.b_xt[2]])
                return
            if self.stage == 2.15:
                for i in range(3):
                    wt, wb = self.wload(self.d_win[l, 2 + i])
                    sc.op("act", lambda e, wt=wt, i=i: e.activation(self.xt[:, i, :], wt[:, 0:256], AF.Identity), reads=[wb], writes=[self.b_xt[i]])
                    sc.op("act", lambda e, wt=wt, i=i: e.activation(self.xt[:, 3 + i, :], wt[:, 768:1024], AF.Identity), reads=[wb], writes=[self.b_xt[3 + i]])
                return
            sga = []
            for c in range(2):
                yield
                p = zchunk(4 + c)
                t, b = self.stt()
                sc.op("act", lambda e, t=t, p=p: e.activation(t[:], p.ap, AF.Silu), reads=p.bufs, writes=[b])
                sga.append((t, b))
            for c in range(2):
                yield
                p = zchunk(0 + c)
                sc.op("dve", lambda e, c=c, p=p: e.tensor_tensor(self.ug[:, c, :], p.ap, sga[c][0][:], ALU.mult),
                      reads=p.bufs + [sga[c][1]], writes=[self.b_ug[c]])
            for c in range(2):
                yield
                p = zchunk(2 + c)
                sc.op("act", lambda e, c=c, p=p: e.activation(self.va[:, c, :], p.ap, AF.Identity), reads=p.bufs, writes=[self.b_va[c]])
            if self.stage < 2.2:
                if self.stage == 2.19:
                    srcs = [(self.va[:, 0, :], self.b_va[0]), (self.va[:, 1, :], self.b_va[1]), (self.ug[:, 0, :], self.b_ug[0]), (self.ug[:, 1, :], self.b_ug[1])]
                    for c in range(4):
                        yield
                        sc.op("act", lambda e, c=c: e.activation(self.xt[:, c, :], srcs[c][0], AF.Identity), reads=[srcs[c][1]], writes=[self.b_xt[c]])
                return
            meanA, bmA, rsA, brsA = self.ln_stats([self.va[:, c, :] for c in range(2)], self.b_va, C_ONES, AW, LN_EPS)
            yield
            if self.stage < 2.4:
                if self.stage == 2.3:
                    srcs = [(self.va[:, 0, :], self.b_va[0]), (self.va[:, 1, :], self.b_va[1]), (self.ug[:, 0, :], self.b_ug[0]), (self.ug[:, 1, :], self.b_ug[1])]
                    for c in range(4):
                        yield
                        sc.op("act", lambda e, c=c: e.activation(self.xt[:, c, :], srcs[c][0], AF.Identity), reads=[srcs[c][1]], writes=[self.b_xt[c]], force=True)
                return
            for c in range(2):
                yield
                t, b = self.stt()
                sc.op("dve", lambda e, c=c, t=t: e.tensor_tensor(t[:], self.va[:, c, :], meanA[:], ALU.subtract), reads=[self.b_va[c], bmA], writes=[b])
                sc.op("dve", lambda e, t=t: e.tensor_tensor(t[:], t[:], rsA[:], ALU.mult), reads=[b, brsA], writes=[b])
                sc.op("act", lambda e, c=c, t=t: e.activation(self.vn[:, c, :], t[:], AF.Identity, bias=self.vcol(l, V_SLB + c), scale=self.vcol(l, V_SLG + c)),
                      reads=[b, self.b_vecs], writes=[self.b_vn[c]])
            if self.stage < 2.6:
                return
            for blk in range(self.NBLK):
                for c in range(2):
                    yield
                    p = self.ps(128)
                    sc.op("pe", lambda e, p=p, c=c, blk=blk: e.transpose(p.ap, self.vn[:, c, blk * 128:(blk + 1) * 128], ident),
                          reads=[self.b_vn[c], self.b_consts], writes=p.bufs)
                    for hh in range(2):
                        sc.op("act", lambda e, p=p, c=c, blk=blk, hh=hh: e.activation(self.vntok[hh][:, blk, c * 128 + hh * 64:c * 128 + hh * 64 + 64], p.ap[:, hh * 64:hh * 64 + 64], AF.Identity),
                              reads=p.bufs, writes=[self.b_vntok[blk]])
            if self.stage < 2.8:
                return
            for blk in range(self.NBLK):
                for c in range(2):
                    yield
                    p = self.ps(128)
                    for hh in range(2):
                        h = 2 * c + hh
                        sc.op("pe", lambda e, p=p, c=c, hh=hh, h=h, blk=blk: e.matmul(
                            p.ap, self.vntok[hh][:, blk, c * 128:(c + 1) * 128],
                            self.lp[:, h * 128:(h + 1) * 128], start=(hh == 0), stop=(hh == 1)),
                            reads=[self.b_vntok[blk], self.b_lp], writes=p.bufs)
                    t, b = self.stt()
                    sc.op("dve", lambda e, p=p, c=c, t=t: e.tensor_tensor(t[:, 0:128], p.ap, self.lp[:, 512 + c * 128:512 + (c + 1) * 128], ALU.add),
                          reads=p.bufs + [self.b_lp], writes=[b])
                    sc.op("dve", lambda e, c=c, t=t, blk=blk: e.tensor_tensor(self.mixT[:, c, blk * 128:(blk + 1) * 128], t[:, 0:128],
                                                                               self.ug[:, c, blk * 128:(blk + 1) * 128], ALU.mult),
                          reads=[b, self.b_ug[c]], writes=[self.b_mix[c]])

            if self.stage < 3:
                return

            yield
        def gen_b():
            sgl = []
            for c in range(2):
                yield
                p = zchunk(8 + c)
                t, b = self.stt()
                sc.op("act", lambda e, t=t, p=p: e.activation(t[:], p.ap, AF.Sigmoid), reads=p.bufs, writes=[b])
                sgl.append((t, b))
            for c in range(2):
                yield
                hoff = (l * 2 + c) * HALO
                sc.op("pool", lambda e, c=c, hoff=hoff: e.tensor_copy(self.ybuf[:, c, 0:HALO], self.halo[:, hoff:hoff + HALO]),
                      reads=[self.b_halo], writes=[self.b_ybuf[c]])
                p = zchunk(6 + c)
                sc.op("dve", lambda e, c=c, p=p: e.tensor_tensor(self.ybuf[:, c, HALO:HALO + T], p.ap, sgl[c][0][:], ALU.mult),
                      reads=p.bufs + [sgl[c][1]], writes=[self.b_ybuf[c]])
                sc.op("pool", lambda e, c=c, hoff=hoff: e.tensor_copy(self.halo[:, hoff:hoff + HALO], self.ybuf[:, c, T:T + HALO]),
                      reads=[self.b_ybuf[c]], writes=[self.b_halo])
            for c in range(2):
                yield
                p = zchunk(10 + c)
                sc.op("act", lambda e, c=c, p=p: e.activation(self.sgb[:, c, :], p.ap, AF.Silu), reads=p.bufs, writes=[self.b_sgb[c]])
            for c in range(2):
                yield
                p = self.ps(T)
                for tap in range(CONVW):
                    di = self.dgptr
                    self.dgptr = (di + 1) % self.NDG
                    dg, dgb = self.diag[di], self.b_diag[di]
                    sc.op("pool", lambda e, dg=dg, c=c, tap=tap: e.tensor_scalar(dg[:], ident, self.vcol(l, V_CW + c * CONVW + tap), None, ALU.mult),
                          reads=[self.b_consts, self.b_vecs], writes=[dgb])
                    sc.op("pe", lambda e, dg=dg, c=c, tap=tap, p=p: e.matmul(p.ap, dg[:], self.ybuf[:, c, tap:tap + T], start=(tap == 0), stop=(tap == CONVW - 1)),
                          reads=[dgb, self.b_ybuf[c]], writes=p.bufs)
                sc.op("act", lambda e, c=c, p=p: e.activation(self.yc[:, c, :], p.ap, AF.Identity, bias=self.vcol(l, V_CB + c)),
                      reads=p.bufs + [self.b_vecs], writes=[self.b_yc[c]])
            meanB, bmB, rsB, brsB = self.ln_stats([self.yc[:, c, :] for c in range(2)], self.b_yc, C_ONES, BW, LN_EPS)
            yield
            for c in range(2):
                yield
                t, b = self.stt()
                sc.op("dve", lambda e, c=c, t=t: e.tensor_tensor(t[:], self.yc[:, c, :], meanB[:], ALU.subtract), reads=[self.b_yc[c], bmB], writes=[b])
                sc.op("dve", lambda e, t=t: e.tensor_tensor(t[:], t[:], rsB[:], ALU.mult), reads=[b, brsB], writes=[b])
                sc.op("act", lambda e, c=c, t=t: e.activation(self.yn[:, c, :], t[:], AF.Silu, bias=self.vcol(l, V_CLB + c), scale=self.vcol(l, V_CLG + c)),
                      reads=[b, self.b_vecs], writes=[self.b_yn[c]])
            for co in range(2):
                yield
                p = self.ps(T)
                for ci in range(2):
                    sc.op("pe", lambda e, p=p, ci=ci, co=co: e.matmul(p.ap, self.lp[:, 768 + ci * 256 + co * 128:768 + ci * 256 + (co + 1) * 128], self.yn[:, ci, :],
                                                                     start=(ci == 0), stop=(ci == 1)),
                          reads=[self.b_lp, self.b_yn[ci]], writes=p.bufs)
                sc.op("dve", lambda e, p=p, co=co: e.scalar_tensor_tensor(self.mixT[:, 2 + co, :], p.ap, self.vcol(l, V_PWB + co), self.sgb[:, co, :], ALU.add, ALU.mult),
                      reads=p.bufs + [self.b_sgb[co], self.b_vecs], writes=[self.b_mix[2 + co]])

            if self.stage < 4:
                return

            yield
        for c in range(4):
            p = zchunk(25 + c)
            sc.op("act", lambda e, c=c, p=p: e.activation(self.sgc[:, c, :], p.ap, AF.Silu), reads=p.bufs, writes=[self.b_sgc[c]])
        for c in range(13):
            p = zchunk(12 + c)
            zl = self.zlast[:, l * 13 + c:l * 13 + c + 1]
            sc.op("act", lambda e, c=c, p=p: e.activation(self.zs[:, c, :], p.ap, AF.Identity, scale=self.dcol(l, DV_OMM + c)),
                  reads=p.bufs + [self.b_dv], writes=[self.b_zs[c]])
            sc.op("dve", lambda e, c=c, p=p: e.scalar_tensor_tensor(self.zs[:, c, 1:T], p.ap[:, 0:T - 1], self.vcol(l, V_MU + c), self.zs[:, c, 1:T], ALU.mult, ALU.add),
                  reads=p.bufs + [self.b_zs[c], self.b_vecs], writes=[self.b_zs[c]])
            sc.op("dve", lambda e, c=c, zl=zl: e.scalar_tensor_tensor(self.zs[:, c, 0:1], zl, self.vcol(l, V_MU + c), self.zs[:, c, 0:1], ALU.mult, ALU.add),
                  reads=[self.b_zlast, self.b_zs[c], self.b_vecs], writes=[self.b_zs[c]])
            sc.op("act", lambda e, p=p, zl=zl: e.activation(zl, p.ap[:, T - 1:T], AF.Identity), reads=p.bufs, writes=[self.b_zlast])
        sc.op("act", lambda e: e.activation(self.zs[0:64, 12, :], self.zs[0:64, 12, :], AF.Tanh), reads=[self.b_zs[12]], writes=[self.b_zs[12]])
        if self.stage < 5 and self.stage != 4.5:
            return
        for pr in range(4):
            self.rwkv_pair(l, pr)
        extra = [gen_a(), gen_b()]
        for j in range(NCH):
            gens = [self.scan_unit(l, pr, j) for pr in range(4)]
            while gens:
                for g in list(gens):
                    try:
                        next(g)
                    except StopIteration:
                        gens.remove(g)
                if extra:
                    try:
                        next(extra[0])
                    except StopIteration:
                        extra.pop(0)
        for g in extra:
            for _ in g:
                pass
        for pr in range(4):
            self.rwkv_post(l, pr)
        if self.stage < 7:
            if self.stage == 4.5:
                srcs = [(self.va[:, 0, :], self.b_va[0]), (self.va[:, 1, :], self.b_va[1]), (self.ug[:, 0, :], self.b_ug[0]), (self.ug[:, 1, :], self.b_ug[1]),
                        (self.sgb[:, 0, :], self.b_sgb[0]), (self.yc[:, 0, :], self.b_yc[0]), (self.zs[:, 0, :], self.b_zs[0]), (self.sgc[:, 0, :], self.b_sgc[0])]
                for c in range(8):
                    sc.op("act", lambda e, c=c: e.activation(self.xt[:, c, :], srcs[c][0], AF.Identity), reads=[srcs[c][1]], writes=[self.b_xt[c]], force=True)
            if self.stage == 6.5:
                for c in range(8):
                    sc.op("act", lambda e, c=c: e.activation(self.xt[:, c, :], self.mixT[:, c, :], AF.Identity), reads=[self.b_mix[c]], writes=[self.b_xt[c]])
            return

        for oc in range(8):
            wt, wb = self.wload(self.d_wout[l, oc])
            p = self.ps(T)
            for kc in range(8):
                sc.op("pe", lambda e, kc=kc, wt=wt, p=p: e.matmul(p.ap, wt[:, kc * 128:(kc + 1) * 128], self.mixT[:, kc, :], start=(kc == 0), stop=(kc == 7)),
                      reads=[wb, self.b_mix[kc]], writes=p.bufs)
            sc.op("act", lambda e, oc=oc, p=p: e.activation(self.mo[:, oc, :], p.ap, AF.Identity), reads=p.bufs, writes=[self.b_mo[oc]])
        sqs1 = []
        for c in range(8):
            t, b = self.stt()
            sc.op("act", lambda e, t=t, c=c: e.activation(t[:], self.mo[:, c, :], AF.Square), reads=[self.b_mo[c]], writes=[b])
            sqs1.append((t, b))
        pss1 = self.ps(T)
        for c in range(8):
            sc.op("pe", lambda e, c=c: e.matmul(pss1.ap, ones, sqs1[c][0][:], start=(c == 0), stop=(c == 7)),
                  reads=[sqs1[c][1], self.b_consts], writes=pss1.bufs)
        rs1, brs1 = self.rstd_from(pss1.ap, pss1.bufs, 1.0 / D, RMS_EPS)
        for c in range(8):
            sc.op("dve", lambda e, c=c: e.scalar_tensor_tensor(self.mo[:, c, :], self.mo[:, c, :], self.vcol(l, V_POSTG + c), rs1[:], ALU.mult, ALU.mult),
                  reads=[self.b_mo[c], brs1, self.b_vecs], writes=[self.b_mo[c]])
            sc.op("dve", lambda e, c=c: e.tensor_tensor(self.xt[:, c, :], self.xt[:, c, :], self.mo[:, c, :], ALU.add),
                  reads=[self.b_xt[c], self.b_mo[c]], writes=[self.b_xt[c]])
        if wdt != F32:
            for c in range(8):
                sc.op("act", lambda e, c=c: e.activation(self.hT[:, c, :], self.xt[:, c, :], AF.Identity), reads=[self.b_xt[c]], writes=[self.b_hT[c]])
            xsrc, xb = self.hT, self.b_hT
        else:
            xsrc, xb = self.xt, self.b_xt
        for oc in range(8):
            wt, wb = self.wload(self.d_gw[l, oc])
            p = self.ps(T)
            for kc in range(8):
                sc.op("pe", lambda e, kc=kc, wt=wt, p=p: e.matmul(p.ap, wt[:, kc * 128:(kc + 1) * 128], xsrc[:, kc, :], start=(kc == 0), stop=(kc == 7)),
                      reads=[wb, xb[kc]], writes=p.bufs)
            sc.op("act", lambda e, oc=oc, p=p: e.activation(self.mo[:, oc, :], p.ap, AF.Sigmoid, bias=self.vcol(l, V_GATEB + oc)),
                  reads=p.bufs + [self.b_vecs], writes=[self.b_mo[oc]])
        for oc in range(8):
            wt, wb = self.wload(self.d_plew[l, oc], 256)
            p = self.ps(T)
            for kc in range(2):
                sc.op("pe", lambda e, kc=kc, wt=wt, p=p: e.matmul(p.ap, wt[:, kc * 128:(kc + 1) * 128], self.pt[:, kc, :], start=(kc == 0), stop=(kc == 1)),
                      reads=[wb, self.b_pt], writes=p.bufs)
            sc.op("dve", lambda e, oc=oc, p=p: e.tensor_tensor(self.mo[:, oc, :], p.ap, self.mo[:, oc, :], ALU.mult),
                  reads=p.bufs + [self.b_mo[oc]], writes=[self.b_mo[oc]])
            sc.op("dve", lambda e, oc=oc: e.tensor_tensor(self.xt[:, oc, :], self.xt[:, oc, :], self.mo[:, oc, :], ALU.add),
                  reads=[self.b_xt[oc], self.b_mo[oc]], writes=[self.b_xt[oc]])

    def rwkv_pair(self, l, pr):
        sc, T, NCH = self.sc, self.T, self.NCH
        ident = self.cst(C_ID, 128)
        blk = self.cst(C_BLK, 128)
        P = dict(self.pt_sh); P.update(self.pt_pp[pr])
        Bf = dict(self.b_pt_sh); Bf.update(self.b_pt_pp[pr])
        pads, b_pads = self.pads4[pr], self.b_pads4[pr]
        Rt, b_Rt = self.Rt4[pr], self.b_Rt4[pr]
        r_ap, r_b = self.zs[:, 0 + pr, :], self.b_zs[0 + pr]
        k_ap, k_b = self.zs[:, 4 + pr, :], self.b_zs[4 + pr]
        v_ap, v_b = self.zs[:, 8 + pr, :], self.b_zs[8 + pr]
        lora_w = self.lp[:, 1280 + pr * 128:1280 + (pr + 1) * 128]
        lora_a = self.lp[:, 1792 + pr * 128:1792 + (pr + 1) * 128]
        p = self.ps(T)
        sc.op("pe", lambda e: e.matmul(p.ap, lora_w, self.zs[:, 12, :], start=True, stop=True), reads=[self.b_lp, self.b_zs[12]], writes=p.bufs)
        sc.op("act", lambda e: e.activation(P["lw"][:], p.ap, AF.Sigmoid, bias=self.vcol(l, V_W0 + pr)), reads=p.bufs + [self.b_vecs], writes=[Bf["lw"]])
        p2 = self.ps(T)
        sc.op("pe", lambda e: e.matmul(p2.ap, lora_a, self.zs[:, 12, :], start=True, stop=True), reads=[self.b_lp, self.b_zs[12]], writes=p2.bufs)
        sc.op("act", lambda e: e.activation(P["aa"][:], p2.ap, AF.Sigmoid, bias=self.vcol(l, V_A0 + pr)), reads=p2.bufs + [self.b_vecs], writes=[Bf["aa"]])
        sc.op("dve", lambda e: e.tensor_scalar(P["lw"][:], P["lw"][:], -DECAY, None, ALU.mult), reads=[Bf["lw"]], writes=[Bf["lw"]])
        sc.op("dve", lambda e: e.tensor_tensor_scan(P["cum"][:], self.cst(C_RESET, T), P["lw"][:], 0.0, ALU.mult, ALU.add),
              reads=[Bf["lw"], self.b_consts], writes=[Bf["cum"]])
        sc.op("dve", lambda e: e.tensor_tensor(P["cume"][:], P["cum"][:], P["lw"][:], ALU.subtract), reads=[Bf["cum"], Bf["lw"]], writes=[Bf["cume"]])
        sc.op("act", lambda e: e.activation(P["E1"][:], P["cum"][:], AF.Exp), reads=[Bf["cum"]], writes=[Bf["E1"]])
        sc.op("act", lambda e: e.activation(P["E2"][:], P["cum"][:], AF.Exp, scale=-1.0), reads=[Bf["cum"]], writes=[Bf["E2"]])
        sc.op("act", lambda e: e.activation(P["E3"][:], P["cume"][:], AF.Exp), reads=[Bf["cume"]], writes=[Bf["E3"]])
        sc.op("dve", lambda e: e.tensor_scalar(P["kk0"][:], k_ap, self.vcol(l, V_KK + pr), None, ALU.mult), reads=[k_b, self.b_vecs], writes=[Bf["kk0"]])
        t, b = self.stt()
        sc.op("act", lambda e: e.activation(t[:], P["kk0"][:], AF.Square), reads=[Bf["kk0"]], writes=[b])
        p3 = self.ps(T)
        sc.op("pe", lambda e: e.matmul(p3.ap, blk, t[:], start=True, stop=True), reads=[b, self.b_consts], writes=p3.bufs)
        rn, brn = self.rstd_from(p3.ap, p3.bufs, 1.0, 0.0, clamp=1e-12)
        sc.op("dve", lambda e: e.tensor_tensor(P["kkn"][:], P["kk0"][:], rn[:], ALU.mult), reads=[Bf["kk0"], brn], writes=[Bf["kkn"]])
        t2, b2 = self.stt()
        sc.op("dve", lambda e: e.tensor_scalar(t2[:], P["aa"][:], self.vcol(l, V_KA + pr), self.dcol(l, DV_OMKA + pr), ALU.mult, ALU.add),
              reads=[Bf["aa"], self.b_vecs, self.b_dv], writes=[b2])
        sc.op("dve", lambda e: e.tensor_tensor(P["kmod"][:], k_ap, t2[:], ALU.mult), reads=[k_b, b2], writes=[Bf["kmod"]])
        sc.op("dve", lambda e: e.tensor_tensor(P["bvec"][:], P["kkn"][:], P["aa"][:], ALU.mult), reads=[Bf["kkn"], Bf["aa"]], writes=[Bf["bvec"]])
        def v3(ap):
            return ap.rearrange("p (j t) -> p j t", t=CH)

        for hh in range(2):
            rows = slice(hh * 64, hh * 64 + 64)
            def pv(n, hh=hh, rows=rows):
                return pads[n][rows, :].rearrange("p (j h t) -> p j h t", h=2, t=CH)[:, :, hh, :]
            sc.op("dve", lambda e, rows=rows, pv=pv: e.scalar_tensor_tensor(pv("A"), v3(P["kkn"][rows, :]), -1.0, v3(P["E3"][rows, :]), ALU.mult, ALU.mult),
                  reads=[Bf["kkn"], Bf["E3"]], writes=b_pads["A"])
            sc.op("dve", lambda e, rows=rows, pv=pv: e.tensor_tensor(pv("B"), v3(P["bvec"][rows, :]), v3(P["E2"][rows, :]), ALU.mult),
                  reads=[Bf["bvec"], Bf["E2"]], writes=b_pads["B"])
            sc.op("dve", lambda e, rows=rows, pv=pv: e.tensor_tensor(pv("K"), v3(P["kmod"][rows, :]), v3(P["E2"][rows, :]), ALU.mult),
                  reads=[Bf["kmod"], Bf["E2"]], writes=b_pads["K"])
            sc.op("act", lambda e, rows=rows, pv=pv: e.activation(pv("V"), v3(self.zs[rows, 8 + pr, :]), AF.Identity),
                  reads=[v_b], writes=b_pads["V"])
        sc.op("dve", lambda e: e.tensor_tensor(Rt[:], r_ap, P["E1"][:], ALU.mult), reads=[r_b, Bf["E1"]], writes=[b_Rt])

    def rwkv_post(self, l, pr):
        sc, T, NCH = self.sc, self.T, self.NCH
        blk = self.cst(C_BLK, 128)
        P = dict(self.pt_sh); P.update(self.pt_pp[pr])
        Bf = dict(self.b_pt_sh); Bf.update(self.b_pt_pp[pr])
        r_ap, r_b = self.zs[:, 0 + pr, :], self.b_zs[0 + pr]
        v_ap, v_b = self.zs[:, 8 + pr, :], self.b_zs[8 + pr]
        yy, byy = P["yy"], Bf["yy"]
        meanC, bmC, rsC, brsC = self.ln_stats([yy[:]], [byy], C_BLK, CH, GN_EPS)
        tpo, bpo = self.stt()
        sc.op("dve", lambda e: e.tensor_tensor(tpo[:], yy[:], meanC[:], ALU.subtract), reads=[byy, bmC], writes=[bpo])
        sc.op("dve", lambda e: e.tensor_tensor(tpo[:], tpo[:], rsC[:], ALU.mult), reads=[bpo, brsC], writes=[bpo])
        sc.op("act", lambda e: e.activation(tpo[:], tpo[:], AF.Identity, bias=self.vcol(l, V_LXB + pr), scale=self.vcol(l, V_LXG + pr)),
              reads=[bpo, self.b_vecs], writes=[bpo])
        t3, b3 = self.stt()
        sc.op("dve", lambda e: e.scalar_tensor_tensor(t3[:], r_ap, self.vcol(l, V_RK + pr), P["kmod"][:], ALU.mult, ALU.mult),
              reads=[r_b, Bf["kmod"], self.b_vecs], writes=[b3])
        p4 = self.ps(T)
        sc.op("pe", lambda e: e.matmul(p4.ap, blk, t3[:], start=True, stop=True), reads=[b3, self.b_consts], writes=p4.bufs)
        sc.op("dve", lambda e: e.tensor_tensor(t3[:], p4.ap, v_ap, ALU.mult), reads=p4.bufs + [v_b], writes=[b3])
        sc.op("dve", lambda e: e.tensor_tensor(tpo[:], tpo[:], t3[:], ALU.add), reads=[bpo, b3], writes=[bpo])
        sc.op("dve", lambda e: e.tensor_tensor(self.mixT[:, 4 + pr, :], tpo[:], self.sgc[:, pr, :], ALU.mult),
              reads=[bpo, self.b_sgc[pr]], writes=[self.b_mix[4 + pr]])

    def scan_unit(self, l, pr, j):
        sc, T = self.sc, self.T
        ident = self.cst(C_ID, 128)
        msu = self.cst(C_MSU, 128)
        msl = self.cst(C_MSL, 128)
        mui = self.cst(C_MUI, 64)
        U = self.ut[pr]
        cs = slice(j * 128, (j + 1) * 128)
        Ap, Bp, Kp, Vp = (self.pads4[pr][n][:, cs] for n in ("A", "B", "K", "V"))
        bA, bB, bK, bV = (self.b_pads4[pr][n][j] for n in ("A", "B", "K", "V"))
        Rst = self.Rt4[pr][:, j * CH:(j + 1) * CH]
        b_Rt = self.b_Rt4[pr]
        CB = self.b_consts

        def mm(out_ps, lhsT, rhs, reads, start=True, stop=True):
            sc.op("pe", lambda e: e.matmul(out_ps.ap, lhsT, rhs, start=start, stop=stop), reads=reads, writes=out_ps.bufs)

        def ev_mask(dst, p, mask):
            sc.op("dve", lambda e: e.tensor_tensor(dst[0][:], p.ap, mask, ALU.mult), reads=p.bufs + [CB], writes=[dst[1]])

        def ev_copy(dst, p, dst_ap=None):
            d = dst[0][:] if dst_ap is None else dst_ap
            sc.op("act", lambda e: e.activation(d, p.ap, AF.Identity), reads=p.bufs, writes=[dst[1]])

        p = self.ps(128); mm(p, Bp, Ap, [bB, bA]); ev_mask(U["B0"], p, msu)
        p = self.ps(128); mm(p, Ap, Bp, [bA, bB]); ev_mask(U["A0"], p, msl)
        p = self.ps(128); mm(p, Kp, Ap, [bK, bA]); ev_mask(U["MakT"], p, msu)
        p = self.ps(64); mm(p, Bp, Rst, [bB, b_Rt]); ev_mask(U["MrbT"], p, mui)
        p = self.ps(64); mm(p, Kp, Rst, [bK, b_Rt]); ev_mask(U["MrkT"], p, mui)
        yield
        def tr(dst, src, bsrc, dst_ap=None):
            p = self.ps(128)
            sc.op("pe", lambda e: e.transpose(p.ap, src, ident), reads=[bsrc, CB], writes=p.bufs)
            ev_copy(dst, p, dst_ap)
        tr(U["Z0"], Ap, bA, U["Z0"][0][:, 0:128])
        tr(U["Btok"], Bp, bB)
        tr(U["Ktok"], Kp, bK)
        tr(U["Vbd"], Vp, bV)
        yield
        p = self.ps(128); mm(p, U["MakT"][0][:], U["Vbd"][0][:], [U["MakT"][1], U["Vbd"][1]])
        ev_copy(U["Z0"], p, U["Z0"][0][:, 128:256])
        Acur, Bcur, Anext, Bnext = U["A0"], U["B0"], U["A1"], U["B1"]
        Zc, Zn = U["Z0"], U["Z1"]
        for n in range(6):
            yield
            p = self.ps(256)
            mm(p, Bcur[0][:], Zc[0][:], [Bcur[1], Zc[1]])
            sc.op("dve", lambda e, p=p, Zc=Zc, Zn=Zn: e.tensor_tensor(Zn[0][:], p.ap, Zc[0][:], ALU.add), reads=p.bufs + [Zc[1]], writes=[Zn[1]])
            Zc, Zn = Zn, Zc
            if n < 5:
                pb = self.ps(128); mm(pb, Acur[0][:], Bcur[0][:], [Acur[1], Bcur[1]])
                if n < 4:
                    pa = self.ps(128); mm(pa, Bcur[0][:], Acur[0][:], [Acur[1], Bcur[1]])
                ev_copy(Bnext, pb)
                if n < 4:
                    ev_copy(Anext, pa)
                Acur, Anext = Anext, Acur
                Bcur, Bnext = Bnext, Bcur
        yield
        Pm, Qm = Zc[0][:, 0:128], Zc[0][:, 128:256]
        bZ = Zc[1]
        p = self.ps(64)
        mm(p, ident, Rst, [CB, b_Rt], start=True, stop=False)
        mm(p, Pm, U["MrbT"][0][:], [bZ, U["MrbT"][1]], start=False, stop=True)
        ev_copy(U["Rp"], p)
        p = self.ps(128)
        mm(p, ident, ident, [CB], start=True, stop=False)
        mm(p, Pm, U["Btok"][0][:], [bZ, U["Btok"][1]], start=False, stop=True)
        ev_copy(U["G0"], p)
        yield
        st_ap = self.state[:, (l * 4 + pr) * 128:(l * 4 + pr + 1) * 128]
        bS = self.b_state[l][pr]
        py = self.ps(64)
        mm(py, Qm, U["MrbT"][0][:], [bZ, U["MrbT"][1]], start=True, stop=False)
        mm(py, U["Vbd"][0][:], U["MrkT"][0][:], [U["Vbd"][1], U["MrkT"][1]], start=False, stop=False)
        mm(py, st_ap, U["Rp"][0][:], [bS, U["Rp"][1]], start=False, stop=True)
        pS = self.ps(128)
        mm(pS, U["Btok"][0][:], Qm, [U["Btok"][1], bZ], start=True, stop=False)
        mm(pS, U["Ktok"][0][:], U["Vbd"][0][:], [U["Ktok"][1], U["Vbd"][1]], start=False, stop=False)
        mm(pS, U["G0"][0][:], st_ap, [U["G0"][1], bS], start=False, stop=True)
        yy, byy = self.pt_pp[pr]["yy"], self.b_pt_pp[pr]["yy"]
        sc.op("act", lambda e: e.activation(yy[:, j * CH:(j + 1) * CH], py.ap, AF.Identity), reads=py.bufs, writes=[byy])
        wc = self.pt_pp[pr]["E1"][:, j * CH + CH - 1:j * CH + CH]
        sc.op("dve", lambda e: e.tensor_scalar(st_ap, pS.ap, wc, None, ALU.mult), reads=pS.bufs + [self.b_pt_pp[pr]["E1"]], writes=[bS])


def make_consts(T):
    NC = C_RESET + T
    c = np.zeros((128, NC), np.float32)
    c[:, C_ID:C_ID + 128] = np.eye(128, dtype=np.float32)
    c[:, C_ONES:C_ONES + 128] = 1.0
    blk = np.zeros((128, 128), np.float32)
    blk[:64, :64] = 1.0
    blk[64:, 64:] = 1.0
    c[:, C_BLK:C_BLK + 128] = blk
    tri_u = np.triu(np.ones((64, 64), np.float32), 1)
    msu = np.zeros((128, 128), np.float32)
    msu[:64, :64] = tri_u
    msu[64:, 64:] = tri_u
    c[:, C_MSU:C_MSU + 128] = msu
    c[:, C_MSL:C_MSL + 128] = msu.T
    ui = np.triu(np.ones((64, 64), np.float32), 0)
    c[:64, C_MUI:C_MUI + 64] = ui
    c[64:, C_MUI:C_MUI + 64] = ui
    c[:64, C_PAD] = 1.0
    c[64:, C_PAD + 1] = 1.0
    c[:64, C_NPAD] = -1.0
    c[64:, C_NPAD + 1] = -1.0
    r = np.ones(T, np.float32)
    r[::CH] = 0.0
    c[:, C_RESET:C_RESET + T] = r[None, :]
    return c


def colmaj(v, n):
    return np.ascontiguousarray(v.reshape(n, 128).T)


def prep_shared(inp, NL, T):
    f = lambda a: np.ascontiguousarray(a, dtype=np.float32)
    sh = {}
    w_in = f(inp["w_in"][:NL])
    sh["win"] = np.ascontiguousarray(w_in.reshape(NL, 8, 128, NOC, 128).transpose(0, 3, 2, 1, 4)).reshape(NL, NOC, 128, 1024)
    sh["wout"] = np.ascontiguousarray(f(inp["w_out"][:NL]).reshape(NL, 8, 128, 8, 128).transpose(0, 3, 2, 1, 4)).reshape(NL, 8, 128, 1024)
    sh["gw"] = np.ascontiguousarray(f(inp["ple_gate_w"][:NL]).reshape(NL, 8, 128, 8, 128).transpose(0, 3, 2, 1, 4)).reshape(NL, 8, 128, 1024)
    sh["plew"] = np.ascontiguousarray(f(inp["ple_w"][:NL]).reshape(NL, 2, 128, 8, 128).transpose(0, 3, 2, 1, 4)).reshape(NL, 8, 128, 256)
    vecs = np.zeros((128, NL * NV), np.float32)
    for l in range(NL):
        o = l * NV
        def put(col, v, n):
            vecs[:, o + col:o + col + n] = colmaj(f(v), n)
        put(V_PREG, inp["pre_norm_g"][l], 8)
        put(V_POSTG, inp["post_norm_g"][l], 8)
        put(V_GATEB, inp["ple_gate_b"][l], 8)
        put(V_SLG, inp["sgu_ln_g"][l], 2)
        put(V_SLB, inp["sgu_ln_b"][l], 2)
        put(V_CB, inp["conv_b"][l], 2)
        put(V_CLG, inp["conv_ln_g"][l], 2)
        put(V_CLB, inp["conv_ln_b"][l], 2)
        put(V_PWB, inp["pw_b"][l], 2)
        put(V_MU, inp["shift_mu"][l], 13)
        put(V_W0, inp["w0"][l], 4)
        put(V_A0, inp["a0"][l], 4)
        put(V_KK, inp["k_k"][l], 4)
        put(V_KA, inp["k_a"][l], 4)
        put(V_RK, inp["r_k"][l].reshape(-1), 4)
        put(V_LXG, inp["lnx_g"][l], 4)
        put(V_LXB, inp["lnx_b"][l], 4)
        cw = f(inp["conv_w"][l])
        for c in range(2):
            vecs[:, o + V_CW + c * CONVW:o + V_CW + (c + 1) * CONVW] = cw[:, c * 128:(c + 1) * 128].T
    sh["vecs"] = vecs
    sh["sguT"] = np.ascontiguousarray(f(inp["sgu_w"][:NL]).transpose(0, 3, 1, 2)).reshape(NL, 128, 512)
    sb = f(inp["sgu_b"][:NL])
    sgub = np.zeros((NL, 128, 256), np.float32)
    for c in range(2):
        for hh in range(2):
            sgub[:, hh * 64:(hh + 1) * 64, c * 128:(c + 1) * 128] = sb[:, 2 * c + hh][:, None, :]
    sh["sgub"] = sgub
    sh["pww"] = np.ascontiguousarray(f(inp["pw_w"][:NL]).reshape(NL, 2, 128, 256).transpose(0, 2, 1, 3)).reshape(NL, 128, 512)
    lora = np.zeros((NL, 128, 1024), np.float32)
    lora[:, 0:64, 0:512] = f(inp["w_up"][:NL])
    lora[:, 64:128, 512:1024] = f(inp["a_up"][:NL])
    sh["lora"] = lora
    sh["consts"] = make_consts(T)
    return sh


_CACHE = {}


def run(inp, NL, NT, T, ncore, **kw):
    key = (NL, NT, T, tuple(sorted(kw.items())))
    S = NT * T
    prog = Prog(NL, NT, T, **kw)
    nc = prog.build()
    sh = prep_shared(inp, NL, T)
    in_maps = []
    for b in range(ncore):
        m = dict(sh)
        m["xT"] = np.ascontiguousarray(np.asarray(inp["x"][b, :S], np.float32).T)
        m["pT"] = np.ascontiguousarray(np.asarray(inp["p"][:NL, b, :S], np.float32).transpose(0, 2, 1))
        in_maps.append(m)
    res = run_bass_kernel_spmd(nc, in_maps, core_ids=list(range(ncore)))
    out = np.stack([np.ascontiguousarray(r["yT"].T) for r in res.results], axis=0)
    return out.astype(np.float32), prog


def kernel(**inputs):
    out, _ = run(inputs, NLAYER, SEQ // 256, 256, NCORE, wdt=BF16, nslot=4)
    return out
```

```python
import math
from contextlib import ExitStack

import numpy as np
import concourse.bass as bass
import concourse.mybir as mybir
from concourse.bass_utils import run_bass_kernel_spmd

F32 = mybir.dt.float32
BF16 = mybir.dt.bfloat16
AF = mybir.ActivationFunctionType
ALU = mybir.AluOpType

D = 1024
SEQ = 4096
NLAYER = 4
NCORE = 8
AW = 256
BW = 256
CW = 512
PLE = 256
CONVW = 31
HALO = CONVW - 1
INC = 3712
NOC = INC // 128
CH = 64
RMS_EPS = 1e-6
LN_EPS = 1e-5
GN_EPS = 64e-5
DECAY = math.exp(-0.5)

V_PREG, V_POSTG, V_GATEB = 0, 8, 16
V_SLG, V_SLB = 24, 26
V_CB, V_CLG, V_CLB, V_PWB = 28, 30, 32, 34
V_MU = 36
V_W0, V_A0, V_KK, V_KA, V_RK, V_LXG, V_LXB = 49, 53, 57, 61, 65, 69, 73
V_CW = 77
NV = V_CW + 2 * CONVW
DV_OMM, DV_OMKA = 0, 13
NDV = 17
C_ID, C_ONES, C_BLK, C_MSU, C_MSL, C_MUI, C_PAD, C_NPAD, C_RESET = 0, 128, 256, 384, 512, 640, 704, 706, 708


class Sem:
    def __init__(self, h):
        self.h = h
        self.cnt = 0


class Buf:
    __slots__ = ("name", "w", "r", "excl")

    def __init__(self, name, excl=False):
        self.name = name
        self.w = None
        self.r = {}
        self.excl = excl


class Eng:
    def __init__(self, name, sem):
        self.name = name
        self.sem = sem
        self.seen = {}
        self.items = []


class Sched:
    def __init__(self, nc, es):
        self.nc = nc
        self.es = es
        self.eng = {}
        for n in ("pe", "act", "dve", "pool", "sp"):
            self.eng[n] = Eng(n, self.new_sem("s_" + n))
        self.nops = 0

    def new_sem(self, name):
        return Sem(self.es.enter_context(self.nc.semaphore(name)))

    def _need(self, E, ev):
        sem, val = ev
        if E.seen.get(sem, 0) >= val:
            return
        E.items.append(("w", sem, val))
        E.seen[sem] = val

    def op(self, en, fn, reads=(), writes=(), dsem=None, force=False):
        import os
        lim = int(os.environ.get("KLIMIT", "0"))
        if lim and self.nops >= lim and not force:
            return None
        E = self.eng[en]
        if any(b.excl for b in reads):
            writes = list(writes) + [b for b in reads if b.excl]
            reads = [b for b in reads if not b.excl]
        for b in reads:
            if b.w is not None:
                if b.w[0] is E.sem and en == "pe":
                    continue
                self._need(E, b.w)
        for b in writes:
            if b.w is not None and b.w[0] is not E.sem:
                self._need(E, b.w)
            for sem, val in b.r.items():
                if sem is not E.sem:
                    self._need(E, (sem, val))
        if dsem is None:
            sem = E.sem
            sem.cnt += 1
            inc = 1
        else:
            sem = dsem
            sem.cnt += 16
            inc = 16
        ev = (sem, sem.cnt)
        E.items.append(("o", fn, sem, inc))
        for b in reads:
            b.r[sem] = sem.cnt
        for b in writes:
            b.w = ev
            b.r = {}
        self.nops += 1
        return ev

    def wait(self, en, ev):
        self._need(self.eng[en], ev)

    def replay(self, en, e):
        for it in self.eng[en].items:
            if it[0] == "w":
                e.wait_ge(it[1].h, it[2])
            else:
                it[1](e).then_inc(it[2].h, it[3])


class PsTile:
    def __init__(self, ap, bufs):
        self.ap = ap
        self.bufs = bufs


class Prog:
    def __init__(self, NL, NT, T, wdt=F32, sdt=F32, nslot=2, ugroup=4, stage=99):
        self.stage = stage
        self.NL, self.NT, self.T = NL, NT, T
        self.S = NT * T
        self.NCH = T // CH
        self.NBLK = T // 128
        self.wdt, self.sdt = wdt, sdt
        self.nslot = nslot
        self.ugroup = ugroup
        self.NC = C_RESET + T
        self.nc = bass.Bass("TRN2", target_bir_lowering=False)
        self.es = ExitStack()

    def dram(self, name, shape, kind="ExternalInput", dt=F32):
        return self.nc.dram_tensor(name, list(shape), dt, kind=kind).ap()

    def sb(self, name, shape, dt=F32):
        t = self.es.enter_context(self.nc.sbuf_tensor("sb_" + name, list(shape), dt))
        return t

    def build(self):
        nc, NL, T, S = self.nc, self.NL, self.T, self.S
        es = self.es
        with es:
            self.sc = Sched(nc, es)
            self.d_x = self.dram("xT", [D, S])
            self.d_p = self.dram("pT", [NL, PLE, S])
            self.d_win = self.dram("win", [NL, NOC, 128, 1024])
            self.d_wout = self.dram("wout", [NL, 8, 128, 1024])
            self.d_gw = self.dram("gw", [NL, 8, 128, 1024])
            self.d_plew = self.dram("plew", [NL, 8, 128, 256])
            self.d_vecs = self.dram("vecs", [128, NL * NV])
            self.d_sguT = self.dram("sguT", [NL, 128, 512])
            self.d_sgub = self.dram("sgub", [NL, 128, 256])
            self.d_pww = self.dram("pww", [NL, 128, 512])
            self.d_lora = self.dram("lora", [NL, 128, 1024])
            self.d_consts = self.dram("consts", [128, self.NC])
            self.d_y = self.dram("yT", [D, S], kind="ExternalOutput")
            self.alloc()
            self.emit()
            with nc.Block() as block:
                @block.tensor
                def _(e):
                    self.sc.replay("pe", e)

                @block.scalar
                def _(e):
                    self.sc.replay("act", e)

                @block.vector
                def _(e):
                    self.sc.replay("dve", e)

                @block.gpsimd
                def _(e):
                    self.sc.replay("pool", e)

                @block.sync
                def _(e):
                    self.sc.replay("sp", e)
        return nc

    def alloc(self):
        NL, T, NCH = self.NL, self.T, self.NCH
        sb = self.sb
        B = Buf
        self.xt = sb("xt", [128, 8, T]); self.b_xt = [B(f"xt{c}") for c in range(8)]
        self.hT = sb("hT", [128, 8, T], self.wdt); self.b_hT = [B(f"hT{c}") for c in range(8)]
        self.zs = sb("zs", [128, 13, T]); self.b_zs = [B(f"zs{c}") for c in range(13)]
        self.va = sb("va", [128, 2, T]); self.b_va = [B(f"va{c}") for c in range(2)]
        self.mixT = sb("mixT", [128, 8, T], self.wdt); self.b_mix = [B(f"mix{c}") for c in range(8)]
        self.mo = sb("mo", [128, 8, T]); self.b_mo = [B(f"mo{c}") for c in range(8)]
        self.wring = [sb(f"wr{i}", [128, 1024], self.wdt) for i in range(self.nslot)]
        self.b_wring = [B(f"wr{i}") for i in range(self.nslot)]
        self.s_wring = [self.sc.new_sem(f"swr{i}") for i in range(self.nslot)]
        self.wptr = 0
        self.NST = 10
        self.st = [sb(f"st{i}", [128, T]) for i in range(self.NST)]
        self.b_st = [B(f"st{i}") for i in range(self.NST)]
        self.stptr = 0
        self.pt = sb("pt", [128, 2, T], self.wdt); self.b_pt = B("pt"); self.s_pt = self.sc.new_sem("spt")
        self.vecs = sb("vecs", [128, NL * NV]); self.b_vecs = B("vecs")
        self.dv = sb("dv", [128, NL * NDV]); self.b_dv = B("dv")
        self.consts = sb("consts", [128, self.NC]); self.b_consts = B("consts")
        self.s_misc = self.sc.new_sem("smisc")
        self.s_x = self.sc.new_sem("sx")
        self.s_y = self.sc.new_sem("sy")
        self.lp = sb("lp", [128, 512 + 256 + 512 + 1024]); self.b_lp = B("lp"); self.s_lp = self.sc.new_sem("slp")
        self.ybuf = sb("ybuf", [128, 2, HALO + T]); self.b_ybuf = [B(f"ybuf{c}") for c in range(2)]
        self.halo = sb("halo", [128, NL * 2 * HALO]); self.b_halo = B("halo")
        self.zlast = sb("zlast", [128, NL * 13]); self.b_zlast = B("zlast")
        self.NDG = 8
        self.diag = [sb(f"dg{i}", [128, 128]) for i in range(self.NDG)]
        self.b_diag = [B(f"dg{i}") for i in range(self.NDG)]
        self.dgptr = 0
        self.identS = sb("identS", [128, 128], self.sdt); self.b_identS = B("identS")
        self.state = sb("state", [128, NL * 4 * 128], self.sdt)
        self.b_state = [[B(f"state{l}_{p}") for p in range(4)] for l in range(NL)]
        self.vn = sb("vn", [128, 2, T]); self.b_vn = [B(f"vn{c}") for c in range(2)]
        self.vntok = [sb(f"vntok{hh}", [128, self.NBLK, 256]) for hh in range(2)]; self.b_vntok = [B(f"vntok{b}") for b in range(self.NBLK)]
        self.ug = sb("ug", [128, 2, T]); self.b_ug = [B(f"ug{c}") for c in range(2)]
        self.sgb = sb("sgb", [128, 2, T]); self.b_sgb = [B(f"sgb{c}") for c in range(2)]
        self.yc = sb("yc", [128, 2, T]); self.b_yc = [B(f"yc{c}") for c in range(2)]
        self.yn = sb("yn", [128, 2, T]); self.b_yn = [B(f"yn{c}") for c in range(2)]
        self.sgc = sb("sgc", [128, 4, T]); self.b_sgc = [B(f"sgc{c}") for c in range(4)]
        names = ["lw", "aa", "cum", "cume", "E2", "E3", "kk0", "kkn", "bvec"]
        self.pt_sh = {n: sb("c_" + n, [128, T]) for n in names}
        self.b_pt_sh = {n: B("c_" + n) for n in names}
        self.pt_pp = [{n: sb(f"c{pr}_" + n, [128, T]) for n in ("E1", "kmod", "yy")} for pr in range(4)]
        self.b_pt_pp = [{n: B(f"c{pr}_" + n) for n in ("E1", "kmod", "yy")} for pr in range(4)]
        self.Rt4 = [sb(f"Rt{pr}", [128, T], self.sdt) for pr in range(4)]; self.b_Rt4 = [B(f"Rt{pr}") for pr in range(4)]
        self.pads4 = [{n: sb(f"pad{pr}_" + n, [128, NCH * 128], self.sdt) for n in ("A", "B", "K", "V")} for pr in range(4)]
        self.b_pads4 = [{n: [B(f"pad{pr}_{n}{j}") for j in range(NCH)] for n in ("A", "B", "K", "V")} for pr in range(4)]
        self.NU = 4
        self.ut = []
        for u in range(self.NU):
            d = {}
            for n, w in (("A0", 128), ("A1", 128), ("B0", 128), ("B1", 128), ("MakT", 128), ("MrbT", 64), ("MrkT", 64),
                         ("Z0", 256), ("Z1", 256), ("Btok", 128), ("Ktok", 128), ("Vbd", 128), ("Rp", 64), ("G0", 128)):
                d[n] = (sb(f"u{u}_{n}", [128, w], self.sdt), B(f"u{u}_{n}"))
            self.ut.append(d)
        self.psb = [self.es.enter_context(self.nc.psum_tensor(f"ps{b}", [128, 512], F32)) for b in range(8)]
        self.b_psq = [B(f"psbank{b}", excl=True) for b in range(8)]
        self.psptr = 0

    def ps(self, ncols):
        b = self.psptr
        self.psptr = (b + 1) % 8
        return PsTile(self.psb[b][:, 0:ncols], [self.b_psq[b]])

    def stt(self):
        i = self.stptr
        self.stptr = (i + 1) % self.NST
        return self.st[i], self.b_st[i]

    def cst(self, off, n, rows=slice(0, 128)):
        return self.consts[rows, off:off + n]

    def vcol(self, l, col, rows=slice(0, 128)):
        return self.vecs[rows, l * NV + col:l * NV + col + 1]

    def dcol(self, l, col):
        return self.dv[:, l * NDV + col:l * NDV + col + 1]

    def emit(self):
        sc, NL, NT, T = self.sc, self.NL, self.NT, self.T
        sc.op("sp", lambda e: e.dma_start(out=self.consts[:], in_=self.d_consts), writes=[self.b_consts], dsem=self.s_misc)
        sc.op("sp", lambda e: e.dma_start(out=self.vecs[:], in_=self.d_vecs), writes=[self.b_vecs], dsem=self.s_misc)
        ev = (self.s_misc, self.s_misc.cnt)
        self.b_consts.w = ev
        self.b_vecs.w = ev
        for l in range(NL):
            sc.op("dve", lambda e, l=l: e.tensor_scalar(self.dv[:, l * NDV + DV_OMM:l * NDV + DV_OMM + 13],
                                                       self.vecs[:, l * NV + V_MU:l * NV + V_MU + 13], -1.0, 1.0, ALU.mult, ALU.add),
                  reads=[self.b_vecs], writes=[self.b_dv])
            sc.op("dve", lambda e, l=l: e.tensor_scalar(self.dv[:, l * NDV + DV_OMKA:l * NDV + DV_OMKA + 4],
                                                       self.vecs[:, l * NV + V_KA:l * NV + V_KA + 4], -1.0, 1.0, ALU.mult, ALU.add),
                  reads=[self.b_vecs], writes=[self.b_dv])
        sc.op("act", lambda e: e.activation(self.identS[:], self.cst(C_ID, 128), AF.Identity), reads=[self.b_consts], writes=[self.b_identS])
        sc.op("pool", lambda e: e.memset(self.state[:], 0.0), writes=[b for bl in self.b_state for b in bl])
        sc.op("pool", lambda e: e.memset(self.halo[:], 0.0), writes=[self.b_halo])
        sc.op("pool", lambda e: e.memset(self.zlast[:], 0.0), writes=[self.b_zlast])
        for pr in range(4):
            for n in ("A", "B", "K", "V"):
                sc.op("pool", lambda e, n=n, pr=pr: e.memset(self.pads4[pr][n][:], 0.0), writes=self.b_pads4[pr][n])
        for hh in range(2):
            sc.op("pool", lambda e, hh=hh: e.memset(self.vntok[hh][:], 0.0), writes=self.b_vntok)
        for ti in range(NT):
            t0 = ti * T
            sc.op("sp", lambda e, t0=t0: e.dma_start(out=self.xt[:], in_=self.d_x[:, t0:t0 + T].rearrange("(c p) t -> p c t", p=128)),
                  writes=self.b_xt, dsem=self.s_x)
            for l in range(NL):
                self.tile_layer(ti, l)
            ev = sc.op("sp", lambda e, t0=t0: e.dma_start(out=self.d_y[:, t0:t0 + T].rearrange("(c p) t -> p c t", p=128), in_=self.xt[:]),
                       reads=self.b_xt, dsem=self.s_y, force=True)
        sc.wait("sp", (self.s_y, self.s_y.cnt))

    def wload(self, src_ap, ncols=1024):
        i = self.wptr
        self.wptr = (i + 1) % self.nslot
        tile, buf, sem = self.wring[i], self.b_wring[i], self.s_wring[i]
        q = "sp" if self.wdt == F32 else "pool"
        self.sc.op(q, lambda e: e.dma_start(out=tile[:, 0:ncols], in_=src_ap), writes=[buf], dsem=sem)
        return tile, buf

    def rstd_from(self, src_ap, src_bufs, scale, eps, clamp=None):
        sc = self.sc
        t, b = self.stt()
        if clamp is not None:
            sc.op("dve", lambda e: e.tensor_scalar(t[:], src_ap, clamp, None, ALU.max), reads=src_bufs, writes=[b])
            sc.op("act", lambda e: e.activation(t[:], t[:], AF.Ln), reads=[b], writes=[b])
        else:
            sc.op("act", lambda e: e.activation(t[:], src_ap, AF.Ln, bias=float(eps), scale=scale), reads=src_bufs + [self.b_consts], writes=[b])
        sc.op("act", lambda e: e.activation(t[:], t[:], AF.Exp, scale=-0.5), reads=[b], writes=[b])
        return t, b

    def ln_stats(self, x_aps, x_bufs, ones_off, nfeat, eps):
        sc, T = self.sc, self.T
        ones = self.cst(ones_off, 128)
        n = len(x_aps)
        sqs = []
        for i in range(n):
            t, b = self.stt()
            sc.op("dve", lambda e, t=t, i=i: e.tensor_tensor(t[:], x_aps[i], x_aps[i], ALU.mult), reads=[x_bufs[i]], writes=[b])
            sqs.append((t, b))
        p1 = self.ps(T)
        for i in range(n):
            sc.op("pe", lambda e, i=i: e.matmul(p1.ap, ones, x_aps[i], start=(i == 0), stop=(i == n - 1)),
                  reads=[x_bufs[i], self.b_consts], writes=p1.bufs)
        p2 = self.ps(T)
        for i in range(n):
            sc.op("pe", lambda e, i=i: e.matmul(p2.ap, ones, sqs[i][0][:], start=(i == 0), stop=(i == n - 1)),
                  reads=[sqs[i][1], self.b_consts], writes=p2.bufs)
        mean, bm = self.stt()
        sc.op("act", lambda e: e.activation(mean[:], p1.ap, AF.Identity, scale=1.0 / nfeat), reads=p1.bufs, writes=[bm])
        msq, bq = self.stt()
        sc.op("dve", lambda e: e.tensor_tensor(msq[:], mean[:], mean[:], ALU.mult), reads=[bm], writes=[bq])
        var, bv = self.stt()
        sc.op("dve", lambda e: e.scalar_tensor_tensor(var[:], p2.ap, 1.0 / nfeat, msq[:], ALU.mult, ALU.subtract),
              reads=p2.bufs + [bq], writes=[bv])
        rs, brs = self.rstd_from(var[:], [bv], 1.0, eps)
        return mean, bm, rs, brs

    def tile_layer(self, ti, l):
        sc, T, NCH = self.sc, self.T, self.NCH
        t0 = ti * T
        wdt = self.wdt
        ident = self.cst(C_ID, 128)
        ones = self.cst(C_ONES, 128)
        sc.op("sp", lambda e: e.dma_start(out=self.lp[:, 0:512], in_=self.d_sguT[l]), writes=[self.b_lp], dsem=self.s_lp)
        sc.op("sp", lambda e: e.dma_start(out=self.lp[:, 512:768], in_=self.d_sgub[l]), writes=[self.b_lp], dsem=self.s_lp)
        sc.op("sp", lambda e: e.dma_start(out=self.lp[:, 768:1280], in_=self.d_pww[l]), writes=[self.b_lp], dsem=self.s_lp)
        sc.op("sp", lambda e: e.dma_start(out=self.lp[:, 1280:2304], in_=self.d_lora[l]), writes=[self.b_lp], dsem=self.s_lp)
        sguT = self.lp[:, 0:512]
        sc.op("pool", lambda e: e.memset(self.lp[64:128, 0:512].rearrange("p (h i) -> p h i", h=4)[:, :, 0:64], 0.0),
              reads=[self.b_lp], writes=[self.b_lp])
        qd = "sp" if wdt == F32 else "pool"
        sc.op(qd, lambda e: e.dma_start(out=self.pt[:], in_=self.d_p[l, :, t0:t0 + T].rearrange("(c p) t -> p c t", p=128)),
              writes=[self.b_pt], dsem=self.s_pt)

        if self.stage < 1:
            return
        sqs0 = []
        for c in range(8):
            t, b = self.stt()
            sc.op("act", lambda e, t=t, c=c: e.activation(t[:], self.xt[:, c, :], AF.Square), reads=[self.b_xt[c]], writes=[b])
            sqs0.append((t, b))
        pss0 = self.ps(T)
        for c in range(8):
            sc.op("pe", lambda e, c=c: e.matmul(pss0.ap, ones, sqs0[c][0][:], start=(c == 0), stop=(c == 7)),
                  reads=[sqs0[c][1], self.b_consts], writes=pss0.bufs)
        rs0, brs0 = self.rstd_from(pss0.ap, pss0.bufs, 1.0 / D, RMS_EPS)
        for c in range(8):
            sc.op("dve", lambda e, c=c: e.scalar_tensor_tensor(self.hT[:, c, :], self.xt[:, c, :], self.vcol(l, V_PREG + c), rs0[:], ALU.mult, ALU.mult),
                  reads=[self.b_xt[c], brs0, self.b_vecs], writes=[self.b_hT[c]])

        def zchunk(oc):
            wt, wb = self.wload(self.d_win[l, oc])
            p = self.ps(T)
            import os
            if os.environ.get("KDUP"):
                sc.op("pe", lambda e: e.matmul(p.ap, wt[:, 0:128], self.hT[:, 0, :], start=True, stop=True), reads=[wb, self.b_hT[0]], writes=p.bufs)
            for kc in range(8):
                sc.op("pe", lambda e, kc=kc: e.matmul(p.ap, wt[:, kc * 128:(kc + 1) * 128], self.hT[:, kc, :], start=(kc == 0), stop=(kc == 7)),
                      reads=[wb, self.b_hT[kc]], writes=p.bufs)
            return p

        if self.stage < 2.1:
            if self.stage == 1.5:
                for c in range(8):
                    sc.op("act", lambda e, c=c: e.activation(self.xt[:, c, :], self.hT[:, c, :], AF.Identity), reads=[self.b_hT[c]], writes=[self.b_xt[c]])
            return
        def gen_a():
            if self.stage == 2.17:
                wt, wb = self.wload(self.d_win[l, 2])
                p = self.ps(T)
                sc.op("pe", lambda e: e.matmul(p.ap, wt[:, 0:128], self.hT[:, 0, :], start=True, stop=True), reads=[wb, self.b_hT[0]], writes=p.bufs)
                sc.op("act", lambda e: e.activation(self.xt[:, 0, :], p.ap, AF.Identity), reads=p.bufs, writes=[self.b_xt[0]])
                p2 = self.ps(T)
                for kc in range(8):
                    sc.op("pe", lambda e, kc=kc: e.matmul(p2.ap, wt[:, kc * 128:(kc + 1) * 128], self.hT[:, kc, :], start=(kc == 0), stop=(kc == 7)),
                          reads=[wb, self.b_hT[kc]], writes=p2.bufs)
                sc.op("act", lambda e: e.activation(self.xt[:, 1, :], p2.ap, AF.Identity), reads=p2.bufs, writes=[self.b_xt[1]])
                p3 = self.ps(T)
                for kc in range(8):
                    sc.op("pe", lambda e, kc=kc: e.matmul(p3.ap, wt[:, kc * 128:(kc + 1) * 128], self.hT[:, kc, :], start=(kc == 0), stop=(kc == 7)),
                          reads=[wb, self.b_hT[kc]], writes=p3.bufs)
                sc.op("dve", lambda e: e.tensor_copy(self.xt[:, 2, :], p3.ap), reads=p3.bufs, writes=[self.b_xt[2]])
                return
            if self.stage == 2.15:
                for i in range(3):
                    wt, wb = self.wload(self.d_win[l, 2 + i])
                    sc.op("act", lambda e, wt=wt, i=i: e.activation(self.xt[:, i, :], wt[:, 0:256], AF.Identity), reads=[wb], writes=[self.b_xt[i]])
                    sc.op("act", lambda e, wt=wt, i=i: e.activation(self.xt[:, 3 + i, :], wt[:, 768:1024], AF.Identity), reads=[wb], writes=[self.b_xt[3 + i]])
                return
            sga = []
            for c in range(2):
                yield
                p = zchunk(4 + c)
                t, b = self.stt()
                sc.op("act", lambda e, t=t, p=p: e.activation(t[:], p.ap, AF.Silu), reads=p.bufs, writes=[b])
                sga.append((t, b))
            for c in range(2):
                yield
                p = zchunk(0 + c)
                sc.op("dve", lambda e, c=c, p=p: e.tensor_tensor(self.ug[:, c, :], p.ap, sga[c][0][:], ALU.mult),
                      reads=p.bufs + [sga[c][1]], writes=[self.b_ug[c]])
            for c in range(2):
                yield
                p = zchunk(2 + c)
                sc.op("act", lambda e, c=c, p=p: e.activation(self.va[:, c, :], p.ap, AF.Identity), reads=p.bufs, writes=[self.b_va[c]])
            if self.stage < 2.2:
                if self.stage == 2.19:
                    srcs = [(self.va[:, 0, :], self.b_va[0]), (self.va[:, 1, :], self.b_va[1]), (self.ug[:, 0, :], self.b_ug[0]), (self.ug[:, 1, :], self.b_ug[1])]
                    for c in range(4):
                        yield
                        sc.op("act", lambda e, c=c: e.activation(self.xt[:, c, :], srcs[c][0], AF.Identity), reads=[srcs[c][1]], writes=[self.b_xt[c]])
                return
            meanA, bmA, rsA, brsA = self.ln_stats([self.va[:, c, :] for c in range(2)], self.b_va, C_ONES, AW, LN_EPS)
            yield
            if self.stage < 2.4:
                if self.stage == 2.3:
                    srcs = [(self.va[:, 0, :], self.b_va[0]), (self.va[:, 1, :], self.b_va[1]), (self.ug[:, 0, :], self.b_ug[0]), (self.ug[:, 1, :], self.b_ug[1])]
                    for c in range(4):
                        yield
                        sc.op("act", lambda e, c=c: e.activation(self.xt[:, c, :], srcs[c][0], AF.Identity), reads=[srcs[c][1]], writes=[self.b_xt[c]], force=True)
                return
            for c in range(2):
                yield
                t, b = self.stt()
                sc.op("dve", lambda e, c=c, t=t: e.tensor_tensor(t[:], self.va[:, c, :], meanA[:], ALU.subtract), reads=[self.b_va[c], bmA], writes=[b])
                sc.op("dve", lambda e, t=t: e.tensor_tensor(t[:], t[:], rsA[:], ALU.mult), reads=[b, brsA], writes=[b])
                sc.op("act", lambda e, c=c, t=t: e.activation(self.vn[:, c, :], t[:], AF.Identity, bias=self.vcol(l, V_SLB + c), scale=self.vcol(l, V_SLG + c)),
                      reads=[b, self.b_vecs], writes=[self.b_vn[c]])
            if self.stage < 2.6:
                return
            for blk in range(self.NBLK):
                for c in range(2):
                    yield
                    p = self.ps(128)
                    sc.op("pe", lambda e, p=p, c=c, blk=blk: e.transpose(p.ap, self.vn[:, c, blk * 128:(blk + 1) * 128], ident),
                          reads=[self.b_vn[c], self.b_consts], writes=p.bufs)
                    for hh in range(2):
                        sc.op("act", lambda e, p=p, c=c, blk=blk, hh=hh: e.activation(self.vntok[hh][:, blk, c * 128 + hh * 64:c * 128 + hh * 64 + 64], p.ap[:, hh * 64:hh * 64 + 64], AF.Identity),
                              reads=p.bufs, writes=[self.b_vntok[blk]])
            if self.stage < 2.8:
                return
            for blk in range(self.NBLK):
                for c in range(2):
                    yield
                    p = self.ps(128)
                    for hh in range(2):
                        h = 2 * c + hh
                        sc.op("pe", lambda e, p=p, c=c, hh=hh, h=h, blk=blk: e.matmul(
                            p.ap, self.vntok[hh][:, blk, c * 128:(c + 1) * 128],
                            self.lp[:, h * 128:(h + 1) * 128], start=(hh == 0), stop=(hh == 1)),
                            reads=[self.b_vntok[blk], self.b_lp], writes=p.bufs)
                    t, b = self.stt()
                    sc.op("dve", lambda e, p=p, c=c, t=t: e.tensor_tensor(t[:, 0:128], p.ap, self.lp[:, 512 + c * 128:512 + (c + 1) * 128], ALU.add),
                          reads=p.bufs + [self.b_lp], writes=[b])
                    sc.op("dve", lambda e, c=c, t=t, blk=blk: e.tensor_tensor(self.mixT[:, c, blk * 128:(blk + 1) * 128], t[:, 0:128],
                                                                               self.ug[:, c, blk * 128:(blk + 1) * 128], ALU.mult),
                          reads=[b, self.b_ug[c]], writes=[self.b_mix[c]])

            if self.stage < 3:
                return

            yield
        def gen_b():
            sgl = []
            for c in range(2):
                yield
                p = zchunk(8 + c)
                t, b = self.stt()
                sc.op("act", lambda e, t=t, p=p: e.activation(t[:], p.ap, AF.Sigmoid), reads=p.bufs, writes=[b])
                sgl.append((t, b))
            for c in range(2):
                yield
                hoff = (l * 2 + c) * HALO
                sc.op("pool", lambda e, c=c, hoff=hoff: e.tensor_copy(self.ybuf[:, c, 0:HALO], self.halo[:, hoff:hoff + HALO]),
                      reads=[self.b_halo], writes=[self.b_ybuf[c]])
                p = zchunk(6 + c)
                sc.op("dve", lambda e, c=c, p=p: e.tensor_tensor(self.ybuf[:, c, HALO:HALO + T], p.ap, sgl[c][0][:], ALU.mult),
                      reads=p.bufs + [sgl[c][1]], writes=[self.b_ybuf[c]])
                sc.op("pool", lambda e, c=c, hoff=hoff: e.tensor_copy(self.halo[:, hoff:hoff + HALO], self.ybuf[:, c, T:T + HALO]),
                      reads=[self.b_ybuf[c]], writes=[self.b_halo])
            for c in range(2):
                yield
                p = zchunk(10 + c)
                sc.op("act", lambda e, c=c, p=p: e.activation(self.sgb[:, c, :], p.ap, AF.Silu), reads=p.bufs, writes=[self.b_sgb[c]])
            for c in range(2):
                yield
                p = self.ps(T)
                for tap in range(CONVW):
                    di = self.dgptr
                    self.dgptr = (di + 1) % self.NDG
                    dg, dgb = self.diag[di], self.b_diag[di]
                    sc.op("pool", lambda e, dg=dg, c=c, tap=tap: e.tensor_scalar(dg[:], ident, self.vcol(l, V_CW + c * CONVW + tap), None, ALU.mult),
                          reads=[self.b_consts, self.b_vecs], writes=[dgb])
                    sc.op("pe", lambda e, dg=dg, c=c, tap=tap, p=p: e.matmul(p.ap, dg[:], self.ybuf[:, c, tap:tap + T], start=(tap == 0), stop=(tap == CONVW - 1)),
                          reads=[dgb, self.b_ybuf[c]], writes=p.bufs)
                sc.op("act", lambda e, c=c, p=p: e.activation(self.yc[:, c, :], p.ap, AF.Identity, bias=self.vcol(l, V_CB + c)),
                      reads=p.bufs + [self.b_vecs], writes=[self.b_yc[c]])
            meanB, bmB, rsB, brsB = self.ln_stats([self.yc[:, c, :] for c in range(2)], self.b_yc, C_ONES, BW, LN_EPS)
            yield
            for c in range(2):
                yield
                t, b = self.stt()
                sc.op("dve", lambda e, c=c, t=t: e.tensor_tensor(t[:], self.yc[:, c, :], meanB[:], ALU.subtract), reads=[self.b_yc[c], bmB], writes=[b])
                sc.op("dve", lambda e, t=t: e.tensor_tensor(t[:], t[:], rsB[:], ALU.mult), reads=[b, brsB], writes=[b])
                sc.op("act", lambda e, c=c, t=t: e.activation(self.yn[:, c, :], t[:], AF.Silu, bias=self.vcol(l, V_CLB + c), scale=self.vcol(l, V_CLG + c)),
                      reads=[b, self.b_vecs], writes=[self.b_yn[c]])
            for co in range(2):
                yield
                p = self.ps(T)
                for ci in range(2):
                    sc.op("pe", lambda e, p=p, ci=ci, co=co: e.matmul(p.ap, self.lp[:, 768 + ci * 256 + co * 128:768 + ci * 256 + (co + 1) * 128], self.yn[:, ci, :],
                                                                     start=(ci == 0), stop=(ci == 1)),
                          reads=[self.b_lp, self.b_yn[ci]], writes=p.bufs)
                sc.op("dve", lambda e, p=p, co=co: e.scalar_tensor_tensor(self.mixT[:, 2 + co, :], p.ap, self.vcol(l, V_PWB + co), self.sgb[:, co, :], ALU.add, ALU.mult),
                      reads=p.bufs + [self.b_sgb[co], self.b_vecs], writes=[self.b_mix[2 + co]])

            if self.stage < 4:
                return

            yield
        for c in range(4):
            p = zchunk(25 + c)
            sc.op("act", lambda e, c=c, p=p: e.activation(self.sgc[:, c, :], p.ap, AF.Silu), reads=p.bufs, writes=[self.b_sgc[c]])
        for c in range(13):
            p = zchunk(12 + c)
            zl = self.zlast[:, l * 13 + c:l * 13 + c + 1]
            sc.op("act", lambda e, c=c, p=p: e.activation(self.zs[:, c, :], p.ap, AF.Identity, scale=self.dcol(l, DV_OMM + c)),
                  reads=p.bufs + [self.b_dv], writes=[self.b_zs[c]])
            sc.op("dve", lambda e, c=c, p=p: e.scalar_tensor_tensor(self.zs[:, c, 1:T], p.ap[:, 0:T - 1], self.vcol(l, V_MU + c), self.zs[:, c, 1:T], ALU.mult, ALU.add),
                  reads=p.bufs + [self.b_zs[c], self.b_vecs], writes=[self.b_zs[c]])
            sc.op("dve", lambda e, c=c, zl=zl: e.scalar_tensor_tensor(self.zs[:, c, 0:1], zl, self.vcol(l, V_MU + c), self.zs[:, c, 0:1], ALU.mult, ALU.add),
                  reads=[self.b_zlast, self.b_zs[c], self.b_vecs], writes=[self.b_zs[c]])
            sc.op("act", lambda e, p=p, zl=zl: e.activation(zl, p.ap[:, T - 1:T], AF.Identity), reads=p.bufs, writes=[self.b_zlast])
        sc.op("act", lambda e: e.activation(self.zs[0:64, 12, :], self.zs[0:64, 12, :], AF.Tanh), reads=[self.b_zs[12]], writes=[self.b_zs[12]])
        if self.stage < 5 and self.stage != 4.5:
            return
        for pr in range(4):
            self.rwkv_pair(l, pr)
        extra = [gen_a(), gen_b()]
        for j in range(NCH):
            gens = [self.scan_unit(l, pr, j) for pr in range(4)]
            while gens:
                for g in list(gens):
                    try:
                        next(g)
                    except StopIteration:
                        gens.remove(g)
                if extra:
                    try:
                        next(extra[0])
                    except StopIteration:
                        extra.pop(0)
        for g in extra:
            for _ in g:
                pass
        for pr in range(4):
            self.rwkv_post(l, pr)
        if self.stage < 7:
            if self.stage == 4.5:
                srcs = [(self.va[:, 0, :], self.b_va[0]), (self.va[:, 1, :], self.b_va[1]), (self.ug[:, 0, :], self.b_ug[0]), (self.ug[:, 1, :], self.b_ug[1]),
                        (self.sgb[:, 0, :], self.b_sgb[0]), (self.yc[:, 0, :], self.b_yc[0]), (self.zs[:, 0, :], self.b_zs[0]), (self.sgc[:, 0, :], self.b_sgc[0])]
                for c in range(8):
                    sc.op("act", lambda e, c=c: e.activation(self.xt[:, c, :], srcs[c][0], AF.Identity), reads=[srcs[c][1]], writes=[self.b_xt[c]], force=True)
            if self.stage == 6.5:
                for c in range(8):
                    sc.op("act", lambda e, c=c: e.activation(self.xt[:, c, :], self.mixT[:, c, :], AF.Identity), reads=[self.b_mix[c]], writes=[self.b_xt[c]])
            return

        for oc in range(8):
            wt, wb = self.wload(self.d_wout[l, oc])
            p = self.ps(T)
            for kc in range(8):
                sc.op("pe", lambda e, kc=kc, wt=wt, p=p: e.matmul(p.ap, wt[:, kc * 128:(kc + 1) * 128], self.mixT[:, kc, :], start=(kc == 0), stop=(kc == 7)),
                      reads=[wb, self.b_mix[kc]], writes=p.bufs)
            sc.op("act", lambda e, oc=oc, p=p: e.activation(self.mo[:, oc, :], p.ap, AF.Identity), reads=p.bufs, writes=[self.b_mo[oc]])
        sqs1 = []
        for c in range(8):
            t, b = self.stt()
            sc.op("act", lambda e, t=t, c=c: e.activation(t[:], self.mo[:, c, :], AF.Square), reads=[self.b_mo[c]], writes=[b])
            sqs1.append((t, b))
        pss1 = self.ps(T)
        for c in range(8):
            sc.op("pe", lambda e, c=c: e.matmul(pss1.ap, ones, sqs1[c][0][:], start=(c == 0), stop=(c == 7)),
                  reads=[sqs1[c][1], self.b_consts], writes=pss1.bufs)
        rs1, brs1 = self.rstd_from(pss1.ap, pss1.bufs, 1.0 / D, RMS_EPS)
        for c in range(8):
            sc.op("dve", lambda e, c=c: e.scalar_tensor_tensor(self.mo[:, c, :], self.mo[:, c, :], self.vcol(l, V_POSTG + c), rs1[:], ALU.mult, ALU.mult),
                  reads=[self.b_mo[c], brs1, self.b_vecs], writes=[self.b_mo[c]])
            sc.op("dve", lambda e, c=c: e.tensor_tensor(self.xt[:, c, :], self.xt[:, c, :], self.mo[:, c, :], ALU.add),
                  reads=[self.b_xt[c], self.b_mo[c]], writes=[self.b_xt[c]])
        if wdt != F32:
            for c in range(8):
                sc.op("act", lambda e, c=c: e.activation(self.hT[:, c, :], self.xt[:, c, :], AF.Identity), reads=[self.b_xt[c]], writes=[self.b_hT[c]])
            xsrc, xb = self.hT, self.b_hT
        else:
            xsrc, xb = self.xt, self.b_xt
        for oc in range(8):
            wt, wb = self.wload(self.d_gw[l, oc])
            p = self.ps(T)
            for kc in range(8):
                sc.op("pe", lambda e, kc=kc, wt=wt, p=p: e.matmul(p.ap, wt[:, kc * 128:(kc + 1) * 128], xsrc[:, kc, :], start=(kc == 0), stop=(kc == 7)),
                      reads=[wb, xb[kc]], writes=p.bufs)
            sc.op("act", lambda e, oc=oc, p=p: e.activation(self.mo[:, oc, :], p.ap, AF.Sigmoid, bias=self.vcol(l, V_GATEB + oc)),
                  reads=p.bufs + [self.b_vecs], writes=[self.b_mo[oc]])
        for oc in range(8):
            wt, wb = self.wload(self.d_plew[l, oc], 256)
            p = self.ps(T)
            for kc in range(2):
                sc.op("pe", lambda e, kc=kc, wt=wt, p=p: e.matmul(p.ap, wt[:, kc * 128:(kc + 1) * 128], self.pt[:, kc, :], start=(kc == 0), stop=(kc == 1)),
                      reads=[wb, self.b_pt], writes=p.bufs)
            sc.op("dve", lambda e, oc=oc, p=p: e.tensor_tensor(self.mo[:, oc, :], p.ap, self.mo[:, oc, :], ALU.mult),
                  reads=p.bufs + [self.b_mo[oc]], writes=[self.b_mo[oc]])
            sc.op("dve", lambda e, oc=oc: e.tensor_tensor(self.xt[:, oc, :], self.xt[:, oc, :], self.mo[:, oc, :], ALU.add),
                  reads=[self.b_xt[oc], self.b_mo[oc]], writes=[self.b_xt[oc]])

    def rwkv_pair(self, l, pr):
        sc, T, NCH = self.sc, self.T, self.NCH
        ident = self.cst(C_ID, 128)
        blk = self.cst(C_BLK, 128)
        P = dict(self.pt_sh); P.update(self.pt_pp[pr])
        Bf = dict(self.b_pt_sh); Bf.update(self.b_pt_pp[pr])
        pads, b_pads = self.pads4[pr], self.b_pads4[pr]
        Rt, b_Rt = self.Rt4[pr], self.b_Rt4[pr]
        r_ap, r_b = self.zs[:, 0 + pr, :], self.b_zs[0 + pr]
        k_ap, k_b = self.zs[:, 4 + pr, :], self.b_zs[4 + pr]
        v_ap, v_b = self.zs[:, 8 + pr, :], self.b_zs[8 + pr]
        lora_w = self.lp[:, 1280 + pr * 128:1280 + (pr + 1) * 128]
        lora_a = self.lp[:, 1792 + pr * 128:1792 + (pr + 1) * 128]
        p = self.ps(T)
        sc.op("pe", lambda e: e.matmul(p.ap, lora_w, self.zs[:, 12, :], start=True, stop=True), reads=[self.b_lp, self.b_zs[12]], writes=p.bufs)
        sc.op("act", lambda e: e.activation(P["lw"][:], p.ap, AF.Sigmoid, bias=self.vcol(l, V_W0 + pr)), reads=p.bufs + [self.b_vecs], writes=[Bf["lw"]])
        p2 = self.ps(T)
        sc.op("pe", lambda e: e.matmul(p2.ap, lora_a, self.zs[:, 12, :], start=True, stop=True), reads=[self.b_lp, self.b_zs[12]], writes=p2.bufs)
        sc.op("act", lambda e: e.activation(P["aa"][:], p2.ap, AF.Sigmoid, bias=self.vcol(l, V_A0 + pr)), reads=p2.bufs + [self.b_vecs], writes=[Bf["aa"]])
        sc.op("dve", lambda e: e.tensor_scalar(P["lw"][:], P["lw"][:], -DECAY, None, ALU.mult), reads=[Bf["lw"]], writes=[Bf["lw"]])
        sc.op("dve", lambda e: e.tensor_tensor_scan(P["cum"][:], self.cst(C_RESET, T), P["lw"][:], 0.0, ALU.mult, ALU.add),
              reads=[Bf["lw"], self.b_consts], writes=[Bf["cum"]])
        sc.op("dve", lambda e: e.tensor_tensor(P["cume"][:], P["cum"][:], P["lw"][:], ALU.subtract), reads=[Bf["cum"], Bf["lw"]], writes=[Bf["cume"]])
        sc.op("act", lambda e: e.activation(P["E1"][:], P["cum"][:], AF.Exp), reads=[Bf["cum"]], writes=[Bf["E1"]])
        sc.op("act", lambda e: e.activation(P["E2"][:], P["cum"][:], AF.Exp, scale=-1.0), reads=[Bf["cum"]], writes=[Bf["E2"]])
        sc.op("act", lambda e: e.activation(P["E3"][:], P["cume"][:], AF.Exp), reads=[Bf["cume"]], writes=[Bf["E3"]])
        sc.op("dve", lambda e: e.tensor_scalar(P["kk0"][:], k_ap, self.vcol(l, V_KK + pr), None, ALU.mult), reads=[k_b, self.b_vecs], writes=[Bf["kk0"]])
        t, b = self.stt()
        sc.op("act", lambda e: e.activation(t[:], P["kk0"][:], AF.Square), reads=[Bf["kk0"]], writes=[b])
        p3 = self.ps(T)
        sc.op("pe", lambda e: e.matmul(p3.ap, blk, t[:], start=True, stop=True), reads=[b, self.b_consts], writes=p3.bufs)
        rn, brn = self.rstd_from(p3.ap, p3.bufs, 1.0, 0.0, clamp=1e-12)
        sc.op("dve", lambda e: e.tensor_tensor(P["kkn"][:], P["kk0"][:], rn[:], ALU.mult), reads=[Bf["kk0"], brn], writes=[Bf["kkn"]])
        t2, b2 = self.stt()
        sc.op("dve", lambda e: e.tensor_scalar(t2[:], P["aa"][:], self.vcol(l, V_KA + pr), self.dcol(l, DV_OMKA + pr), ALU.mult, ALU.add),
              reads=[Bf["aa"], self.b_vecs, self.b_dv], writes=[b2])
        sc.op("dve", lambda e: e.tensor_tensor(P["kmod"][:], k_ap, t2[:], ALU.mult), reads=[k_b, b2], writes=[Bf["kmod"]])
        sc.op("dve", lambda e: e.tensor_tensor(P["bvec"][:], P["kkn"][:], P["aa"][:], ALU.mult), reads=[Bf["kkn"], Bf["aa"]], writes=[Bf["bvec"]])
        def v3(ap):
            return ap.rearrange("p (j t) -> p j t", t=CH)

        for hh in range(2):
            rows = slice(hh * 64, hh * 64 + 64)
            def pv(n, hh=hh, rows=rows):
                return pads[n][rows, :].rearrange("p (j h t) -> p j h t", h=2, t=CH)[:, :, hh, :]
            sc.op("dve", lambda e, rows=rows, pv=pv: e.scalar_tensor_tensor(pv("A"), v3(P["kkn"][rows, :]), -1.0, v3(P["E3"][rows, :]), ALU.mult, ALU.mult),
                  reads=[Bf["kkn"], Bf["E3"]], writes=b_pads["A"])
            sc.op("dve", lambda e, rows=rows, pv=pv: e.tensor_tensor(pv("B"), v3(P["bvec"][rows, :]), v3(P["E2"][rows, :]), ALU.mult),
                  reads=[Bf["bvec"], Bf["E2"]], writes=b_pads["B"])
            sc.op("dve", lambda e, rows=rows, pv=pv: e.tensor_tensor(pv("K"), v3(P["kmod"][rows, :]), v3(P["E2"][rows, :]), ALU.mult),
                  reads=[Bf["kmod"], Bf["E2"]], writes=b_pads["K"])
            sc.op("act", lambda e, rows=rows, pv=pv: e.activation(pv("V"), v3(self.zs[rows, 8 + pr, :]), AF.Identity),
                  reads=[v_b], writes=b_pads["V"])
        sc.op("dve", lambda e: e.tensor_tensor(Rt[:], r_ap, P["E1"][:], ALU.mult), reads=[r_b, Bf["E1"]], writes=[b_Rt])

    def rwkv_post(self, l, pr):
        sc, T, NCH = self.sc, self.T, self.NCH
        blk = self.cst(C_BLK, 128)
        P = dict(self.pt_sh); P.update(self.pt_pp[pr])
        Bf = dict(self.b_pt_sh); Bf.update(self.b_pt_pp[pr])
        r_ap, r_b = self.zs[:, 0 + pr, :], self.b_zs[0 + pr]
        v_ap, v_b = self.zs[:, 8 + pr, :], self.b_zs[8 + pr]
        yy, byy = P["yy"], Bf["yy"]
        meanC, bmC, rsC, brsC = self.ln_stats([yy[:]], [byy], C_BLK, CH, GN_EPS)
        tpo, bpo = self.stt()
        sc.op("dve", lambda e: e.tensor_tensor(tpo[:], yy[:], meanC[:], ALU.subtract), reads=[byy, bmC], writes=[bpo])
        sc.op("dve", lambda e: e.tensor_tensor(tpo[:], tpo[:], rsC[:], ALU.mult), reads=[bpo, brsC], writes=[bpo])
        sc.op("act", lambda e: e.activation(tpo[:], tpo[:], AF.Identity, bias=self.vcol(l, V_LXB + pr), scale=self.vcol(l, V_LXG + pr)),
              reads=[bpo, self.b_vecs], writes=[bpo])
        t3, b3 = self.stt()
        sc.op("dve", lambda e: e.scalar_tensor_tensor(t3[:], r_ap, self.vcol(l, V_RK + pr), P["kmod"][:], ALU.mult, ALU.mult),
              reads=[r_b, Bf["kmod"], self.b_vecs], writes=[b3])
        p4 = self.ps(T)
        sc.op("pe", lambda e: e.matmul(p4.ap, blk, t3[:], start=True, stop=True), reads=[b3, self.b_consts], writes=p4.bufs)
        sc.op("dve", lambda e: e.tensor_tensor(t3[:], p4.ap, v_ap, ALU.mult), reads=p4.bufs + [v_b], writes=[b3])
        sc.op("dve", lambda e: e.tensor_tensor(tpo[:], tpo[:], t3[:], ALU.add), reads=[bpo, b3], writes=[bpo])
        sc.op("dve", lambda e: e.tensor_tensor(self.mixT[:, 4 + pr, :], tpo[:], self.sgc[:, pr, :], ALU.mult),
              reads=[bpo, self.b_sgc[pr]], writes=[self.b_mix[4 + pr]])

    def scan_unit(self, l, pr, j):
        sc, T = self.sc, self.T
        ident = self.identS[:]
        msu = self.cst(C_MSU, 128)
        msl = self.cst(C_MSL, 128)
        mui = self.cst(C_MUI, 64)
        U = self.ut[pr]
        cs = slice(j * 128, (j + 1) * 128)
        Ap, Bp, Kp, Vp = (self.pads4[pr][n][:, cs] for n in ("A", "B", "K", "V"))
        bA, bB, bK, bV = (self.b_pads4[pr][n][j] for n in ("A", "B", "K", "V"))
        Rst = self.Rt4[pr][:, j * CH:(j + 1) * CH]
        b_Rt = self.b_Rt4[pr]
        CB = self.b_consts

        def mm(out_ps, lhsT, rhs, reads, start=True, stop=True):
            sc.op("pe", lambda e: e.matmul(out_ps.ap, lhsT, rhs, start=start, stop=stop), reads=reads, writes=out_ps.bufs)

        def ev_mask(dst, p, mask):
            sc.op("dve", lambda e: e.tensor_tensor(dst[0][:], p.ap, mask, ALU.mult), reads=p.bufs + [CB], writes=[dst[1]])

        def ev_copy(dst, p, dst_ap=None):
            d = dst[0][:] if dst_ap is None else dst_ap
            sc.op("act", lambda e: e.activation(d, p.ap, AF.Identity), reads=p.bufs, writes=[dst[1]])

        p = self.ps(128); mm(p, Bp, Ap, [bB, bA]); ev_mask(U["B0"], p, msu)
        p = self.ps(128); mm(p, Ap, Bp, [bA, bB]); ev_mask(U["A0"], p, msl)
        p = self.ps(128); mm(p, Kp, Ap, [bK, bA]); ev_mask(U["MakT"], p, msu)
        p = self.ps(64); mm(p, Bp, Rst, [bB, b_Rt]); ev_mask(U["MrbT"], p, mui)
        p = self.ps(64); mm(p, Kp, Rst, [bK, b_Rt]); ev_mask(U["MrkT"], p, mui)
        yield
        def tr(dst, src, bsrc, dst_ap=None):
            p = self.ps(128)
            sc.op("pe", lambda e: e.matmul(p.ap, src, ident, start=True, stop=True), reads=[bsrc, self.b_identS], writes=p.bufs)
            ev_copy(dst, p, dst_ap)
        tr(U["Z0"], Ap, bA, U["Z0"][0][:, 0:128])
        tr(U["Btok"], Bp, bB)
        tr(U["Ktok"], Kp, bK)
        tr(U["Vbd"], Vp, bV)
        yield
        p = self.ps(128); mm(p, U["MakT"][0][:], U["Vbd"][0][:], [U["MakT"][1], U["Vbd"][1]])
        ev_copy(U["Z0"], p, U["Z0"][0][:, 128:256])
        Acur, Bcur, Anext, Bnext = U["A0"], U["B0"], U["A1"], U["B1"]
        Zc, Zn = U["Z0"], U["Z1"]
        for n in range(6):
            yield
            p = self.ps(256)
            mm(p, Bcur[0][:], Zc[0][:], [Bcur[1], Zc[1]])
            sc.op("dve", lambda e, p=p, Zc=Zc, Zn=Zn: e.tensor_tensor(Zn[0][:], p.ap, Zc[0][:], ALU.add), reads=p.bufs + [Zc[1]], writes=[Zn[1]])
            Zc, Zn = Zn, Zc
            if n < 5:
                pb = self.ps(128); mm(pb, Acur[0][:], Bcur[0][:], [Acur[1], Bcur[1]])
                if n < 4:
                    pa = self.ps(128); mm(pa, Bcur[0][:], Acur[0][:], [Acur[1], Bcur[1]])
                ev_copy(Bnext, pb)
                if n < 4:
                    ev_copy(Anext, pa)
                Acur, Anext = Anext, Acur
                Bcur, Bnext = Bnext, Bcur
        yield
        Pm, Qm = Zc[0][:, 0:128], Zc[0][:, 128:256]
        bZ = Zc[1]
        p = self.ps(64)
        mm(p, ident, Rst, [self.b_identS, b_Rt], start=True, stop=False)
        mm(p, Pm, U["MrbT"][0][:], [bZ, U["MrbT"][1]], start=False, stop=True)
        ev_copy(U["Rp"], p)
        p = self.ps(128)
        mm(p, ident, ident, [self.b_identS], start=True, stop=False)
        mm(p, Pm, U["Btok"][0][:], [bZ, U["Btok"][1]], start=False, stop=True)
        ev_copy(U["G0"], p)
        yield
        st_ap = self.state[:, (l * 4 + pr) * 128:(l * 4 + pr + 1) * 128]
        bS = self.b_state[l][pr]
        py = self.ps(64)
        mm(py, Qm, U["MrbT"][0][:], [bZ, U["MrbT"][1]], start=True, stop=False)
        mm(py, U["Vbd"][0][:], U["MrkT"][0][:], [U["Vbd"][1], U["MrkT"][1]], start=False, stop=False)
        mm(py, st_ap, U["Rp"][0][:], [bS, U["Rp"][1]], start=False, stop=True)
        pS = self.ps(128)
        mm(pS, U["Btok"][0][:], Qm, [U["Btok"][1], bZ], start=True, stop=False)
        mm(pS, U["Ktok"][0][:], U["Vbd"][0][:], [U["Ktok"][1], U["Vbd"][1]], start=False, stop=False)
        mm(pS, U["G0"][0][:], st_ap, [U["G0"][1], bS], start=False, stop=True)
        yy, byy = self.pt_pp[pr]["yy"], self.b_pt_pp[pr]["yy"]
        sc.op("act", lambda e: e.activation(yy[:, j * CH:(j + 1) * CH], py.ap, AF.Identity), reads=py.bufs, writes=[byy])
        wc = self.pt_pp[pr]["E1"][:, j * CH + CH - 1:j * CH + CH]
        sc.op("dve", lambda e: e.tensor_scalar(st_ap, pS.ap, wc, None, ALU.mult), reads=pS.bufs + [self.b_pt_pp[pr]["E1"]], writes=[bS])


def make_consts(T):
    NC = C_RESET + T
    c = np.zeros((128, NC), np.float32)
    c[:, C_ID:C_ID + 128] = np.eye(128, dtype=np.float32)
    c[:, C_ONES:C_ONES + 128] = 1.0
    blk = np.zeros((128, 128), np.float32)
    blk[:64, :64] = 1.0
    blk[64:, 64:] = 1.0
    c[:, C_BLK:C_BLK + 128] = blk
    tri_u = np.triu(np.ones((64, 64), np.float32), 1)
    msu = np.zeros((128, 128), np.float32)
    msu[:64, :64] = tri_u
    msu[64:, 64:] = tri_u
    c[:, C_MSU:C_MSU + 128] = msu
    c[:, C_MSL:C_MSL + 128] = msu.T
    ui = np.triu(np.ones((64, 64), np.float32), 0)
    c[:64, C_MUI:C_MUI + 64] = ui
    c[64:, C_MUI:C_MUI + 64] = ui
    c[:64, C_PAD] = 1.0
    c[64:, C_PAD + 1] = 1.0
    c[:64, C_NPAD] = -1.0
    c[64:, C_NPAD + 1] = -1.0
    r = np.ones(T, np.float32)
    r[::CH] = 0.0
    c[:, C_RESET:C_RESET + T] = r[None, :]
    return c


def colmaj(v, n):
    return np.ascontiguousarray(v.reshape(n, 128).T)


def prep_shared(inp, NL, T):
    f = lambda a: np.ascontiguousarray(a, dtype=np.float32)
    sh = {}
    w_in = f(inp["w_in"][:NL])
    sh["win"] = np.ascontiguousarray(w_in.reshape(NL, 8, 128, NOC, 128).transpose(0, 3, 2, 1, 4)).reshape(NL, NOC, 128, 1024)
    sh["wout"] = np.ascontiguousarray(f(inp["w_out"][:NL]).reshape(NL, 8, 128, 8, 128).transpose(0, 3, 2, 1, 4)).reshape(NL, 8, 128, 1024)
    sh["gw"] = np.ascontiguousarray(f(inp["ple_gate_w"][:NL]).reshape(NL, 8, 128, 8, 128).transpose(0, 3, 2, 1, 4)).reshape(NL, 8, 128, 1024)
    sh["plew"] = np.ascontiguousarray(f(inp["ple_w"][:NL]).reshape(NL, 2, 128, 8, 128).transpose(0, 3, 2, 1, 4)).reshape(NL, 8, 128, 256)
    vecs = np.zeros((128, NL * NV), np.float32)
    for l in range(NL):
        o = l * NV
        def put(col, v, n):
            vecs[:, o + col:o + col + n] = colmaj(f(v), n)
        put(V_PREG, inp["pre_norm_g"][l], 8)
        put(V_POSTG, inp["post_norm_g"][l], 8)
        put(V_GATEB, inp["ple_gate_b"][l], 8)
        put(V_SLG, inp["sgu_ln_g"][l], 2)
        put(V_SLB, inp["sgu_ln_b"][l], 2)
        put(V_CB, inp["conv_b"][l], 2)
        put(V_CLG, inp["conv_ln_g"][l], 2)
        put(V_CLB, inp["conv_ln_b"][l], 2)
        put(V_PWB, inp["pw_b"][l], 2)
        put(V_MU, inp["shift_mu"][l], 13)
        put(V_W0, inp["w0"][l], 4)
        put(V_A0, inp["a0"][l], 4)
        put(V_KK, inp["k_k"][l], 4)
        put(V_KA, inp["k_a"][l], 4)
        put(V_RK, inp["r_k"][l].reshape(-1), 4)
        put(V_LXG, inp["lnx_g"][l], 4)
        put(V_LXB, inp["lnx_b"][l], 4)
        cw = f(inp["conv_w"][l])
        for c in range(2):
            vecs[:, o + V_CW + c * CONVW:o + V_CW + (c + 1) * CONVW] = cw[:, c * 128:(c + 1) * 128].T
    sh["vecs"] = vecs
    sh["sguT"] = np.ascontiguousarray(f(inp["sgu_w"][:NL]).transpose(0, 3, 1, 2)).reshape(NL, 128, 512)
    sb = f(inp["sgu_b"][:NL])
    sgub = np.zeros((NL, 128, 256), np.float32)
    for c in range(2):
        for hh in range(2):
            sgub[:, hh * 64:(hh + 1) * 64, c * 128:(c + 1) * 128] = sb[:, 2 * c + hh][:, None, :]
    sh["sgub"] = sgub
    sh["pww"] = np.ascontiguousarray(f(inp["pw_w"][:NL]).reshape(NL, 2, 128, 256).transpose(0, 2, 1, 3)).reshape(NL, 128, 512)
    lora = np.zeros((NL, 128, 1024), np.float32)
    lora[:, 0:64, 0:512] = f(inp["w_up"][:NL])
    lora[:, 64:128, 512:1024] = f(inp["a_up"][:NL])
    sh["lora"] = lora
    sh["consts"] = make_consts(T)
    return sh


_CACHE = {}


def run(inp, NL, NT, T, ncore, **kw):
    key = (NL, NT, T, tuple(sorted(kw.items())))
    S = NT * T
    prog = Prog(NL, NT, T, **kw)
    nc = prog.build()
    sh = prep_shared(inp, NL, T)
    in_maps = []
    for b in range(ncore):
        m = dict(sh)
        m["xT"] = np.ascontiguousarray(np.asarray(inp["x"][b, :S], np.float32).T)
        m["pT"] = np.ascontiguousarray(np.asarray(inp["p"][:NL, b, :S], np.float32).transpose(0, 2, 1))
        in_maps.append(m)
    res = run_bass_kernel_spmd(nc, in_maps, core_ids=list(range(ncore)))
    out = np.stack([np.ascontiguousarray(r["yT"].T) for r in res.results], axis=0)
    return out.astype(np.float32), prog


def kernel(**inputs):
    out, _ = run(inputs, NLAYER, SEQ // 256, 256, NCORE, wdt=BF16, sdt=BF16, nslot=4)
    return out
```
